# Optimizing a Trainium2 kernel written in Bass

```python
import math
import jax, jax.numpy as jnp
from jax import lax
import numpy as np

D_MODEL = 1024
BATCH = 16
SEQ = 2048
DEPTH = 2

GRID_W = 64
CTX_LEN = 256
N_MIXERS = 2
N_SSD_LAYERS = (DEPTH + N_MIXERS - 1) // N_MIXERS
N_ATTN_LAYERS = DEPTH // N_MIXERS
EPS = 1e-6
SSD_EXPAND = 2
D_INNER = SSD_EXPAND * D_MODEL
SSD_HEADDIM = 64
SSD_HEADS = D_INNER // SSD_HEADDIM
SSD_GROUPS = 8
SSD_HPG = SSD_HEADS // SSD_GROUPS
D_STATE = 128
CONV_W = 5
SSD_CHUNK = 128
CONV_DIM = D_INNER + 2 * SSD_GROUPS * D_STATE
SSD_IN_DIM = 2 * D_INNER + 2 * SSD_GROUPS * D_STATE + 2 * SSD_HEADS
HEAD_DIM = 64
N_Q_HEADS = D_MODEL // HEAD_DIM
N_KV_HEADS = 4
Q_PER_KV = N_Q_HEADS // N_KV_HEADS
D_ATTN = N_Q_HEADS * HEAD_DIM
QKV_DIM = (N_Q_HEADS + 2 * N_KV_HEADS) * HEAD_DIM
ROPE_AXIS_DIM = HEAD_DIM // 2
ROPE_THETA = 10000.0
Q_BLOCK = 128
N_EXPERTS = 16
D_EXPERT = D_MODEL
EC_CAPACITY = 2

kernel_name = "hybrid_ssd_gqa_ec_moe_dit"


def rmsnorm(x, w):
    xf = x.astype(jnp.float32)
    y = xf * lax.rsqrt(jnp.mean(xf * xf, axis=-1, keepdims=True) + EPS)
    return (y * w.astype(jnp.float32)).astype(x.dtype)


def dwconv_centred(u, w, b):
    out = lax.conv_general_dilated(
        u, w[:, None, :].astype(u.dtype), window_strides=(1,),
        padding=[(CONV_W // 2, CONV_W // 2)],
        dimension_numbers=('NWC', 'WIO', 'NWC'), feature_group_count=u.shape[-1])
    return out + b.astype(u.dtype)


def ssd_chunked(xs, dt, A, Bm, Cm, h0):
    b, L = xs.shape[:2]
    Q = SSD_CHUNK
    nc = L // Q
    x = xs.reshape(b, nc, Q, SSD_GROUPS, SSD_HPG, SSD_HEADDIM)
    dtc = dt.reshape(b, nc, Q, SSD_GROUPS, SSD_HPG)
    Bc = Bm.reshape(b, nc, Q, SSD_GROUPS, D_STATE)
    Cc = Cm.reshape(b, nc, Q, SSD_GROUPS, D_STATE)
    a = jnp.moveaxis(dtc * A.reshape(SSD_GROUPS, SSD_HPG), 2, -1)
    acum = jnp.cumsum(a, axis=-1)
    tri = jnp.tril(jnp.ones((Q, Q), dtype=bool))
    seg = jnp.exp(jnp.where(tri, acum[..., :, None] - acum[..., None, :], -jnp.inf))
    xdt = x * dtc[..., None].astype(x.dtype)
    cb = jnp.einsum('bclgn,bcsgn->bcgls', Cc, Bc)
    y_diag = jnp.einsum('bcgrls,bcsgrp->bclgrp', cb[:, :, :, None] * seg, xdt)
    decay_end = jnp.exp(acum[..., -1:] - acum)
    states = jnp.einsum('bcsgn,bcgrs,bcsgrp->bcgrpn', Bc, decay_end, xdt)
    chunk_decay = jnp.exp(acum[..., -1])

    def step(h, inp):
        dec, st = inp
        return dec[..., None, None] * h + st, h

    h_last, h_prev = lax.scan(step, h0, (jnp.moveaxis(chunk_decay, 1, 0), jnp.moveaxis(states, 1, 0)))
    h_prev = jnp.moveaxis(h_prev, 0, 1)
    y_off = jnp.einsum('bclgn,bcgrpn,bcgrl->bclgrp', Cc, h_prev, jnp.exp(acum))
    y = (y_diag + y_off).reshape(b, L, SSD_HEADS, SSD_HEADDIM)
    return y, h_last


def ssd_mixer(h_ctx, h_lat, in_w, conv_w, conv_b, dt_bias, A_log, D_skip, norm_w, out_w, ctx_out):
    def project(u):
        b, L = u.shape[:2]
        z, xbc, dtr = jnp.split(u @ in_w, [D_INNER, D_INNER + CONV_DIM], axis=-1)
        xbc = jax.nn.silu(dwconv_centred(xbc, conv_w, conv_b))
        xs, Bm, Cm = jnp.split(xbc, [D_INNER, D_INNER + SSD_GROUPS * D_STATE], axis=-1)
        dt = jax.nn.softplus(dtr.astype(jnp.float32).reshape(b, L, 2, SSD_HEADS) + dt_bias)
        return (z, xs.reshape(b, L, SSD_HEADS, SSD_HEADDIM),
                Bm.reshape(b, L, SSD_GROUPS, D_STATE), Cm.reshape(b, L, SSD_GROUPS, D_STATE), dt)

    zc, xc, Bc, Cc, dtc = project(h_ctx)
    zl, xl, Bl, Cl, dtl = project(h_lat)
    A = -jnp.exp(A_log.astype(jnp.float32))
    b = h_lat.shape[0]
    h0 = jnp.zeros((b, SSD_GROUPS, SSD_HPG, SSD_HEADDIM, D_STATE), jnp.float32)
    flip = lambda t: jnp.flip(t, axis=1)
    yc_f, hc_f = ssd_chunked(xc, dtc[:, :, 0], A[0], Bc, Cc, h0)
    yl_f, _ = ssd_chunked(xl, dtl[:, :, 0], A[0], Bl, Cl, hc_f)
    yc_b, hc_b = ssd_chunked(flip(xc), flip(dtc[:, :, 1]), A[1], flip(Bc), flip(Cc), h0)
    yl_b, _ = ssd_chunked(flip(xl), flip(dtl[:, :, 1]), A[1], flip(Bl), flip(Cl), hc_b)

    def finish(y_f, y_b, xs, z):
        bb, L = xs.shape[:2]
        y = y_f + flip(y_b) + D_skip[:, None] * xs
        y = y.reshape(bb, L, D_INNER).astype(z.dtype)
        return rmsnorm(y * jax.nn.silu(z), norm_w) @ out_w

    out_lat = finish(yl_f, yl_b, xl, zl)
    out_ctx = finish(yc_f, yc_b, xc, zc) if ctx_out else None
    return out_ctx, out_lat


def rope_2d(t, cos, sin):
    half = t.shape[-1] // 2
    t1, t2 = t[..., :half], t[..., half:]
    return jnp.concatenate([t1 * cos - t2 * sin, t2 * cos + t1 * sin], axis=-1)


def attn_mixer(h_ctx, h_lat, qkv_w, q_norm_w, k_norm_w, out_w, cos, sin, ctx_out):
    scale = 1.0 / math.sqrt(HEAD_DIM)

    def project(u):
        b, L = u.shape[:2]
        q, k, v = jnp.split(u @ qkv_w, [D_ATTN, D_ATTN + N_KV_HEADS * HEAD_DIM], axis=-1)
        q = rmsnorm(q.reshape(b, L, N_KV_HEADS, Q_PER_KV, HEAD_DIM), q_norm_w)
        k = rmsnorm(k.reshape(b, L, N_KV_HEADS, HEAD_DIM), k_norm_w)
        return q, k, v.reshape(b, L, N_KV_HEADS, HEAD_DIM)

    qc, kc, vc = project(h_ctx)
    ql, kl, vl = project(h_lat)
    cos = cos.astype(ql.dtype)
    sin = sin.astype(ql.dtype)
    ql = rope_2d(ql, cos[:, None, None], sin[:, None, None])
    kl = rope_2d(kl, cos[:, None], sin[:, None])
    k_all = jnp.concatenate([kl, kc], axis=1)
    v_all = jnp.concatenate([vl, vc], axis=1)

    def attend(q_blk, k, v):
        s = jnp.einsum('bqkgd,bskd->bkgqs', q_blk, k, preferred_element_type=jnp.float32) * scale
        p = jax.nn.softmax(s, axis=-1).astype(v.dtype)
        return jnp.einsum('bkgqs,bskd->bqkgd', p, v)

    b, S = h_lat.shape[:2]
    nb = S // Q_BLOCK
    qb = jnp.moveaxis(ql.reshape(b, nb, Q_BLOCK, N_KV_HEADS, Q_PER_KV, HEAD_DIM), 1, 0)
    o = lax.map(lambda q_blk: attend(q_blk, k_all, v_all), qb)
    out_lat = jnp.moveaxis(o, 0, 1).reshape(b, S, D_ATTN) @ out_w
    out_ctx = None
    if ctx_out:
        bc, Lc = h_ctx.shape[:2]
        out_ctx = attend(qc, kc, vc).reshape(bc, Lc, D_ATTN) @ out_w
    return out_ctx, out_lat


def moe_ec(h, router_w, w1, w3, w2):
    b, T, _ = h.shape
    cap = EC_CAPACITY * T // N_EXPERTS
    logits = jnp.einsum('btd,de->bte', h, router_w, preferred_element_type=jnp.float32)
    aff = jax.nn.softmax(logits, axis=-1)
    gate, idx = lax.top_k(jnp.swapaxes(aff, 1, 2), cap)
    bidx = jnp.arange(b)[:, None, None]
    xg = h[bidx, idx]
    a = jnp.einsum('becd,edf->becf', xg, w1)
    g = jnp.einsum('becd,edf->becf', xg, w3)
    y = jnp.einsum('becf,efd->becd', jax.nn.silu(a) * g, w2)
    y = y * gate[..., None].astype(y.dtype)
    return jnp.zeros_like(h).at[bidx, idx].add(y.astype(h.dtype))


def setup_inputs(seed: int = 0) -> dict:
    key = jax.random.key(seed)
    ks = jax.random.split(key, 26)
    f32 = jnp.float32
    nrm = lambda k, shape, s: jax.random.normal(k, shape, f32) * s
    dt0 = jnp.exp(jax.random.uniform(ks[11], (N_SSD_LAYERS, 2, SSD_HEADS), f32, math.log(1e-3), math.log(1e-1)))
    return {
        "x": nrm(ks[0], (BATCH, SEQ, D_MODEL), 1.0),
        "c": nrm(ks[1], (BATCH, D_MODEL), 1.0),
        "ctx": nrm(ks[2], (BATCH, CTX_LEN, D_MODEL), 1.0),
        "c_ctx": nrm(ks[3], (D_MODEL,), 1.0),
        "ada_w": nrm(ks[4], (DEPTH, D_MODEL, 6 * D_MODEL), D_MODEL ** -0.5),
        "ada_b": nrm(ks[5], (DEPTH, 6 * D_MODEL), 0.02),
        "norm1_w": 1.0 + nrm(ks[6], (DEPTH, D_MODEL), 0.02),
        "norm2_w": 1.0 + nrm(ks[7], (DEPTH, D_MODEL), 0.02),
        "ssd_in_w": nrm(ks[8], (N_SSD_LAYERS, D_MODEL, SSD_IN_DIM), D_MODEL ** -0.5),
        "ssd_conv_w": nrm(ks[9], (N_SSD_LAYERS, CONV_W, CONV_DIM), CONV_W ** -0.5),
        "ssd_conv_b": nrm(ks[10], (N_SSD_LAYERS, CONV_DIM), 0.02),
        "ssd_dt_bias": dt0 + jnp.log(-jnp.expm1(-dt0)),
        "ssd_A_log": jnp.log(jax.random.uniform(ks[12], (N_SSD_LAYERS, 2, SSD_HEADS), f32, 1.0, 16.0)),
        "ssd_D": 1.0 + nrm(ks[13], (N_SSD_LAYERS, SSD_HEADS), 0.1),
        "ssd_norm_w": 1.0 + nrm(ks[14], (N_SSD_LAYERS, D_INNER), 0.02),
        "ssd_out_w": nrm(ks[15], (N_SSD_LAYERS, D_INNER, D_MODEL), D_INNER ** -0.5),
        "attn_qkv_w": nrm(ks[16], (N_ATTN_LAYERS, D_MODEL, QKV_DIM), D_MODEL ** -0.5),
        "attn_q_norm_w": 1.0 + nrm(ks[17], (N_ATTN_LAYERS, HEAD_DIM), 0.02),
        "attn_k_norm_w": 1.0 + nrm(ks[18], (N_ATTN_LAYERS, HEAD_DIM), 0.02),
        "attn_out_w": nrm(ks[19], (N_ATTN_LAYERS, D_ATTN, D_MODEL), D_ATTN ** -0.5),
        "moe_router_w": nrm(ks[20], (DEPTH, D_MODEL, N_EXPERTS), D_MODEL ** -0.5),
        "moe_w1": nrm(ks[21], (DEPTH, N_EXPERTS, D_MODEL, D_EXPERT), D_MODEL ** -0.5),
        "moe_w3": nrm(ks[22], (DEPTH, N_EXPERTS, D_MODEL, D_EXPERT), D_MODEL ** -0.5),
        "moe_w2": nrm(ks[23], (DEPTH, N_EXPERTS, D_EXPERT, D_MODEL), D_EXPERT ** -0.5),
        "final_norm_w": 1.0 + nrm(ks[24], (D_MODEL,), 0.02),
    }


def reference(x, c, ctx, c_ctx, ada_w, ada_b, norm1_w, norm2_w, ssd_in_w, ssd_conv_w, ssd_conv_b,
              ssd_dt_bias, ssd_A_log, ssd_D, ssd_norm_w, ssd_out_w, attn_qkv_w, attn_q_norm_w,
              attn_k_norm_w, attn_out_w, moe_router_w, moe_w1, moe_w3, moe_w2, final_norm_w):
    S = x.shape[1]
    rows = S // GRID_W
    row = jnp.repeat(jnp.arange(rows), GRID_W, total_repeat_length=S).astype(jnp.float32)
    col = jnp.tile(jnp.arange(GRID_W), rows).astype(jnp.float32)
    inv_freq = ROPE_THETA ** (-jnp.arange(0, ROPE_AXIS_DIM, 2, dtype=jnp.float32) / ROPE_AXIS_DIM)
    ang = jnp.concatenate([row[:, None] * inv_freq, col[:, None] * inv_freq], axis=-1)
    cos, sin = jnp.cos(ang), jnp.sin(ang)

    silu_c = jax.nn.silu(c)
    silu_cc = jax.nn.silu(c_ctx)
    for i in range(DEPTH):
        last = i == DEPTH - 1
        j = i // N_MIXERS
        sh1l, sc1l, g1l, sh2l, sc2l, g2l = [m[:, None, :] for m in jnp.split(silu_c @ ada_w[i] + ada_b[i], 6, axis=-1)]
        sh1c, sc1c, g1c, sh2c, sc2c, g2c = jnp.split(silu_cc @ ada_w[i] + ada_b[i], 6, axis=-1)
        h_lat = rmsnorm(x, norm1_w[i]) * (1.0 + sc1l) + sh1l
        h_ctx = rmsnorm(ctx, norm1_w[i]) * (1.0 + sc1c) + sh1c
        if i % N_MIXERS == 0:
            o_ctx, o_lat = ssd_mixer(h_ctx, h_lat, ssd_in_w[j], ssd_conv_w[j], ssd_conv_b[j], ssd_dt_bias[j],
                                     ssd_A_log[j], ssd_D[j], ssd_norm_w[j], ssd_out_w[j], not last)
        else:
            o_ctx, o_lat = attn_mixer(h_ctx, h_lat, attn_qkv_w[j], attn_q_norm_w[j], attn_k_norm_w[j],
                                      attn_out_w[j], cos, sin, not last)
        x = x + g1l * o_lat
        x = x + g2l * moe_ec(rmsnorm(x, norm2_w[i]) * (1.0 + sc2l) + sh2l,
                             moe_router_w[i], moe_w1[i], moe_w3[i], moe_w2[i])
        if not last:
            ctx = ctx + g1c * o_ctx
            ctx = ctx + g2c * moe_ec(rmsnorm(ctx, norm2_w[i]) * (1.0 + sc2c) + sh2c,
                                     moe_router_w[i], moe_w1[i], moe_w3[i], moe_w2[i])
    return rmsnorm(x, final_norm_w)
```

```python
from contextlib import ExitStack
import math
import numpy as np
import concourse.bass as bass
import concourse.mybir as mybir
from concourse.bass_utils import run_bass_kernel_spmd

F32 = mybir.dt.float32
BF16 = mybir.dt.bfloat16
AF = mybir.ActivationFunctionType
ALU = mybir.AluOpType
AX = mybir.AxisListType

ENGS = ["pe", "dve", "act", "pool", "sp"]
NLANES = 28
NLANES_HW = 16


class Res:
    __slots__ = ("w", "r", "name")

    def __init__(self, name=""):
        self.w = None
        self.r = []
        self.name = name


class View:
    __slots__ = ("ap", "res")

    def __init__(self, ap, res):
        self.ap = ap
        self.res = res

    def __getitem__(self, idx):
        return View(self.ap[idx], self.res)

    def rearrange(self, *a, **k):
        return View(self.ap.rearrange(*a, **k), self.res)

    def bitcast(self, dt):
        return View(self.ap.bitcast(dt), self.res)

    def bc(self, axis, n):
        a = self.ap.unsqueeze(axis)
        shp = list(a.shape)
        shp[axis] = n
        return View(a.to_broadcast(shp), self.res)


class Tl:
    def __init__(self, t, nslots=1, name=""):
        self.t = t
        self.nslots = nslots
        self.res = [Res(f"{name}[{i}]") for i in range(nslots)]
        self.name = name

    def __getitem__(self, idx):
        return View(self.t[idx], self.res)

    def all(self):
        return View(self.t[:], self.res)

    def s(self, i, j=None):
        if j is None:
            return View(self.t[:, i], [self.res[i]])
        return View(self.t[:, i:j], self.res[i:j])


class Rot:
    def __init__(self, tiles):
        self.tiles = tiles
        self.i = 0

    def next(self):
        t = self.tiles[self.i % len(self.tiles)]
        self.i += 1
        return t


class Sched:
    def __init__(self, nc, stack):
        self.nc = nc
        self.stack = stack
        self.q = {e: [] for e in ENGS}
        self.sem = {e: stack.enter_context(nc.semaphore(f"s_{e}")) for e in ENGS}
        self.cnt = {e: 0 for e in ENGS}
        self.lane_sem = [stack.enter_context(nc.semaphore(f"l_{i}")) for i in range(NLANES)]
        self.lane_cnt = [0] * NLANES
        self.lane_next = 0
        self.lane_next_sw = 0
        self.seen = {e: {} for e in ENGS}
        self.ninst = 0
        self.psum_banks = []
        self.uid = 0

    def sb(self, name, shape, dtype, nslots=1):
        self.uid += 1
        nm = f"{name}_{self.uid}"
        t = self.stack.enter_context(self.nc.sbuf_tensor(nm, list(shape), dtype))
        return Tl(t, nslots, nm)

    def rot(self, name, shape, dtype, n):
        return Rot([self.sb(f"{name}{i}", shape, dtype) for i in range(n)])

    def dram(self, name, shape, dtype, kind="Internal", nslots=1):
        t = self.nc.dram_tensor(name, list(shape), dtype, kind=kind)
        return Tl(t.ap(), nslots, name)

    def init_psum(self, n=8):
        for i in range(n):
            t = self.stack.enter_context(self.nc.psum_tensor(f"ps{i}", [128, 512], F32))
            self.psum_banks.append(Tl(t, 1, f"ps{i}"))

    def pool(self, idx):
        return Rot([self.psum_banks[i] for i in idx])

    def _collect(self, eng, reads, writes):
        waits = {}

        def need(tok):
            if tok is None:
                return
            kind, key, val = tok
            if kind == "e" and key == eng and eng == "pe":
                return
            k = (kind, key)
            if waits.get(k, 0) < val:
                waits[k] = val

        for v in reads:
            for r in v.res:
                need(r.w)
        for v in writes:
            for r in v.res:
                need(r.w)
                for tok in r.r:
                    need(tok)
        out = []
        seen = self.seen[eng]
        for k, val in waits.items():
            if seen.get(k, 0) >= val:
                continue
            seen[k] = val
            out.append((k, val))
        return out

    def _mark(self, tok, reads, writes):
        for v in writes:
            for r in v.res:
                r.w = tok
                r.r = []
        for v in reads:
            for r in v.res:
                r.r = [t for t in r.r if (t[0], t[1]) != (tok[0], tok[1])]
                r.r.append(tok)

    def _semof(self, k):
        kind, key = k
        return self.sem[key] if kind == "e" else self.lane_sem[key]

    def emit(self, eng, fn, reads=(), writes=()):
        reads = list(reads)
        writes = list(writes)
        waits = self._collect(eng, reads, writes)
        self.cnt[eng] += 1
        val = self.cnt[eng]
        sem = self.sem[eng]
        wl = [(self._semof(k), v) for k, v in waits]

        def run(e, wl=wl, fn=fn, sem=sem):
            for s, v in wl:
                e.wait_ge(s, v)
            fn(e).then_inc(sem, 1)

        self.q[eng].append(run)
        self._mark(("e", eng, val), reads, writes)
        self.ninst += 1

    def dma(self, out, in_, eng="sp", **kw):
        if eng == "pool":
            lane = NLANES_HW + self.lane_next_sw % (NLANES - NLANES_HW)
            self.lane_next_sw += 1
        else:
            lane = self.lane_next % NLANES_HW
            self.lane_next += 1
        reads = [in_]
        writes = [out]
        waits = self._collect(eng, reads, writes)
        prev = self.lane_cnt[lane]
        k = ("d", lane)
        if prev > 0 and self.seen[eng].get(k, 0) < prev:
            self.seen[eng][k] = prev
            waits = [w for w in waits if w[0] != k] + [(k, prev)]
        self.lane_cnt[lane] += 16
        val = self.lane_cnt[lane]
        sem = self.lane_sem[lane]
        wl = [(self._semof(k2), v) for k2, v in waits]
        oap, iap = out.ap, in_.ap

        def run(e, wl=wl, sem=sem, oap=oap, iap=iap, kw=kw):
            for s, v in wl:
                e.wait_ge(s, v)
            e.dma_start(out=oap, in_=iap, **kw).then_inc(sem, 16)

        self.q[eng].append(run)
        self._mark(("d", lane, val), reads, writes)
        self.ninst += 1

    def wait_all(self, eng, views):
        waits = self._collect(eng, list(views), [])
        wl = [(self._semof(k), v) for k, v in waits]

        def run(e, wl=wl):
            for s, v in wl:
                e.wait_ge(s, v)

        self.q[eng].append(run)

    def barrier(self):
        snap_e = dict(self.cnt)
        snap_l = list(self.lane_cnt)
        for eng in ENGS:
            wl = []
            seen = self.seen[eng]
            for e2, v in snap_e.items():
                if v > 0 and seen.get(("e", e2), 0) < v and not (e2 == eng and eng == "pe"):
                    seen[("e", e2)] = v
                    wl.append((self.sem[e2], v))
            for i, v in enumerate(snap_l):
                if v > 0 and seen.get(("d", i), 0) < v:
                    seen[("d", i)] = v
                    wl.append((self.lane_sem[i], v))

            def run(e, wl=wl):
                for s, v in wl:
                    e.wait_ge(s, v)

            self.q[eng].append(run)

    def flush(self):
        nc = self.nc
        q = self.q
        self.q = {e: [] for e in ENGS}
        with nc.Block() as block:
            @block.sync
            def _(e):
                for f in q["sp"]:
                    f(e)

            @block.tensor
            def _(e):
                for f in q["pe"]:
                    f(e)

            @block.vector
            def _(e):
                for f in q["dve"]:
                    f(e)

            @block.scalar
            def _(e):
                for f in q["act"]:
                    f(e)

            @block.gpsimd
            def _(e):
                for f in q["pool"]:
                    f(e)

    def phase_end(self):
        self.barrier()
        self.flush()

    def finish(self, final_views):
        self.wait_all("sp", final_views)
        self.flush()

    def mm(self, out, lhsT, rhs, start=True, stop=True):
        self.emit("pe", lambda e: e.matmul(out.ap, lhsT.ap, rhs.ap, start=start, stop=stop),
                  reads=[lhsT, rhs], writes=[out])

    def tr(self, out, in_, ident):
        self.emit("pe", lambda e: e.transpose(out.ap, in_.ap, ident.ap), reads=[in_, ident], writes=[out])

    def act(self, out, in_, func, bias=None, scale=None, accum_out=None):
        reads = [in_]
        writes = [out]
        kw = {}
        if bias is not None:
            if isinstance(bias, View):
                reads.append(bias)
                kw["bias"] = bias.ap
            else:
                kw["bias"] = bias
        if scale is not None:
            if isinstance(scale, View):
                reads.append(scale)
                kw["scale"] = scale.ap
            else:
                kw["scale"] = scale
        if accum_out is not None:
            writes.append(accum_out)
            kw["accum_out"] = accum_out.ap
        self.emit("act", lambda e: e.activation(out.ap, in_.ap, func, **kw), reads=reads, writes=writes)

    def tt(self, out, a, b, op, eng="dve"):
        self.emit(eng, lambda e: e.tensor_tensor(out=out.ap, in0=a.ap, in1=b.ap, op=op), reads=[a, b], writes=[out])

    def ts(self, out, a, s1, op0, s2=None, op1=None, accum_out=None, eng="dve"):
        reads = [a]
        writes = [out]
        s1v = s1.ap if isinstance(s1, View) else s1
        s2v = s2.ap if isinstance(s2, View) else s2
        if isinstance(s1, View):
            reads.append(s1)
        if isinstance(s2, View):
            reads.append(s2)
        kw = {}
        if op1 is not None:
            kw["op1"] = op1
        if accum_out is not None:
            kw["accum_out"] = accum_out.ap
            writes.append(accum_out)
        self.emit(eng, lambda e: e.tensor_scalar(out=out.ap, in0=a.ap, scalar1=s1v, scalar2=s2v, op0=op0, **kw),
                  reads=reads, writes=writes)

    def stt(self, out, a, s, b, op0, op1, eng="dve"):
        reads = [a, b]
        sv = s.ap if isinstance(s, View) else s
        if isinstance(s, View):
            reads.append(s)
        self.emit(eng, lambda e: e.scalar_tensor_tensor(out=out.ap, in0=a.ap, scalar=sv, in1=b.ap, op0=op0, op1=op1),
                  reads=reads, writes=[out])

    def copy(self, out, in_, eng="dve"):
        if eng == "act":
            self.emit("act", lambda e: e.activation(out.ap, in_.ap, AF.Copy), reads=[in_], writes=[out])
        else:
            self.emit(eng, lambda e: e.tensor_copy(out=out.ap, in_=in_.ap), reads=[in_], writes=[out])

    def memset(self, out, val, eng="dve"):
        self.emit(eng, lambda e: e.memset(out.ap, val), reads=[], writes=[out])

    def reduce(self, out, in_, op, axis=None):
        axis = axis or AX.X
        self.emit("dve", lambda e: e.tensor_reduce(out=out.ap, in_=in_.ap, axis=axis, op=op), reads=[in_], writes=[out])

    def recip(self, out, in_):
        self.emit("dve", lambda e: e.reciprocal(out=out.ap, in_=in_.ap), reads=[in_], writes=[out])


def pbf(bank):
    return View(bank.t[:].bitcast(BF16), bank.res)

D = 1024
LAT = 2048
CTXL = 256
NT = 18
TOK = NT * 128
DI = 2048
NH = 32
NG = 8
EPS = 1e-6
NB = 2


class K:
    pass


def setup_consts(S, k):
    nc = S.nc
    k.iota = S.sb("iota", [128, 256], F32)
    k.pid = S.sb("pid", [128, 1], F32)
    k.pid128 = S.sb("pid128", [128, 1], F32)
    k.eps = S.sb("epsc", [128, 1], F32)
    S.emit("pool", lambda e: e.iota(k.iota.t[:], [[1, 256]], base=0, channel_multiplier=0,
                                    allow_small_or_imprecise_dtypes=True), writes=[k.iota.all()])
    S.emit("pool", lambda e: e.iota(k.pid.t[:], [[0, 1]], base=0, channel_multiplier=1,
                                    allow_small_or_imprecise_dtypes=True), writes=[k.pid.all()])
    S.ts(k.pid128.all(), k.pid.all(), 128.0, ALU.add)
    S.memset(k.eps.all(), EPS)
    io = k.iota[:, 0:128]

    def mk(name, dt, op):
        t = S.sb(name, [128, 128], dt)
        S.ts(t.all(), io, k.pid.all(), op)
        return t

    k.identb = mk("identb", BF16, ALU.is_equal)
    k.identf = mk("identf", F32, ALU.is_equal)
    k.U_le = mk("U_le", BF16, ALU.is_ge)
    k.L_ge = mk("L_ge", BF16, ALU.is_le)
    k.SLt = mk("SLt", BF16, ALU.is_lt)
    k.SUt = mk("SUt", BF16, ALU.is_gt)
    k.U_le_f = mk("U_le_f", F32, ALU.is_ge)
    k.L_ge_f = mk("L_ge_f", F32, ALU.is_le)
    k.ones_f = S.sb("ones_f", [128, 128], F32)
    S.memset(k.ones_f.all(), 1.0)
    k.ones_b = S.sb("ones_b", [128, 128], BF16)
    S.memset(k.ones_b.all(), 1.0)


def load_bc(S, dst, dram_tl, ap):
    S.dma(dst, View(ap.partition_broadcast(dst.ap.shape[0]), dram_tl.res))


def rms_rstd(S, k, rs, src, n, junk):
    ss = k.small.next()
    S.act(junk, src, AF.Square, accum_out=ss[:, 0:1])
    S.act(ss[:, 1:2], ss[:, 0:1], AF.Sqrt, bias=k.eps.all(), scale=1.0 / n)
    S.recip(rs, ss[:, 1:2])


def phase_mod(S, k, layer):
    with ExitStack() as st:
        S.stack = st
        cin = S.sb("cin", [128, 3, 8], F32)
        for b in range(NB):
            S.dma(cin[:, b, :], View(k.c.t[b].rearrange("(p kk) -> p kk", kk=8), k.c.res))
        S.dma(cin[:, 2, :], View(k.c_ctx.t.rearrange("(p kk) -> p kk", kk=8), k.c_ctx.res))
        sc = S.sb("silc", [128, 8, 3], BF16)
        S.act(sc.all().rearrange("p kk v -> p v kk"), cin.all(), AF.Silu)
        bias = S.sb("adab", [3, 6144], F32)
        load_bc(S, bias.all(), k.ada_b, k.ada_b.t[layer])
        nw = S.sb("nw12", [3, 2, 1024], F32)
        load_bc(S, nw[:, 0, :], k.norm1_w, k.norm1_w.t[layer])
        load_bc(S, nw[:, 1, :], k.norm2_w, k.norm2_w.t[layer])
        res = S.sb("modres", [3, 6144], F32)
        wrot = S.rot("adaw", [128, 8, 1024], BF16, 2)
        pp = S.pool([0, 1, 2, 3])
        for blk in range(6):
            w = wrot.next()
            S.dma(w.all(), View(k.ada_w.t[layer].rearrange("(p kk) n -> p kk n", kk=8)[:, :, blk * 1024:(blk + 1) * 1024],
                                k.ada_w.res), eng="pool")
            for half in range(2):
                ps = pp.next()
                for kk in range(8):
                    S.mm(ps[0:3, :], sc[:, kk, :], w[:, kk, half * 512:(half + 1) * 512], start=(kk == 0), stop=(kk == 7))
                c0 = blk * 1024 + half * 512
                S.tt(res[:, c0:c0 + 512], ps[0:3, :], bias[:, c0:c0 + 512], ALU.add)
        for j, blk in enumerate((1, 4)):
            S.stt(res[:, blk * 1024:(blk + 1) * 1024], res[:, blk * 1024:(blk + 1) * 1024], 1.0, nw[:, j, :], ALU.add, ALU.mult)
        S.dma(k.modv[layer].all(), res.all())
        S.phase_end()


def load_mod(S, k, layer, row, blk, dst):
    load_bc(S, dst, k.modv[layer], k.modv[layer].t[row, blk * 1024:(blk + 1) * 1024])


def norm_T(S, k, b, layer, which, src_fn, hT, tiles, pp):
    blkA, blkS = (1, 0) if which == 1 else (4, 3)
    mods = {}
    for kind in (0, 1):
        if not any((ti < 2) == (kind == 0) for ti, _ in tiles):
            continue
        A = S.sb("modA", [128, 1024], F32)
        sh = S.sb("modS", [128, 1024], F32)
        row = 2 if kind == 0 else b
        load_mod(S, k, layer, row, blkA, A.all())
        load_mod(S, k, layer, row, blkS, sh.all())
        mods[kind] = (A, sh)
    xrot = S.rot("nx", [128, 1024], F32, 3)
    trot = S.rot("ntmp", [128, 1024], F32, 2)
    hrot = S.rot("nhb", [128, 1024], BF16, 2)
    for ti, col in tiles:
        A, sh = mods[0 if ti < 2 else 1]
        xt = xrot.next()
        S.dma(xt.all(), src_fn(ti))
        tmp = trot.next()
        rs = k.small.next()
        rms_rstd(S, k, rs[:, 2:3], xt.all(), 1024, tmp.all())
        S.stt(tmp.all(), xt.all(), rs[:, 2:3], A.all(), ALU.mult, ALU.mult)
        hb = hrot.next()
        S.tt(hb.all(), tmp.all(), sh.all(), ALU.add)
        ps = pp.next()
        pv = pbf(ps)
        for kk in range(8):
            S.tr(pv[:, kk * 128:(kk + 1) * 128], hb[:, kk * 128:(kk + 1) * 128], k.identb.all())
        S.copy(hT[:, :, col:col + 128], pv.rearrange("p (kk t) -> p kk t", kk=8), eng="act")

def ssd_mixer(S, k, b, layer, src_fn, dst_fn):
    with ExitStack() as st_outer:
        S.stack = st_outer
        hT = S.sb("hT", [128, 8, TOK], BF16)
        with ExitStack() as st:
            S.stack = st
            norm_T(S, k, b, layer, 1, src_fn, hT, [(i, i * 128) for i in range(NT)], S.pool([0, 1, 2, 3]))
            S.phase_end()
        with ExitStack() as st:
            S.stack = st
            ssd_phase_a(S, k, hT)
            S.phase_end()
        for d in (0, 1):
            with ExitStack() as st:
                S.stack = st
                ssd_scan(S, k, b, layer, hT, d, src_fn, dst_fn)
                S.phase_end()
        S.stack = st_outer
    with ExitStack() as st:
        S.stack = st
        ssd_phase_d(S, k, b, layer, src_fn, dst_fn)
        S.phase_end()
    S.stack = None


def ssd_phase_a(S, k, hT):
    stg = S.sb("cstg", [6, 4096], F32)
    S.dma(stg[0:5, :], k.ssd_conv_w[0])
    S.dma(stg[5:6, :], View(k.ssd_conv_b.t[0:1, :], k.ssd_conv_b.res))
    cw = S.sb("cw", [128, 32, 8], F32)
    pp = S.pool([0, 1, 2, 3])
    for cc0 in range(0, 32, 8):
        ps = pp.next()
        for j in range(8):
            cc = cc0 + j
            S.tr(ps[:, j * 8:j * 8 + 6], stg[0:6, cc * 128:(cc + 1) * 128], k.identf[0:6, 0:6])
        S.copy(cw[:, cc0:cc0 + 8, :], ps[:, 0:64].rearrange("p (j e) -> p j e", e=8))
    wrot = S.rot("wA", [128, 8, 512], BF16, 2)
    urot = S.rot("uA", [128, TOK], F32, 2)
    arot = S.rot("accA", [128, TOK], F32, 2)
    crot = S.rot("cvA", [128, TOK], BF16, 2)
    trot = S.rot("tmA", [128, 8, 128], BF16, 3)
    pp2 = S.pool([4, 5, 6, 7])
    blocks = [(0, 256), (256, 512), (768, 512), (1280, 512), (1792, 512)]
    segs = [(0, 256), (256, TOK)]
    w = None
    for cc in range(32):
        if cc % 4 == 0:
            w = wrot.next()
            c0 = 2048 + cc * 128
            S.dma(w.all(), View(k.ssd_in_w.t[0].rearrange("(kk p) n -> p kk n", p=128)[:, :, c0:c0 + 512], k.ssd_in_w.res),
                  eng="pool")
        u = urot.next()
        for (t0, n) in blocks:
            ps = pp.next()
            for kk in range(8):
                S.mm(ps[:, 0:n], w[:, kk, (cc % 4) * 128:(cc % 4 + 1) * 128], hT[:, kk, t0:t0 + n],
                     start=(kk == 0), stop=(kk == 7))
            S.copy(u[:, t0:t0 + n], ps[:, 0:n], eng="act")
        acc = arot.next()
        for (a, e) in segs:
            S.ts(acc[:, a:e], u[:, a:e], cw[:, cc, 2:3], ALU.mult, s2=cw[:, cc, 5:6], op1=ALU.add)
            for tap in (0, 1, 3, 4):
                dl = tap - 2
                lo = a + max(0, -dl)
                hi = e - max(0, dl)
                S.stt(acc[:, lo:hi], u[:, lo + dl:hi + dl], cw[:, cc, tap:tap + 1], acc[:, lo:hi], ALU.mult, ALU.add)
        cv = crot.next()
        S.act(cv.all(), acc.all(), AF.Silu)
        if cc >= 16:
            S.dma(View(k.bc_d.t[cc - 16], k.bc_d.res), cv.all())
        if cc < 24:
            for i0 in range(0, NT, 8):
                n = min(8, NT - i0)
                ps = pp2.next()
                pv = pbf(ps)
                for j in range(n):
                    S.tr(pv[:, j * 128:(j + 1) * 128], cv[:, (i0 + j) * 128:(i0 + j + 1) * 128], k.identb.all())
                tm = trot.next()
                S.copy(tm[:, 0:n, :], pv[:, 0:n * 128].rearrange("p (j c) -> p j c", j=n))
                S.dma(View(k.xtm_d.t[i0:i0 + n, :, cc * 128:(cc + 1) * 128].rearrange("i t c -> t i c"), k.xtm_d.res),
                      tm[:, 0:n, :])


def ssd_scan(S, k, b, layer, hT, d, src_fn, dst_fn):
    fwd = (d == 0)
    dc = d * 32
    wdt = S.sb("wdt", [128, 8, 64], BF16)
    S.dma(wdt.all(), View(k.ssd_in_w.t[0].rearrange("(kk p) n -> p kk n", p=128)[:, :, 6144:6208], k.ssd_in_w.res), eng="pool")
    dtb = S.sb("dtb", [128, 64], F32)
    load_bc(S, dtb.all(), k.ssd_dt_bias, k.ssd_dt_bias.t[0].rearrange("a h -> (a h)"))
    Abc = S.sb("Abc", [128, 64], F32)
    load_bc(S, Abc.all(), k.ssd_A_log, k.ssd_A_log.t[0].rearrange("a h -> (a h)"))
    S.act(Abc.all(), Abc.all(), AF.Exp)
    S.ts(Abc.all(), Abc.all(), -1.0, ALU.mult)
    h = S.sb("hst", [128, NG, 256], F32, nslots=NG)
    hb = S.sb("hstb", [128, NG, 256], BF16, nslots=NG)
    S.memset(h.all(), 0.0)
    S.memset(hb.all(), 0.0)
    if not fwd:
        wz = S.sb("wz", [128, 8, 2048], BF16)
        for q in range(4):
            S.dma(wz[:, :, q * 512:(q + 1) * 512],
                  View(k.ssd_in_w.t[0].rearrange("(kk p) n -> p kk n", p=128)[:, :, q * 512:(q + 1) * 512], k.ssd_in_w.res), eng="pool")
        Dbc = S.sb("Dbc", [128, 32], F32)
        load_bc(S, Dbc.all(), k.ssd_D, k.ssd_D.t[0])
        nwbc = S.sb("snw", [128, 2048], F32)
        load_bc(S, nwbc.all(), k.ssd_norm_w, k.ssd_norm_w.t[0])
        yfrot = S.rot("yf", [128, 2048], F32, 1)
        szrot = S.rot("sz", [128, 2048], F32, 1)
        gnrot = S.rot("gn", [128, 2048], BF16, 1)
    xbrot = S.rot("xB", [128, 3072], BF16, 2)
    bcrot = S.rot("bct", [128, 16, 128], BF16, 2)
    ydrot = S.rot("ydir", [128, 2048], F32, 2)
    xdtrot = S.rot("xdt", [128, 2048], BF16, 2)
    xddrot = S.rot("xdd", [128, 2048], BF16, 2)
    smrot = S.rot("ssm", [128, 8, 32], F32, 2)
    lharot = S.rot("lha", [128, 32, 128], BF16, 2)
    cbrot = S.rot("cbm", [128, 128], BF16, 4)
    sgrot = S.rot("seg", [128, 512], BF16, 4)
    mtrot = S.rot("MT", [128, 4, 128], BF16, 4)
    t3rot = S.rot("t3", [128, 256], F32, 6)
    ppA = S.pool([0, 1, 2, 3] if fwd else [0, 1, 2])
    ppB = S.pool([3])
    ppY = S.pool([4, 5, 6, 7])
    U = k.U_le if fwd else k.L_ge
    Uf = k.U_le_f if fwd else k.L_ge_f
    LM = k.SLt if fwd else k.SUt
    order = list(range(NT)) if fwd else [1, 0] + list(range(NT - 1, 1, -1))

    def prologue(i):
        c = {}
        c["i"] = i
        xB = c["xB"] = xbrot.next()
        S.dma(xB.all(), View(k.xtm_d.t[i], k.xtm_d.res))
        bct = c["bct"] = bcrot.next()
        S.dma(bct.all(), View(k.bc_d.t[:, :, i * 128:(i + 1) * 128].rearrange("c n t -> n c t"), k.bc_d.res))
        sm = c["sm"] = smrot.next()
        ps = ppA.next()
        for kk in range(8):
            S.mm(ps[:, 0:64], hT[:, kk, i * 128:(i + 1) * 128], wdt[:, kk, :], start=(kk == 0), stop=(kk == 7))
        S.tt(sm[:, 0, :], ps[:, dc:dc + 32], dtb[:, dc:dc + 32], ALU.add)
        S.act(sm[:, 1, :], sm[:, 0, :], AF.Exp)
        S.act(sm[:, 2, :], sm[:, 1, :], AF.Ln, bias=1.0)
        S.tt(sm[:, 3, :], sm[:, 2, :], Abc[:, dc:dc + 32], ALU.mult)
        lha = c["lha"] = lharot.next()
        S.tt(lha.all(), LM.all().bc(1, 32), sm[:, 3, :].bc(2, 128), ALU.mult)
        ps2 = ppA.next()
        S.mm(ps2[:, 0:32], Uf.all(), sm[:, 3, :])
        S.mm(ps2[:, 32:64], k.ones_f.all(), sm[:, 3, :])
        S.copy(sm[:, 4:6, :], ps2[:, 0:64].rearrange("p (a h) -> p a h", a=2))
        S.act(sm[:, 6, :], sm[:, 4, :], AF.Exp)
        S.tt(sm[:, 0, :], sm[:, 5, :], sm[:, 4, :], ALU.subtract)
        S.act(sm[:, 7, :], sm[:, 0, :], AF.Exp)
        S.act(sm[:, 1, :], sm[:, 5, :], AF.Exp)
        S.tt(sm[:, 0, :], sm[:, 2, :], sm[:, 7, :], ALU.mult)
        xdt = c["xdt"] = xdtrot.next()
        xdd = c["xdd"] = xddrot.next()
        x3 = c["x3"] = xB[:, 0:2048].rearrange("p (h j) -> p h j", h=32)
        S.tt(xdt.all().rearrange("p (h j) -> p h j", h=32), x3, sm[:, 2, :].bc(2, 64), ALU.mult)
        S.tt(xdd.all().rearrange("p (h j) -> p h j", h=32), x3, sm[:, 0, :].bc(2, 64), ALU.mult)
        c["ydir"] = ydrot.next()
        return c

    def stA(c, g):
        bct = c["bct"]
        BT = bct[:, g, :]
        CT = bct[:, 8 + g, :]
        pc = ppA.next()
        S.mm(pc[:, 0:128], BT, CT)
        pD = ppA.next()
        for r in range(4):
            S.mm(pD[:, r * 128:(r + 1) * 128], c["lha"][:, 4 * g + r, :], U.all())
        c[("a", g)] = (pc, pD, CT)

    def stB(c, g):
        pc, pD, CT = c[("a", g)]
        cbm = cbrot.next()
        S.tt(cbm.all(), pc[:, 0:128], U.all(), ALU.mult)
        seg = sgrot.next()
        S.act(seg.all(), pD.all(), AF.Exp)
        c[("f", g)] = (cbm, seg, CT)

    def stC1(c, g):
        cbm, seg, CT = c[("f", g)]
        MT = mtrot.next()
        S.tt(MT.all(), seg.all().rearrange("p (r l) -> p r l", r=4), cbm.all().bc(1, 4), ALU.mult)
        c[("m", g)] = MT

    def stC2(c, g):
        cbm, seg, CT = c[("f", g)]
        MT = c[("m", g)]
        xB, xdt, xdd = c["xB"], c["xdt"], c["xdd"]
        py = ppY.next()
        for r in range(4):
            hh = 4 * g + r
            S.mm(py[:, r * 64:(r + 1) * 64], MT[:, r, :], xdt[:, hh * 64:(hh + 1) * 64])
        S.mm(py[:, 256:512], CT, hb.s(g))
        pst = ppY.next()
        S.mm(pst[:, 0:256], xB[:, 2048 + g * 128:2048 + (g + 1) * 128], xdd[:, g * 256:(g + 1) * 256])
        c[("p", g)] = (py, pst)

    def stE(c, g):
        py, pst = c[("p", g)]
        sm, ydir = c["sm"], c["ydir"]
        t3 = t3rot.next()
        S.tt(t3.all().rearrange("p (r j) -> p r j", r=4), py[:, 256:512].rearrange("p (r j) -> p r j", r=4),
             sm[:, 6, 4 * g:4 * g + 4].bc(2, 64), ALU.mult)
        S.tt(ydir[:, g * 256:(g + 1) * 256], t3.all(), py[:, 0:256], ALU.add)
        t4 = t3rot.next()
        S.tt(t4.all().rearrange("p (r j) -> p r j", r=4), h.s(g).rearrange("p (r j) -> p r j", r=4),
             sm[:, 1, 4 * g:4 * g + 4].bc(2, 64), ALU.mult)
        S.tt(h.s(g), t4.all(), pst[:, 0:256], ALU.add)
        S.copy(hb.s(g), h.s(g), eng="act")

    def epilogue(c):
        i = c["i"]
        ydir = c["ydir"]
        if fwd:
            S.dma(View(k.yf_d.t[i], k.yf_d.res), ydir.all())
            return
        yf = yfrot.next()
        S.dma(yf.all(), View(k.yf_d.t[i], k.yf_d.res))
        S.tt(ydir.all(), ydir.all(), yf.all(), ALU.add)
        S.tt(yf.all().rearrange("p (h j) -> p h j", h=32), c["x3"], Dbc.all().bc(2, 64), ALU.mult)
        S.tt(ydir.all(), ydir.all(), yf.all(), ALU.add)
        sz = szrot.next()
        for q in range(4):
            pz = ppB.next()
            for kk in range(8):
                S.mm(pz.all(), hT[:, kk, i * 128:(i + 1) * 128], wz[:, kk, q * 512:(q + 1) * 512],
                     start=(kk == 0), stop=(kk == 7))
            S.act(sz[:, q * 512:(q + 1) * 512], pz.all(), AF.Silu)
        S.tt(ydir.all(), ydir.all(), sz.all(), ALU.mult)
        rs = k.small.next()
        rms_rstd(S, k, rs[:, 2:3], ydir.all(), 2048, sz.all())
        gn = gnrot.next()
        S.stt(gn.all(), ydir.all(), rs[:, 2:3], nwbc.all(), ALU.mult, ALU.mult)
        S.dma(View(k.gn_d.t[i], k.gn_d.res), gn.all())

    ctxs = {0: prologue(order[0])}
    n = len(order) * NG
    for st_ in range(n + 2):
        if st_ < n:
            ci, g = divmod(st_, NG)
            if g == 3 and ci + 1 < len(order):
                ctxs[ci + 1] = prologue(order[ci + 1])
            stA(ctxs[ci], g)
        if 1 <= st_ <= n:
            ci, g = divmod(st_ - 1, NG)
            stC1(ctxs[ci], g)
        if st_ < n:
            ci, g = divmod(st_, NG)
            stB(ctxs[ci], g)
        if 1 <= st_ <= n:
            ci, g = divmod(st_ - 1, NG)
            stC2(ctxs[ci], g)
        if st_ >= 2:
            ci, g = divmod(st_ - 2, NG)
            stE(ctxs[ci], g)
            if g == NG - 1:
                epilogue(ctxs[ci])
                del ctxs[ci]


def ssd_phase_d(S, k, b, layer, src_fn, dst_fn):
    wo = S.sb("wo", [128, 16, 1024], BF16)
    for q in range(4):
        S.dma(wo[:, q * 4:(q + 1) * 4, :],
              View(k.ssd_out_w.t[0].rearrange("(kk p) n -> p kk n", p=128)[:, q * 4:(q + 1) * 4, :], k.ssd_out_w.res), eng="pool")
    g1 = {}
    for kind in (0, 1):
        g1[kind] = S.sb("g1", [128, 1024], F32)
        load_mod(S, k, layer, 2 if kind == 0 else b, 2, g1[kind].all())
    gnrot = S.rot("gnD", [128, 2048], BF16, 2)
    gTrot = S.rot("gT", [128, 16, 128], BF16, 2)
    xrrot = S.rot("xr", [128, 1024], F32, 2)
    xorot = S.rot("xo", [128, 1024], F32, 2)
    ppB = S.pool([0, 1, 2, 3])
    ppC = S.pool([4, 5, 6, 7])
    for i in range(NT):
        kind = 0 if i < 2 else 1
        gn = gnrot.next()
        S.dma(gn.all(), View(k.gn_d.t[i], k.gn_d.res))
        gT = gTrot.next()
        for hf in range(2):
            pt = ppB.next()
            pv = pbf(pt)
            for j in range(8):
                c = hf * 8 + j
                S.tr(pv[:, j * 128:(j + 1) * 128], gn[:, c * 128:(c + 1) * 128], k.identb.all())
            S.copy(gT[:, hf * 8:(hf + 1) * 8, :], pv.rearrange("p (j t) -> p j t", j=8), eng="act")
        xr = xrrot.next()
        S.dma(xr.all(), src_fn(i))
        xo = xorot.next()
        for hf in range(2):
            po = ppC.next()
            for c in range(16):
                S.mm(po.all(), gT[:, c, :], wo[:, c, hf * 512:(hf + 1) * 512], start=(c == 0), stop=(c == 15))
            S.tt(xo[:, hf * 512:(hf + 1) * 512], po.all(), g1[kind][:, hf * 512:(hf + 1) * 512], ALU.mult)
        S.tt(xo.all(), xo.all(), xr.all(), ALU.add)
        S.dma(dst_fn(i), xo.all())

NE = 16
U32 = mybir.dt.uint32


def moe_layer(S, k, layer, xs_fn):
    has_ctx = (layer == 0)
    ngrp = 2 if has_ctx else 1
    GNT = [16, 2]
    GCAP = [256, 32]
    GTI0 = [2, 0]
    NSLOT = 512 + (64 if has_ctx else 0)
    with ExitStack() as st_outer:
        S.stack = st_outer
        slT = [[S.sb(f"slT{g}{b}", [16, GNT[g] * 128], BF16) for b in range(NB)] for g in range(ngrp)]
        with ExitStack() as st_mid:
            S.stack = st_mid
            hn = [[S.sb(f"hn{g}{b}", [128, GNT[g], 1024], BF16) for b in range(NB)] for g in range(ngrp)]
            sl = [S.sb(f"sl{g}", [128, NB, GNT[g], NE], F32) for g in range(ngrp)]
            affhl = [S.sb(f"affhl{g}", [128, NB, GNT[g], NE, 2], BF16) for g in range(ngrp)]
            with ExitStack() as st:
                S.stack = st
                moe_route(S, k, layer, xs_fn, ngrp, GNT, GCAP, GTI0, hn, sl, affhl, slT)
                S.phase_end()
            with ExitStack() as st:
                S.stack = st
                moe_experts(S, k, layer, ngrp, GNT, hn, sl, affhl, NSLOT)
                S.phase_end()
        with ExitStack() as st:
            S.stack = st
            moe_scatter(S, k, layer, xs_fn, ngrp, GNT, GTI0, slT)
            S.phase_end()
    S.stack = None


def moe_route(S, k, layer, xs_fn, ngrp, GNT, GCAP, GTI0, hn, sl, affhl, slT):
    rw = S.sb("rw", [128, 8, NE], F32)
    S.dma(rw.all(), View(k.moe_router_w.t[layer].rearrange("(kk p) e -> p kk e", p=128), k.moe_router_w.res))
    aff = [S.sb(f"aff{g}", [128, NB, GNT[g], NE], F32) for g in range(ngrp)]
    xrot = S.rot("mx", [128, 1024], F32, 3)
    trot = S.rot("mtmp", [128, 1024], F32, 2)
    hfrot = S.rot("mhf", [128, 1024], F32, 2)
    hTrot = S.rot("mhfT", [128, 8, 128], F32, 2)
    erot = S.rot("mexp", [128, NE], F32, 3)
    ppA = S.pool([0, 1, 2, 3])
    ppB = S.pool([4, 5])
    ppC = S.pool([6, 7])
    for g in range(ngrp):
        for b in range(NB):
            A = S.sb("m2A", [128, 1024], F32)
            sh = S.sb("m2S", [128, 1024], F32)
            row = 2 if g == 1 else b
            load_mod(S, k, layer, row, 4, A.all())
            load_mod(S, k, layer, row, 3, sh.all())
            for j in range(GNT[g]):
                ti = GTI0[g] + j
                xt = xrot.next()
                S.dma(xt.all(), xs_fn(b)(ti))
                tmp = trot.next()
                rs = k.small.next()
                rms_rstd(S, k, rs[:, 2:3], xt.all(), 1024, tmp.all())
                S.stt(tmp.all(), xt.all(), rs[:, 2:3], A.all(), ALU.mult, ALU.mult)
                hf = hfrot.next()
                S.tt(hf.all(), tmp.all(), sh.all(), ALU.add)
                S.copy(hn[g][b][:, j, :], hf.all(), eng="act")
                hfT = hTrot.next()
                for q in range(2):
                    ps = ppA.next()
                    for c in range(4):
                        kk = q * 4 + c
                        S.tr(ps[:, c * 128:(c + 1) * 128], hf[:, kk * 128:(kk + 1) * 128], k.identf.all())
                    S.copy(hfT[:, q * 4:(q + 1) * 4, :], ps.all().rearrange("p (c t) -> p c t", c=4), eng="act")
                pl = ppB.next()
                for kk in range(8):
                    S.mm(pl[:, 0:NE], hfT[:, kk, :], rw[:, kk, :], start=(kk == 0), stop=(kk == 7))
                sm = k.small.next()
                S.reduce(sm[:, 0:1], pl[:, 0:NE], ALU.max)
                S.ts(sm[:, 1:2], sm[:, 0:1], -1.0, ALU.mult)
                ex = erot.next()
                S.act(ex.all(), pl[:, 0:NE], AF.Exp, bias=sm[:, 1:2], accum_out=sm[:, 2:3])
                S.recip(sm[:, 3:4], sm[:, 2:3])
                S.ts(aff[g][:, b, j, :], ex.all(), sm[:, 3:4], ALU.mult)
    lo = [S.sb(f"lo{g}", [128, NB, NE], F32) for g in range(ngrp)]
    hi = [S.sb(f"hi{g}", [128, NB, NE], F32) for g in range(ngrp)]
    mid = [S.sb(f"mid{g}", [128, NB, NE], F32) for g in range(ngrp)]
    cnt = [S.sb(f"cnt{g}", [128, NB, NE], F32) for g in range(ngrp)]
    selp = [S.sb(f"selp{g}", [128, NB, NE], F32) for g in range(ngrp)]
    seln = [S.sb(f"seln{g}", [128, NB, NE], F32) for g in range(ngrp)]
    ge = [S.sb(f"ge{g}", [128, NB, GNT[g], NE], BF16) for g in range(ngrp)]
    for g in range(ngrp):
        S.memset(lo[g].all(), 0.0)
        S.memset(hi[g].all(), 1.5)
    for it in range(31):
        for g in range(ngrp):
            nt = GNT[g]
            n = NB * nt * NE
            S.tt(mid[g].all(), lo[g].all(), hi[g].all(), ALU.add)
            S.ts(mid[g].all(), mid[g].all(), 0.5, ALU.mult)
            S.tt(ge[g].all(), aff[g].all(), mid[g].all().bc(2, nt), ALU.is_ge)
            pc = ppC.next()
            S.mm(pc[:, 0:n], k.ones_b.all(), ge[g].all().rearrange("p s t e -> p (s t e)"))
            S.reduce(cnt[g].all(), pc[:, 0:n].rearrange("p (s t e) -> p s e t", s=NB, t=nt), ALU.add)
            S.ts(selp[g].all(), cnt[g].all(), float(GCAP[g]), ALU.is_ge)
            S.ts(seln[g].all(), cnt[g].all(), float(GCAP[g]), ALU.is_lt)
            S.emit("dve", lambda e, g=g: e.copy_predicated(lo[g].t[:], selp[g].t[:].bitcast(U32), mid[g].t[:]),
                   reads=[selp[g].all(), mid[g].all()], writes=[lo[g].all()])
            S.emit("dve", lambda e, g=g: e.copy_predicated(hi[g].t[:], seln[g].t[:].bitcast(U32), mid[g].t[:]),
                   reads=[seln[g].all(), mid[g].all()], writes=[hi[g].all()])
    for g in range(ngrp):
        nt = GNT[g]
        n = NB * nt * NE
        cap = GCAP[g]
        S.tt(ge[g].all(), aff[g].all(), lo[g].all().bc(2, nt), ALU.is_ge)
        gef = ge[g].all().rearrange("p s t e -> p (s t e)")
        pr = ppC.next()
        S.mm(pr[:, 0:n], k.SUt.all(), gef)
        ptot = ppC.next()
        S.mm(ptot[:, 0:n], k.ones_b.all(), gef)
        tot = S.sb(f"tot{g}", [128, NB, nt, NE], F32)
        S.copy(tot.all(), ptot[:, 0:n].rearrange("p (s t e) -> p s t e", s=NB, t=nt))
        off = S.sb(f"off{g}", [128, NB, nt, NE], F32)
        S.memset(off[:, :, 0, :], 0.0)
        for j in range(1, nt):
            S.tt(off[:, :, j, :], off[:, :, j - 1, :], tot[:, :, j - 1, :], ALU.add)
        rank = S.sb(f"rank{g}", [128, NB, nt, NE], F32)
        S.tt(rank.all(), pr[:, 0:n].rearrange("p (s t e) -> p s t e", s=NB, t=nt), off.all(), ALU.add)
        ok = S.sb(f"ok{g}", [128, NB, nt, NE], F32)
        S.ts(ok.all(), rank.all(), float(cap), ALU.is_lt)
        S.tt(ok.all(), ok.all(), ge[g].all(), ALU.mult)
        for b in range(NB):
            offs = 1.0 + (32.0 * b if g == 1 else 0.0)
            S.stt(sl[g][:, b], rank[:, b], offs, ok[:, b], ALU.add, ALU.mult)
        S.ts(sl[g].all(), sl[g].all(), -1.0, ALU.add)
        for b in range(NB):
            for j0 in range(0, nt, 4):
                m = min(4, nt - j0)
                ps = ppA.next()
                for c in range(m):
                    S.tr(ps[0:NE, c * 128:(c + 1) * 128], sl[g][:, b, j0 + c, :], k.identf.all())
                S.copy(slT[g][b][:, j0 * 128:(j0 + m) * 128], ps[0:NE, 0:m * 128])
        S.copy(affhl[g][:, :, :, :, 0], aff[g].all())
        S.tt(affhl[g][:, :, :, :, 1], aff[g].all(), affhl[g][:, :, :, :, 0], ALU.subtract)


def moe_experts(S, k, layer, ngrp, GNT, hn, sl, affhl, NSLOT):
    has_ctx = ngrp == 2
    wrot = S.rot("mw", [128, 8, 1024], BF16, 4)
    xgT = S.sb("xgT", [128, 8, NSLOT], BF16)
    hT = S.sb("mhT", [128, 8, NSLOT], BF16)
    serot = S.rot("Se", [128, 16, 256], BF16, 2)
    secrot = S.rot("SeC", [128, NB, 2, 64], BF16, 2)
    sarot = S.rot("msa", [128, 512], F32, 2)
    yrot = S.rot("my", [128, 1024], BF16, 2)
    gates = S.rot("mgate", [128, 8], F32, 2)
    ppA = S.pool([0, 1, 2])
    ppB = S.pool([3, 4, 5])
    ppG = S.pool([6, 7])
    nchunks = [(0, 512)] + ([(512, 64)] if has_ctx else [])
    jchunks = [(0, 128), (128, 128), (256, 128), (384, 128)] + ([(512, 64)] if has_ctx else [])
    wsrc = {1: k.moe_w1, 3: k.moe_w3, 2: k.moe_w2}

    def loadw(which, e):
        w = wrot.next()
        src = wsrc[which]
        for q in range(2):
            S.dma(w[:, q * 4:(q + 1) * 4, :],
                  View(src.t[layer, e].rearrange("(kk p) n -> p kk n", p=128)[:, q * 4:(q + 1) * 4, :], src.res), eng="pool")
        return w

    for e in range(NE):
        w1 = loadw(1, e)
        w3 = loadw(3, e)
        gt = gates.next()
        pgt = ppG.next()
        for b in range(NB):
            Se = serot.next()
            for tt_ in range(16):
                S.ts(Se[:, tt_, :], k.iota[:, 0:256], sl[0][:, b, tt_, e:e + 1], ALU.is_equal)
            for kk0 in range(0, 8, 2):
                pg = ppA.next()
                for c in range(2):
                    kk = kk0 + c
                    for tt_ in range(16):
                        S.mm(pg[:, c * 256:(c + 1) * 256], hn[0][b][:, tt_, kk * 128:(kk + 1) * 128], Se[:, tt_, :],
                             start=(tt_ == 0), stop=(tt_ == 15))
                S.copy(xgT[:, kk0:kk0 + 2, b * 256:(b + 1) * 256], pg.all().rearrange("p (c j) -> p c j", c=2), eng="act")
            for jc in range(2):
                col = (b * 2 + jc) * 2
                for tt_ in range(16):
                    S.mm(pgt[:, col:col + 2], Se[:, tt_, jc * 128:(jc + 1) * 128], affhl[0][:, b, tt_, e, :],
                         start=(tt_ == 0), stop=(tt_ == 15))
        if has_ctx:
            SeC = secrot.next()
            for b in range(NB):
                for tt_ in range(2):
                    S.ts(SeC[:, b, tt_, :], k.iota[:, 0:64], sl[1][:, b, tt_, e:e + 1], ALU.is_equal)
            for kk0 in range(0, 8, 4):
                pg = ppA.next()
                for c in range(4):
                    kk = kk0 + c
                    n = 0
                    for b in range(NB):
                        for tt_ in range(2):
                            S.mm(pg[:, c * 64:(c + 1) * 64], hn[1][b][:, tt_, kk * 128:(kk + 1) * 128], SeC[:, b, tt_, :],
                                 start=(n == 0), stop=(n == 3))
                            n += 1
                S.copy(xgT[:, kk0:kk0 + 4, 512:576], pg[:, 0:256].rearrange("p (c j) -> p c j", c=4), eng="act")
            n = 0
            for b in range(NB):
                for tt_ in range(2):
                    S.mm(pgt[0:64, 8:10], SeC[:, b, tt_, :], affhl[1][:, b, tt_, e, :], start=(n == 0), stop=(n == 3))
                    n += 1
        ncol = 5 if has_ctx else 4
        S.reduce(gt[:, 0:ncol], pgt[:, 0:2 * ncol].rearrange("p (c two) -> p c two", two=2), ALU.add)
        for fc in range(8):
            for (n0, n) in nchunks:
                pa = ppA.next()
                for kk in range(8):
                    S.mm(pa[:, 0:n], w1[:, kk, fc * 128:(fc + 1) * 128], xgT[:, kk, n0:n0 + n], start=(kk == 0), stop=(kk == 7))
                pb = ppB.next()
                for kk in range(8):
                    S.mm(pb[:, 0:n], w3[:, kk, fc * 128:(fc + 1) * 128], xgT[:, kk, n0:n0 + n], start=(kk == 0), stop=(kk == 7))
                sa = sarot.next()
                S.act(sa[:, 0:n], pa[:, 0:n], AF.Silu)
                S.tt(hT[:, fc, n0:n0 + n], sa[:, 0:n], pb[:, 0:n], ALU.mult)
        w2 = loadw(2, e)
        for ji, (j0, m) in enumerate(jchunks):
            y = yrot.next()
            for hf in range(2):
                py = ppB.next()
                for fc in range(8):
                    S.mm(py[0:m, :], hT[:, fc, j0:j0 + m], w2[:, fc, hf * 512:(hf + 1) * 512], start=(fc == 0), stop=(fc == 7))
                S.act(y[0:m, hf * 512:(hf + 1) * 512], py[0:m, :], AF.Copy, scale=gt[0:m, ji:ji + 1])
            S.dma(View(k.y_d.t[e, j0:j0 + m, :], k.y_d.res), y[0:m, :])


def moe_scatter(S, k, layer, xs_fn, ngrp, GNT, GTI0, slT):
    sel16 = S.sb("sel16", [16, NE, 128], BF16)
    selio = S.sb("selio", [16, NE, 128], F32)
    S.emit("pool", lambda e: e.iota(selio.t[:], [[1, NE], [0, 128]], base=0, channel_multiplier=0,
                                    allow_small_or_imprecise_dtypes=True), writes=[selio.all()])
    S.ts(sel16.all(), selio.all(), k.pid[0:16, :], ALU.is_equal)
    pid32 = S.sb("pid32", [128, 1], F32)
    S.ts(pid32.all(), k.pid.all(), 32.0, ALU.add)
    yall = S.sb("yall", [128, 2 * NE, 1024], BF16)
    strot = S.rot("ST", [128, 2, NE, 128], BF16, 2)
    xrot = S.rot("sx", [128, 1024], F32, 2)
    xorot = S.rot("sxo", [128, 1024], F32, 2)
    ppA = S.pool([0, 1, 2, 3])
    ppB = S.pool([4, 5, 6, 7])
    for g in range(ngrp):
        for b in range(NB):
            g2 = S.sb("g2v", [128, 1024], F32)
            load_mod(S, k, layer, 2 if g == 1 else b, 5, g2.all())
            if g == 0:
                kr = 128
                njc = 2
                for e in range(NE):
                    S.dma(yall[:, 2 * e:2 * e + 2, :],
                          View(k.y_d.t[e, b * 256:(b + 1) * 256, :].rearrange("(jc j) d -> j jc d", jc=2), k.y_d.res))
                cmpv = [k.pid, k.pid128]
            else:
                kr = 32
                njc = 1
                S.dma(yall[0:32, 0:NE, :],
                      View(k.y_d.t[:, 512 + b * 32:512 + (b + 1) * 32, :].rearrange("e j d -> j e d"), k.y_d.res))
                cmpv = [k.pid if b == 0 else pid32]
            for j in range(GNT[g]):
                ti = GTI0[g] + j
                banks = [ppA.next() for _ in range(4)]
                for e in range(NE):
                    S.mm(banks[e // 4][:, (e % 4) * 128:(e % 4 + 1) * 128], sel16[:, e, :], slT[g][b][:, j * 128:(j + 1) * 128])
                ST = strot.next()
                for jc in range(njc):
                    for q in range(4):
                        S.ts(ST[:, jc, q * 4:(q + 1) * 4, :], banks[q].all().rearrange("p (e t) -> p e t", e=4),
                             cmpv[jc].all(), ALU.is_equal)
                xt = xrot.next()
                S.dma(xt.all(), xs_fn(b)(ti))
                xo = xorot.next()
                for hf in range(2):
                    po = ppB.next()
                    n = 0
                    tot = NE * njc
                    for e in range(NE):
                        for jc in range(njc):
                            yi = 2 * e + jc if g == 0 else e
                            S.mm(po.all(), ST[0:kr, jc, e, :], yall[0:kr, yi, hf * 512:(hf + 1) * 512],
                                 start=(n == 0), stop=(n == tot - 1))
                            n += 1
                    S.tt(xo[:, hf * 512:(hf + 1) * 512], po.all(), g2[:, hf * 512:(hf + 1) * 512], ALU.mult)
                S.tt(xo.all(), xo.all(), xt.all(), ALU.add)
                S.dma(xs_fn(b)(ti), xo.all())

def attn_mixer(S, k, b, layer, xs):
    NLT = NT - 2
    with ExitStack() as st_outer:
        S.stack = st_outer
        qT = S.sb("qT", [128, 8, LAT], BF16)
        kTd = S.sb("kTd", [128, 4, TOK], BF16)
        vaug = S.sb("vaug", [128, NT, 4, 72], BF16)
        vaugO = S.sb("vaugO", [128, NT, 4, 128], BF16)
        with ExitStack() as st_h:
            S.stack = st_h
            hT = S.sb("ahT", [128, 8, TOK], BF16)
            with ExitStack() as st:
                S.stack = st
                norm_T(S, k, b, layer, 1, xs, hT, [(i, i * 128) for i in range(NT)], S.pool([0, 1, 2, 3]))
                S.phase_end()
            import os
            if os.environ.get("ATT_STOP") != "norm":
                with ExitStack() as st:
                    S.stack = st
                    attn_qkv(S, k, hT, qT, kTd, vaug, vaugO)
                    S.phase_end()
        import os
        if os.environ.get("ATT_STOP") not in ("qkv", "norm"):
            with ExitStack() as st:
                S.stack = st
                attn_core(S, k, b, layer, xs, qT, kTd, vaug, vaugO)
                S.phase_end()
    S.stack = None


def attn_qkv(S, k, hT, qT, kTd, vaug, vaugO):
    w = S.sb("wqkv", [128, 8, 1536], BF16)
    for q in range(3):
        S.dma(w[:, :, q * 512:(q + 1) * 512],
              View(k.attn_qkv_w.t[0].rearrange("(kk p) n -> p kk n", p=128)[:, :, q * 512:(q + 1) * 512], k.attn_qkv_w.res), eng="pool")
    qw = S.sb("qwbc", [128, 64], F32)
    kw = S.sb("kwbc", [128, 64], F32)
    load_bc(S, qw.all(), k.attn_q_norm_w, k.attn_q_norm_w.t[0])
    load_bc(S, kw.all(), k.attn_k_norm_w, k.attn_k_norm_w.t[0])
    S.memset(vaug.all(), 1.0)
    S.memset(vaugO.all(), 0.0)
    qkrot = S.rot("qk", [128, 20, 64], F32, 2)
    sqrot = S.rot("qsq", [128, 20, 64], F32, 1)
    qnrot = S.rot("qn", [128, 20, 64], F32, 2)
    qrrot = S.rot("qr", [128, 20, 64], BF16, 2)
    kdrot = S.rot("kd", [128, 4, 2, 64], BF16, 2)
    csrot = S.rot("cs", [128, 2, 32], F32, 2)
    r1rot = S.rot("rp1", [128, 20, 32], F32, 2)
    r2rot = S.rot("rp2", [128, 20, 32], F32, 2)
    ssrot = S.rot("qss", [128, 20], F32, 3)
    ppA = S.pool([0, 1, 2, 3, 4, 5])
    ppB = S.pool([6, 7])
    import os
    LVL = int(os.environ.get("QKV_LVL", "9"))
    if LVL < 1:
        return
    for i in range(NT):
        banks = []
        for nb_ in range(3):
            ps = ppA.next()
            for kk in range(8):
                S.mm(ps.all(), hT[:, kk, i * 128:(i + 1) * 128], w[:, kk, nb_ * 512:(nb_ + 1) * 512], start=(kk == 0), stop=(kk == 7))
            banks.append(ps)
        VAR = os.environ.get("QKV_VAR", "Z")
        if VAR == "A":
            continue
        qk = qkrot.next()
        S.copy(qk[:, 0:8, :], banks[0].all().rearrange("p (h d) -> p h d", h=8), eng="act")
        S.copy(qk[:, 8:16, :], banks[1].all().rearrange("p (h d) -> p h d", h=8), eng="act")
        S.copy(qk[:, 16:20, :], banks[2][:, 0:256].rearrange("p (h d) -> p h d", h=4), eng="act")
        if VAR == "B":
            continue
        S.copy(vaug[:, i, :, 0:64], banks[2][:, 256:512].rearrange("p (h d) -> p h d", h=4), eng="act")
        S.copy(vaugO[:, i, :, 64:128], banks[2][:, 256:512].rearrange("p (h d) -> p h d", h=4), eng="act")
        S.copy(vaugO[:, i, :, 0:1], k.ones_b[:, 0:4].rearrange("p (h o) -> p h o", o=1))
        if LVL < 2:
            continue
        sq = sqrot.next()
        S.tt(sq.all(), qk.all(), qk.all(), ALU.mult)
        ss = ssrot.next()
        S.reduce(ss.all(), sq.all(), ALU.add)
        S.act(ss.all(), ss.all(), AF.Ln, bias=k.eps.all(), scale=1.0 / 64)
        S.act(ss.all(), ss.all(), AF.Exp, scale=-0.5)
        qn = qnrot.next()
        S.tt(qn.all(), qk.all(), ss.all().bc(2, 64), ALU.mult)
        S.tt(qn[:, 0:16, :], qn[:, 0:16, :], qw.all().bc(1, 16), ALU.mult)
        S.tt(qn[:, 16:20, :], qn[:, 16:20, :], kw.all().bc(1, 4), ALU.mult)
        if LVL < 3:
            continue
        qr = qrrot.next()
        if i >= 2:
            cs = csrot.next()
            t0 = (i - 2) * 128
            S.dma(cs[:, 0, :], View(k.cos.t[t0:t0 + 128, :], k.cos.res))
            S.dma(cs[:, 1, :], View(k.sin.t[t0:t0 + 128, :], k.sin.res))
            cosb = cs[:, 0, :].bc(1, 20)
            sinb = cs[:, 1, :].bc(1, 20)
            t1 = qn[:, :, 0:32]
            t2 = qn[:, :, 32:64]
            a1 = r1rot.next()
            a2 = r2rot.next()
            S.tt(a1.all(), t1, cosb, ALU.mult)
            S.tt(a2.all(), t2, sinb, ALU.mult)
            S.tt(qr[:, :, 0:32], a1.all(), a2.all(), ALU.subtract)
            a3 = r1rot.next()
            a4 = r2rot.next()
            S.tt(a3.all(), t2, cosb, ALU.mult)
            S.tt(a4.all(), t1, sinb, ALU.mult)
            S.tt(qr[:, :, 32:64], a3.all(), a4.all(), ALU.add)
        else:
            S.copy(qr.all(), qn.all())
        if LVL < 4:
            continue
        if i >= 2:
            ps = ppB.next()
            pv = pbf(ps)
            for pr in range(8):
                S.tr(pv[:, pr * 128:(pr + 1) * 128], qr[:, 2 * pr:2 * pr + 2, :].rearrange("p h d -> p (h d)"), k.identb.all())
            S.copy(qT[:, :, (i - 2) * 128:(i - 1) * 128], pv.rearrange("p (c t) -> p c t", c=8), eng="act")
        if LVL < 5:
            continue
        kd = kdrot.next()
        S.copy(kd[:, :, 0, :], qr[:, 16:20, :])
        S.copy(kd[:, :, 1, :], qr[:, 16:20, :], eng="act")
        ps = ppB.next()
        pv = pbf(ps)
        for kv in range(4):
            S.tr(pv[:, kv * 128:(kv + 1) * 128], kd[:, kv].rearrange("p a d -> p (a d)"), k.identb.all())
        S.copy(kTd[:, :, i * 128:(i + 1) * 128], pv[:, 0:512].rearrange("p (c t) -> p c t", c=4))


def attn_core(S, k, b, layer, xs, qT, kTd, vaug, vaugO):
    wo = S.sb("awo", [128, 8, 1024], BF16)
    for q in range(2):
        S.dma(wo[:, q * 4:(q + 1) * 4, :],
              View(k.attn_out_w.t[0].rearrange("(kk p) n -> p kk n", p=128)[:, q * 4:(q + 1) * 4, :], k.attn_out_w.res), eng="pool")
    g1 = S.sb("ag1", [128, 1024], F32)
    load_mod(S, k, layer, b, 2, g1.all())
    ptrot = S.rot("PT", [128, 512], BF16, 3)
    oTrot = S.rot("aoT", [128, 8, 512], BF16, 2)
    rdrot = S.rot("rden", [128, 512], F32, 2)
    rhrot = S.rot("rdhl", [128, 2, 512], BF16, 2)
    nmrot = S.rot("onum", [128, 512], F32, 2)
    xrot = S.rot("ax", [128, 1024], F32, 2)
    xorot = S.rot("axo", [128, 1024], F32, 2)
    ppS = S.pool([0, 1, 2])
    ppAcc = S.pool([3, 4])
    ppBc = S.pool([5])
    ppO = S.pool([6, 7])
    for tqb in range(4):
        tq0 = tqb * 512
        oT = oTrot.next()
        steps = [(h, tk) for h in range(16) for tk in range(NT)]

        def emit_scores(h, tk):
            pair, e = h // 2, h % 2
            kv = h // 4
            p0, p1 = e * 64, (e + 1) * 64
            psc = ppS.next()
            S.mm(psc.all(), kTd[p0:p1, kv, tk * 128:(tk + 1) * 128], qT[p0:p1, pair, tq0:tq0 + 512])
            return psc

        pending = emit_scores(*steps[0])
        acc = None
        for si, (h, tk) in enumerate(steps):
            pair, e = h // 2, h % 2
            kv = h // 4
            psc = pending
            if si + 1 < len(steps):
                pending = emit_scores(*steps[si + 1])
            if tk == 0:
                acc = ppAcc.next()
            PT = ptrot.next()
            S.act(PT.all(), psc.all(), AF.Exp, scale=0.125)
            if e == 0:
                S.mm(acc[0:65, :], vaug[:, tk, kv, 0:65], PT.all(), start=(tk == 0), stop=(tk == NT - 1))
            else:
                S.mm(acc.all(), vaugO[:, tk, kv, :], PT.all(), start=(tk == 0), stop=(tk == NT - 1))
            if tk == NT - 1:
                rd = rdrot.next()
                nm = nmrot.next()
                bc = ppBc.next()
                rh = rhrot.next()
                pr0 = 64 if e == 0 else 0
                S.recip(rd[pr0:pr0 + 1, :], acc[pr0:pr0 + 1, :])
                S.copy(rh[pr0:pr0 + 1, 0, :], rd[pr0:pr0 + 1, :])
                S.tt(rh[pr0:pr0 + 1, 1, :], rd[pr0:pr0 + 1, :], rh[pr0:pr0 + 1, 0, :], ALU.subtract)
                if e == 0:
                    S.mm(bc[0:64, :], k.ones_b[64:65, 0:64], rh[64:65, 0, :], start=True, stop=False)
                    S.mm(bc[0:64, :], k.ones_b[64:65, 0:64], rh[64:65, 1, :], start=False, stop=True)
                    S.copy(nm[0:64, :], acc[0:64, :])
                    S.tt(oT[0:64, pair, :], nm[0:64, :], bc[0:64, :], ALU.mult)
                else:
                    S.mm(bc.all(), k.ones_b[0:1, :], rh[0:1, 0, :], start=True, stop=False)
                    S.mm(bc.all(), k.ones_b[0:1, :], rh[0:1, 1, :], start=False, stop=True)
                    S.copy(nm[64:128, :], acc[64:128, :])
                    S.tt(oT[64:128, pair, :], nm[64:128, :], bc[64:128, :], ALU.mult)
        for r in range(4):
            ti = 2 + tqb * 4 + r
            xt = xrot.next()
            S.dma(xt.all(), xs(ti))
            xo = xorot.next()
            for hf in range(2):
                po = ppO.next()
                for kk in range(8):
                    S.mm(po.all(), oT[:, kk, r * 128:(r + 1) * 128], wo[:, kk, hf * 512:(hf + 1) * 512],
                         start=(kk == 0), stop=(kk == 7))
                S.tt(xo[:, hf * 512:(hf + 1) * 512], po.all(), g1[:, hf * 512:(hf + 1) * 512], ALU.mult)
            S.tt(xo.all(), xo.all(), xt.all(), ALU.add)
            S.dma(xs(ti), xo.all())


def final_norm(S, k, xs_fn):
    with ExitStack() as st:
        S.stack = st
        fw = S.sb("fnw", [128, 1024], F32)
        load_bc(S, fw.all(), k.final_norm_w, k.final_norm_w.t)
        xrot = S.rot("fx", [128, 1024], F32, 3)
        jrot = S.rot("fj", [128, 1024], F32, 2)
        orot = S.rot("fo", [128, 1024], F32, 3)
        for b in range(NB):
            for j in range(NT - 2):
                xt = xrot.next()
                S.dma(xt.all(), xs_fn(b)(2 + j))
                rs = k.small.next()
                rms_rstd(S, k, rs[:, 2:3], xt.all(), 1024, jrot.next().all())
                xo = orot.next()
                S.stt(xo.all(), xt.all(), rs[:, 2:3], fw.all(), ALU.mult, ALU.mult)
                S.dma(View(k.out.t[b, j * 128:(j + 1) * 128, :], k.out.res), xo.all())
        S.phase_end()
    S.stack = None

WEIGHT_SPECS = [
    ("ada_w", [2, 1024, 6144]), ("ada_b", [2, 6144]), ("norm1_w", [2, 1024]), ("norm2_w", [2, 1024]),
    ("ssd_in_w", [1, 1024, 6208]), ("ssd_conv_w", [1, 5, 4096]), ("ssd_conv_b", [1, 4096]),
    ("ssd_dt_bias", [1, 2, 32]), ("ssd_A_log", [1, 2, 32]), ("ssd_D", [1, 32]), ("ssd_norm_w", [1, 2048]),
    ("ssd_out_w", [1, 2048, 1024]), ("attn_qkv_w", [1, 1024, 1536]), ("attn_q_norm_w", [1, 64]),
    ("attn_k_norm_w", [1, 64]), ("attn_out_w", [1, 1024, 1024]), ("moe_router_w", [2, 1024, 16]),
    ("moe_w1", [2, 16, 1024, 1024]), ("moe_w3", [2, 16, 1024, 1024]), ("moe_w2", [2, 16, 1024, 1024]),
    ("final_norm_w", [1024]),
]


def build_program(stop=None, dbg=False, only=None):
    nc = bass.Bass("TRN2", target_bir_lowering=False)
    k = K()
    with ExitStack() as st:
        S = Sched(nc, st)
        S.init_psum(8)

        def din(name, shape):
            t = nc.dram_tensor(name, list(shape), F32, kind="ExternalInput")
            return Tl(t.ap(), 1, name)

        k.x = din("x", [NB, LAT, D])
        k.c = din("c", [NB, D])
        k.ctx = din("ctx", [NB, CTXL, D])
        k.c_ctx = din("c_ctx", [D])
        for name, shape in WEIGHT_SPECS:
            setattr(k, name, din(name, shape))
        k.cos = din("rope_cos", [LAT, 32])
        k.sin = din("rope_sin", [LAT, 32])
        k.out = Tl(nc.dram_tensor("out", [NB, LAT, D], F32, kind="ExternalOutput").ap(), 1, "out")
        xkind = "ExternalOutput"
        k.xs = [Tl(nc.dram_tensor(f"xs{b}", [NT, 128, D], F32, kind=xkind).ap(), NT, f"xs{b}") for b in range(NB)]
        k.modv = [S.dram(f"modv{l}", [3, 6144], F32) for l in range(2)]
        k.xtm_d = S.dram("xtm_d", [NT, 128, 3072], BF16)
        k.bc_d = S.dram("bc_d", [16, 128, TOK], BF16)
        k.yf_d = S.dram("yf_d", [NT, 128, 2048], F32)
        k.gn_d = S.dram("gn_d", [NT, 128, 2048], BF16)
        k.y_d = S.dram("y_d", [16, 576, 1024], BF16)
        k.small = S.rot("small", [128, 4], F32, 12)
        setup_consts(S, k)
        S.phase_end()

        def in_src(b):
            def f(i):
                if i < 2:
                    return View(k.ctx.t[b, i * 128:(i + 1) * 128, :], k.ctx.res)
                return View(k.x.t[b, (i - 2) * 128:(i - 1) * 128, :], k.x.res)
            return f

        def xs_fn(b):
            def f(i):
                return View(k.xs[b].t[i], [k.xs[b].res[i]])
            return f

        stages = []
        stages.append(("mod0", lambda: phase_mod(S, k, 0)))
        for b in range(NB):
            stages.append((f"ssd{b}", lambda b=b: ssd_mixer(S, k, b, 0, in_src(b), xs_fn(b))))
        stages.append(("moe0", lambda: moe_layer(S, k, 0, xs_fn)))
        stages.append(("mod1", lambda: phase_mod(S, k, 1)))
        for b in range(NB):
            stages.append((f"attn{b}", lambda b=b: attn_mixer(S, k, b, 1, xs_fn(b))))
        stages.append(("moe1", lambda: moe_layer(S, k, 1, xs_fn)))
        stages.append(("final", lambda: final_norm(S, k, xs_fn)))
        if only is not None:
            with ExitStack() as st2:
                S.stack = st2
                rot = S.rot("initx", [128, D], F32, 3)
                for b in range(NB):
                    xi = din(f"xs_init{b}", [NT, 128, D])
                    for i in range(NT):
                        t = rot.next()
                        S.dma(t.all(), View(xi.t[i], xi.res))
                        S.dma(xs_fn(b)(i), t.all())
                S.phase_end()
        for name, fn in stages:
            if only is not None and name not in only:
                continue
            S.stack = st
            fn()
            if stop == name:
                break
        S.stack = st
        outs = [k.out.all()] + [x.all() for x in k.xs]
        S.finish(outs)
        k.ninst = S.ninst
    return nc, k


_ROPE = None


def rope_tables():
    global _ROPE
    if _ROPE is None:
        S_ = LAT
        row = np.repeat(np.arange(S_ // 64), 64).astype(np.float32)
        col = np.tile(np.arange(64), S_ // 64).astype(np.float32)
        inv = (np.float32(10000.0) ** (-np.arange(0, 32, 2, dtype=np.float32) / np.float32(32))).astype(np.float32)
        ang = np.concatenate([row[:, None] * inv, col[:, None] * inv], axis=-1).astype(np.float32)
        _ROPE = (np.cos(ang).astype(np.float32), np.sin(ang).astype(np.float32))
    return _ROPE


_PROG = {}


def kernel(**inputs):
    n_cores = 8
    if "full" not in _PROG:
        _PROG["full"] = build_program()[0]
    nc = _PROG["full"]
    cos, sin = rope_tables()
    shared = {name: np.ascontiguousarray(inputs[name], dtype=np.float32) for name, _ in WEIGHT_SPECS}
    shared["c_ctx"] = np.ascontiguousarray(inputs["c_ctx"], dtype=np.float32)
    shared["rope_cos"] = cos
    shared["rope_sin"] = sin
    in_maps = []
    for cid in range(n_cores):
        m = dict(shared)
        sl = slice(cid * NB, (cid + 1) * NB)
        m["x"] = np.ascontiguousarray(inputs["x"][sl], dtype=np.float32)
        m["c"] = np.ascontiguousarray(inputs["c"][sl], dtype=np.float32)
        m["ctx"] = np.ascontiguousarray(inputs["ctx"][sl], dtype=np.float32)
        in_maps.append(m)
    res = run_bass_kernel_spmd(nc, in_maps, core_ids=list(range(n_cores)))
    return np.concatenate([np.asarray(r["out"]) for r in res.results], axis=0).astype(np.float32)
```

```python
from contextlib import ExitStack
import math
import numpy as np
import concourse.bass as bass
import concourse.mybir as mybir
from concourse.bass_utils import run_bass_kernel_spmd

F32 = mybir.dt.float32
BF16 = mybir.dt.bfloat16
AF = mybir.ActivationFunctionType
ALU = mybir.AluOpType
AX = mybir.AxisListType

ENGS = ["pe", "dve", "act", "pool", "sp"]
NLANES = 28
NLANES_HW = 16


class Res:
    __slots__ = ("w", "r", "name")

    def __init__(self, name=""):
        self.w = None
        self.r = []
        self.name = name


class View:
    __slots__ = ("ap", "res")

    def __init__(self, ap, res):
        self.ap = ap
        self.res = res

    def __getitem__(self, idx):
        return View(self.ap[idx], self.res)

    def rearrange(self, *a, **k):
        return View(self.ap.rearrange(*a, **k), self.res)

    def bitcast(self, dt):
        return View(self.ap.bitcast(dt), self.res)

    def bc(self, axis, n):
        a = self.ap.unsqueeze(axis)
        shp = list(a.shape)
        shp[axis] = n
        return View(a.to_broadcast(shp), self.res)


class Tl:
    def __init__(self, t, nslots=1, name=""):
        self.t = t
        self.nslots = nslots
        self.res = [Res(f"{name}[{i}]") for i in range(nslots)]
        self.name = name

    def __getitem__(self, idx):
        return View(self.t[idx], self.res)

    def all(self):
        return View(self.t[:], self.res)

    def s(self, i, j=None):
        if j is None:
            return View(self.t[:, i], [self.res[i]])
        return View(self.t[:, i:j], self.res[i:j])


class Rot:
    def __init__(self, tiles):
        self.tiles = tiles
        self.i = 0

    def next(self):
        t = self.tiles[self.i % len(self.tiles)]
        self.i += 1
        return t


class Sched:
    def __init__(self, nc, stack):
        self.nc = nc
        self.stack = stack
        self.q = {e: [] for e in ENGS}
        self.sem = {e: stack.enter_context(nc.semaphore(f"s_{e}")) for e in ENGS}
        self.cnt = {e: 0 for e in ENGS}
        self.lane_sem = [stack.enter_context(nc.semaphore(f"l_{i}")) for i in range(NLANES)]
        self.lane_cnt = [0] * NLANES
        self.lane_next = 0
        self.lane_next_sw = 0
        self.seen = {e: {} for e in ENGS}
        self.ninst = 0
        self.psum_banks = []
        self.uid = 0

    def sb(self, name, shape, dtype, nslots=1):
        self.uid += 1
        nm = f"{name}_{self.uid}"
        t = self.stack.enter_context(self.nc.sbuf_tensor(nm, list(shape), dtype))
        return Tl(t, nslots, nm)

    def rot(self, name, shape, dtype, n):
        return Rot([self.sb(f"{name}{i}", shape, dtype) for i in range(n)])

    def dram(self, name, shape, dtype, kind="Internal", nslots=1):
        t = self.nc.dram_tensor(name, list(shape), dtype, kind=kind)
        return Tl(t.ap(), nslots, name)

    def init_psum(self, n=8):
        for i in range(n):
            t = self.stack.enter_context(self.nc.psum_tensor(f"ps{i}", [128, 512], F32))
            self.psum_banks.append(Tl(t, 1, f"ps{i}"))

    def pool(self, idx):
        return Rot([self.psum_banks[i] for i in idx])

    def _collect(self, eng, reads, writes):
        waits = {}

        def need(tok):
            if tok is None:
                return
            kind, key, val = tok
            if kind == "e" and key == eng and eng == "pe":
                return
            k = (kind, key)
            if waits.get(k, 0) < val:
                waits[k] = val

        for v in reads:
            for r in v.res:
                need(r.w)
        for v in writes:
            for r in v.res:
                need(r.w)
                for tok in r.r:
                    need(tok)
        out = []
        seen = self.seen[eng]
        for k, val in waits.items():
            if seen.get(k, 0) >= val:
                continue
            seen[k] = val
            out.append((k, val))
        return out

    def _mark(self, tok, reads, writes):
        for v in writes:
            for r in v.res:
                r.w = tok
                r.r = []
        for v in reads:
            for r in v.res:
                r.r = [t for t in r.r if (t[0], t[1]) != (tok[0], tok[1])]
                r.r.append(tok)

    def _semof(self, k):
        kind, key = k
        return self.sem[key] if kind == "e" else self.lane_sem[key]

    def emit(self, eng, fn, reads=(), writes=()):
        reads = list(reads)
        writes = list(writes)
        waits = self._collect(eng, reads, writes)
        self.cnt[eng] += 1
        val = self.cnt[eng]
        sem = self.sem[eng]
        wl = [(self._semof(k), v) for k, v in waits]

        def run(e, wl=wl, fn=fn, sem=sem):
            for s, v in wl:
                e.wait_ge(s, v)
            fn(e).then_inc(sem, 1)

        self.q[eng].append(run)
        self._mark(("e", eng, val), reads, writes)
        self.ninst += 1

    def dma(self, out, in_, eng="sp", **kw):
        if eng == "pool":
            lane = NLANES_HW + self.lane_next_sw % (NLANES - NLANES_HW)
            self.lane_next_sw += 1
        else:
            lane = self.lane_next % NLANES_HW
            self.lane_next += 1
        reads = [in_]
        writes = [out]
        waits = self._collect(eng, reads, writes)
        prev = self.lane_cnt[lane]
        k = ("d", lane)
        if prev > 0 and self.seen[eng].get(k, 0) < prev:
            self.seen[eng][k] = prev
            waits = [w for w in waits if w[0] != k] + [(k, prev)]
        self.lane_cnt[lane] += 16
        val = self.lane_cnt[lane]
        sem = self.lane_sem[lane]
        wl = [(self._semof(k2), v) for k2, v in waits]
        oap, iap = out.ap, in_.ap

        def run(e, wl=wl, sem=sem, oap=oap, iap=iap, kw=kw):
            for s, v in wl:
                e.wait_ge(s, v)
            e.dma_start(out=oap, in_=iap, **kw).then_inc(sem, 16)

        self.q[eng].append(run)
        self._mark(("d", lane, val), reads, writes)
        self.ninst += 1

    def wait_all(self, eng, views):
        waits = self._collect(eng, list(views), [])
        wl = [(self._semof(k), v) for k, v in waits]

        def run(e, wl=wl):
            for s, v in wl:
                e.wait_ge(s, v)

        self.q[eng].append(run)

    def barrier(self):
        snap_e = dict(self.cnt)
        snap_l = list(self.lane_cnt)
        for eng in ENGS:
            wl = []
            seen = self.seen[eng]
            for e2, v in snap_e.items():
                if v > 0 and seen.get(("e", e2), 0) < v and not (e2 == eng and eng == "pe"):
                    seen[("e", e2)] = v
                    wl.append((self.sem[e2], v))
            for i, v in enumerate(snap_l):
                if v > 0 and seen.get(("d", i), 0) < v:
                    seen[("d", i)] = v
                    wl.append((self.lane_sem[i], v))

            def run(e, wl=wl):
                for s, v in wl:
                    e.wait_ge(s, v)

            self.q[eng].append(run)

    def flush(self):
        nc = self.nc
        q = self.q
        self.q = {e: [] for e in ENGS}
        with nc.Block() as block:
            @block.sync
            def _(e):
                for f in q["sp"]:
                    f(e)

            @block.tensor
            def _(e):
                for f in q["pe"]:
                    f(e)

            @block.vector
            def _(e):
                for f in q["dve"]:
                    f(e)

            @block.scalar
            def _(e):
                for f in q["act"]:
                    f(e)

            @block.gpsimd
            def _(e):
                for f in q["pool"]:
                    f(e)

    def phase_end(self):
        self.barrier()
        self.flush()

    def finish(self, final_views):
        self.wait_all("sp", final_views)
        self.flush()

    def mm(self, out, lhsT, rhs, start=True, stop=True):
        self.emit("pe", lambda e: e.matmul(out.ap, lhsT.ap, rhs.ap, start=start, stop=stop),
                  reads=[lhsT, rhs], writes=[out])

    def tr(self, out, in_, ident):
        self.emit("pe", lambda e: e.transpose(out.ap, in_.ap, ident.ap), reads=[in_, ident], writes=[out])

    def act(self, out, in_, func, bias=None, scale=None, accum_out=None):
        reads = [in_]
        writes = [out]
        kw = {}
        if bias is not None:
            if isinstance(bias, View):
                reads.append(bias)
                kw["bias"] = bias.ap
            else:
                kw["bias"] = bias
        if scale is not None:
            if isinstance(scale, View):
                reads.append(scale)
                kw["scale"] = scale.ap
            else:
                kw["scale"] = scale
        if accum_out is not None:
            writes.append(accum_out)
            kw["accum_out"] = accum_out.ap
        self.emit("act", lambda e: e.activation(out.ap, in_.ap, func, **kw), reads=reads, writes=writes)

    def tt(self, out, a, b, op, eng="dve"):
        self.emit(eng, lambda e: e.tensor_tensor(out=out.ap, in0=a.ap, in1=b.ap, op=op), reads=[a, b], writes=[out])

    def ts(self, out, a, s1, op0, s2=None, op1=None, accum_out=None, eng="dve"):
        reads = [a]
        writes = [out]
        s1v = s1.ap if isinstance(s1, View) else s1
        s2v = s2.ap if isinstance(s2, View) else s2
        if isinstance(s1, View):
            reads.append(s1)
        if isinstance(s2, View):
            reads.append(s2)
        kw = {}
        if op1 is not None:
            kw["op1"] = op1
        if accum_out is not None:
            kw["accum_out"] = accum_out.ap
            writes.append(accum_out)
        self.emit(eng, lambda e: e.tensor_scalar(out=out.ap, in0=a.ap, scalar1=s1v, scalar2=s2v, op0=op0, **kw),
                  reads=reads, writes=writes)

    def stt(self, out, a, s, b, op0, op1, eng="dve"):
        reads = [a, b]
        sv = s.ap if isinstance(s, View) else s
        if isinstance(s, View):
            reads.append(s)
        self.emit(eng, lambda e: e.scalar_tensor_tensor(out=out.ap, in0=a.ap, scalar=sv, in1=b.ap, op0=op0, op1=op1),
                  reads=reads, writes=[out])

    def copy(self, out, in_, eng="dve"):
        if eng == "act":
            self.emit("act", lambda e: e.activation(out.ap, in_.ap, AF.Copy), reads=[in_], writes=[out])
        else:
            self.emit(eng, lambda e: e.tensor_copy(out=out.ap, in_=in_.ap), reads=[in_], writes=[out])

    def memset(self, out, val, eng="dve"):
        self.emit(eng, lambda e: e.memset(out.ap, val), reads=[], writes=[out])

    def reduce(self, out, in_, op, axis=None):
        axis = axis or AX.X
        self.emit("dve", lambda e: e.tensor_reduce(out=out.ap, in_=in_.ap, axis=axis, op=op), reads=[in_], writes=[out])

    def recip(self, out, in_):
        self.emit("dve", lambda e: e.reciprocal(out=out.ap, in_=in_.ap), reads=[in_], writes=[out])


def pbf(bank):
    return View(bank.t[:].bitcast(BF16), bank.res)

D = 1024
LAT = 2048
CTXL = 256
NT = 18
TOK = NT * 128
DI = 2048
NH = 32
NG = 8
EPS = 1e-6
NB = 2


class K:
    pass


def setup_consts(S, k):
    nc = S.nc
    k.iota = S.sb("iota", [128, 256], F32)
    k.pid = S.sb("pid", [128, 1], F32)
    k.pid128 = S.sb("pid128", [128, 1], F32)
    k.eps = S.sb("epsc", [128, 1], F32)
    S.emit("pool", lambda e: e.iota(k.iota.t[:], [[1, 256]], base=0, channel_multiplier=0,
                                    allow_small_or_imprecise_dtypes=True), writes=[k.iota.all()])
    S.emit("pool", lambda e: e.iota(k.pid.t[:], [[0, 1]], base=0, channel_multiplier=1,
                                    allow_small_or_imprecise_dtypes=True), writes=[k.pid.all()])
    S.ts(k.pid128.all(), k.pid.all(), 128.0, ALU.add)
    S.memset(k.eps.all(), EPS)
    io = k.iota[:, 0:128]

    def mk(name, dt, op):
        t = S.sb(name, [128, 128], dt)
        S.ts(t.all(), io, k.pid.all(), op)
        return t

    k.identb = mk("identb", BF16, ALU.is_equal)
    k.identf = mk("identf", F32, ALU.is_equal)
    k.U_le = mk("U_le", BF16, ALU.is_ge)
    k.L_ge = mk("L_ge", BF16, ALU.is_le)
    k.SLt = mk("SLt", BF16, ALU.is_lt)
    k.SUt = mk("SUt", BF16, ALU.is_gt)
    k.U_le_f = mk("U_le_f", F32, ALU.is_ge)
    k.L_ge_f = mk("L_ge_f", F32, ALU.is_le)
    k.ones_f = S.sb("ones_f", [128, 128], F32)
    S.memset(k.ones_f.all(), 1.0)
    k.ones_b = S.sb("ones_b", [128, 128], BF16)
    S.memset(k.ones_b.all(), 1.0)


def load_bc(S, dst, dram_tl, ap):
    S.dma(dst, View(ap.partition_broadcast(dst.ap.shape[0]), dram_tl.res))


def rms_rstd(S, k, rs, src, n, junk):
    ss = k.small.next()
    S.act(junk, src, AF.Square, accum_out=ss[:, 0:1])
    S.act(ss[:, 1:2], ss[:, 0:1], AF.Sqrt, bias=k.eps.all(), scale=1.0 / n)
    S.recip(rs, ss[:, 1:2])


def phase_mod(S, k, layer):
    with ExitStack() as st:
        S.stack = st
        cin = S.sb("cin", [128, 3, 8], F32)
        for b in range(NB):
            S.dma(cin[:, b, :], View(k.c.t[b].rearrange("(p kk) -> p kk", kk=8), k.c.res))
        S.dma(cin[:, 2, :], View(k.c_ctx.t.rearrange("(p kk) -> p kk", kk=8), k.c_ctx.res))
        sc = S.sb("silc", [128, 8, 3], BF16)
        S.act(sc.all().rearrange("p kk v -> p v kk"), cin.all(), AF.Silu)
        bias = S.sb("adab", [3, 6144], F32)
        load_bc(S, bias.all(), k.ada_b, k.ada_b.t[layer])
        nw = S.sb("nw12", [3, 2, 1024], F32)
        load_bc(S, nw[:, 0, :], k.norm1_w, k.norm1_w.t[layer])
        load_bc(S, nw[:, 1, :], k.norm2_w, k.norm2_w.t[layer])
        res = S.sb("modres", [3, 6144], F32)
        wrot = S.rot("adaw", [128, 8, 1024], BF16, 2)
        pp = S.pool([0, 1, 2, 3])
        for blk in range(6):
            w = wrot.next()
            S.dma(w.all(), View(k.ada_w.t[layer].rearrange("(p kk) n -> p kk n", kk=8)[:, :, blk * 1024:(blk + 1) * 1024],
                                k.ada_w.res), eng="pool")
            for half in range(2):
                ps = pp.next()
                for kk in range(8):
                    S.mm(ps[0:3, :], sc[:, kk, :], w[:, kk, half * 512:(half + 1) * 512], start=(kk == 0), stop=(kk == 7))
                c0 = blk * 1024 + half * 512
                S.tt(res[:, c0:c0 + 512], ps[0:3, :], bias[:, c0:c0 + 512], ALU.add)
        for j, blk in enumerate((1, 4)):
            S.stt(res[:, blk * 1024:(blk + 1) * 1024], res[:, blk * 1024:(blk + 1) * 1024], 1.0, nw[:, j, :], ALU.add, ALU.mult)
        S.dma(k.modv[layer].all(), res.all())
        S.phase_end()


def load_mod(S, k, layer, row, blk, dst):
    load_bc(S, dst, k.modv[layer], k.modv[layer].t[row, blk * 1024:(blk + 1) * 1024])


def norm_T(S, k, b, layer, which, src_fn, hT, tiles, pp):
    blkA, blkS = (1, 0) if which == 1 else (4, 3)
    mods = {}
    for kind in (0, 1):
        if not any((ti < 2) == (kind == 0) for ti, _ in tiles):
            continue
        A = S.sb("modA", [128, 1024], F32)
        sh = S.sb("modS", [128, 1024], F32)
        row = 2 if kind == 0 else b
        load_mod(S, k, layer, row, blkA, A.all())
        load_mod(S, k, layer, row, blkS, sh.all())
        mods[kind] = (A, sh)
    xrot = S.rot("nx", [128, 1024], F32, 3)
    trot = S.rot("ntmp", [128, 1024], F32, 2)
    hrot = S.rot("nhb", [128, 1024], BF16, 2)
    for ti, col in tiles:
        A, sh = mods[0 if ti < 2 else 1]
        xt = xrot.next()
        S.dma(xt.all(), src_fn(ti))
        tmp = trot.next()
        rs = k.small.next()
        rms_rstd(S, k, rs[:, 2:3], xt.all(), 1024, tmp.all())
        S.stt(tmp.all(), xt.all(), rs[:, 2:3], A.all(), ALU.mult, ALU.mult)
        hb = hrot.next()
        S.tt(hb.all(), tmp.all(), sh.all(), ALU.add)
        ps = pp.next()
        pv = pbf(ps)
        for kk in range(8):
            S.tr(pv[:, kk * 128:(kk + 1) * 128], hb[:, kk * 128:(kk + 1) * 128], k.identb.all())
        S.copy(hT[:, :, col:col + 128], pv.rearrange("p (kk t) -> p kk t", kk=8), eng="act")

def ssd_mixer(S, k, b, layer, src_fn, dst_fn):
    with ExitStack() as st_outer:
        S.stack = st_outer
        hT = S.sb("hT", [128, 8, TOK], BF16)
        with ExitStack() as st:
            S.stack = st
            norm_T(S, k, b, layer, 1, src_fn, hT, [(i, i * 128) for i in range(NT)], S.pool([0, 1, 2, 3]))
            S.phase_end()
        with ExitStack() as st:
            S.stack = st
            ssd_phase_a(S, k, hT)
            S.phase_end()
        for d in (0, 1):
            with ExitStack() as st:
                S.stack = st
                ssd_scan(S, k, b, layer, hT, d, src_fn, dst_fn)
                S.phase_end()
        S.stack = st_outer
    with ExitStack() as st:
        S.stack = st
        ssd_phase_d(S, k, b, layer, src_fn, dst_fn)
        S.phase_end()
    S.stack = None


def ssd_phase_a(S, k, hT):
    stg = S.sb("cstg", [6, 4096], F32)
    S.dma(stg[0:5, :], k.ssd_conv_w[0])
    S.dma(stg[5:6, :], View(k.ssd_conv_b.t[0:1, :], k.ssd_conv_b.res))
    cw = S.sb("cw", [128, 32, 8], F32)
    pp = S.pool([0, 1, 2, 3])
    for cc0 in range(0, 32, 8):
        ps = pp.next()
        for j in range(8):
            cc = cc0 + j
            S.tr(ps[:, j * 8:j * 8 + 6], stg[0:6, cc * 128:(cc + 1) * 128], k.identf[0:6, 0:6])
        S.copy(cw[:, cc0:cc0 + 8, :], ps[:, 0:64].rearrange("p (j e) -> p j e", e=8))
    wrot = S.rot("wA", [128, 8, 512], BF16, 2)
    urot = S.rot("uA", [128, TOK], F32, 2)
    arot = S.rot("accA", [128, TOK], F32, 2)
    crot = S.rot("cvA", [128, TOK], BF16, 2)
    trot = S.rot("tmA", [128, 8, 128], BF16, 3)
    pp2 = S.pool([4, 5, 6, 7])
    blocks = [(0, 256), (256, 512), (768, 512), (1280, 512), (1792, 512)]
    segs = [(0, 256), (256, TOK)]
    w = None
    for cc in range(32):
        if cc % 4 == 0:
            w = wrot.next()
            c0 = 2048 + cc * 128
            S.dma(w.all(), View(k.ssd_in_w.t[0].rearrange("(kk p) n -> p kk n", p=128)[:, :, c0:c0 + 512], k.ssd_in_w.res),
                  eng="pool")
        u = urot.next()
        for (t0, n) in blocks:
            ps = pp.next()
            for kk in range(8):
                S.mm(ps[:, 0:n], w[:, kk, (cc % 4) * 128:(cc % 4 + 1) * 128], hT[:, kk, t0:t0 + n],
                     start=(kk == 0), stop=(kk == 7))
            S.copy(u[:, t0:t0 + n], ps[:, 0:n], eng="act")
        acc = arot.next()
        for (a, e) in segs:
            S.ts(acc[:, a:e], u[:, a:e], cw[:, cc, 2:3], ALU.mult, s2=cw[:, cc, 5:6], op1=ALU.add)
            for tap in (0, 1, 3, 4):
                dl = tap - 2
                lo = a + max(0, -dl)
                hi = e - max(0, dl)
                S.stt(acc[:, lo:hi], u[:, lo + dl:hi + dl], cw[:, cc, tap:tap + 1], acc[:, lo:hi], ALU.mult, ALU.add)
        cv = crot.next()
        S.act(cv.all(), acc.all(), AF.Silu)
        if cc >= 16:
            S.dma(View(k.bc_d.t[cc - 16], k.bc_d.res), cv.all())
        if cc < 24:
            for i0 in range(0, NT, 8):
                n = min(8, NT - i0)
                ps = pp2.next()
                pv = pbf(ps)
                for j in range(n):
                    S.tr(pv[:, j * 128:(j + 1) * 128], cv[:, (i0 + j) * 128:(i0 + j + 1) * 128], k.identb.all())
                tm = trot.next()
                S.copy(tm[:, 0:n, :], pv[:, 0:n * 128].rearrange("p (j c) -> p j c", j=n))
                S.dma(View(k.xtm_d.t[i0:i0 + n, :, cc * 128:(cc + 1) * 128].rearrange("i t c -> t i c"), k.xtm_d.res),
                      tm[:, 0:n, :])


def ssd_scan(S, k, b, layer, hT, d, src_fn, dst_fn):
    fwd = (d == 0)
    dc = d * 32
    wdt = S.sb("wdt", [128, 8, 64], BF16)
    S.dma(wdt.all(), View(k.ssd_in_w.t[0].rearrange("(kk p) n -> p kk n", p=128)[:, :, 6144:6208], k.ssd_in_w.res), eng="pool")
    dtb = S.sb("dtb", [128, 64], F32)
    load_bc(S, dtb.all(), k.ssd_dt_bias, k.ssd_dt_bias.t[0].rearrange("a h -> (a h)"))
    Abc = S.sb("Abc", [128, 64], F32)
    load_bc(S, Abc.all(), k.ssd_A_log, k.ssd_A_log.t[0].rearrange("a h -> (a h)"))
    S.act(Abc.all(), Abc.all(), AF.Exp)
    S.ts(Abc.all(), Abc.all(), -1.0, ALU.mult)
    h = S.sb("hst", [128, NG, 256], F32, nslots=NG)
    hb = S.sb("hstb", [128, NG, 256], BF16, nslots=NG)
    S.memset(h.all(), 0.0)
    S.memset(hb.all(), 0.0)
    if not fwd:
        wz = S.sb("wz", [128, 8, 2048], BF16)
        for q in range(4):
            S.dma(wz[:, :, q * 512:(q + 1) * 512],
                  View(k.ssd_in_w.t[0].rearrange("(kk p) n -> p kk n", p=128)[:, :, q * 512:(q + 1) * 512], k.ssd_in_w.res), eng="pool")
        Dbc = S.sb("Dbc", [128, 32], F32)
        load_bc(S, Dbc.all(), k.ssd_D, k.ssd_D.t[0])
        nwbc = S.sb("snw", [128, 2048], F32)
        load_bc(S, nwbc.all(), k.ssd_norm_w, k.ssd_norm_w.t[0])
        yfrot = S.rot("yf", [128, 2048], F32, 1)
        szrot = S.rot("sz", [128, 2048], F32, 1)
        gnrot = S.rot("gn", [128, 2048], BF16, 1)
    xbrot = S.rot("xB", [128, 3072], BF16, 2)
    bcrot = S.rot("bct", [128, 16, 128], BF16, 2)
    ydrot = S.rot("ydir", [128, 2048], F32, 2)
    xdtrot = S.rot("xdt", [128, 2048], BF16, 2)
    xddrot = S.rot("xdd", [128, 2048], BF16, 2)
    smrot = S.rot("ssm", [128, 8, 32], F32, 2)
    lharot = S.rot("lha", [128, 32, 128], BF16, 2)
    cbrot = S.rot("cbm", [128, 128], BF16, 4)
    sgrot = S.rot("seg", [128, 512], BF16, 4)
    mtrot = S.rot("MT", [128, 4, 128], BF16, 4)
    t3rot = S.rot("t3", [128, 256], F32, 6)
    ppA = S.pool([0, 1, 2, 3] if fwd else [0, 1, 2])
    ppB = S.pool([3])
    ppY = S.pool([4, 5, 6, 7])
    U = k.U_le if fwd else k.L_ge
    Uf = k.U_le_f if fwd else k.L_ge_f
    LM = k.SLt if fwd else k.SUt
    order = list(range(NT)) if fwd else [1, 0] + list(range(NT - 1, 1, -1))

    def prologue(i):
        c = {}
        c["i"] = i
        xB = c["xB"] = xbrot.next()
        S.dma(xB.all(), View(k.xtm_d.t[i], k.xtm_d.res))
        bct = c["bct"] = bcrot.next()
        S.dma(bct.all(), View(k.bc_d.t[:, :, i * 128:(i + 1) * 128].rearrange("c n t -> n c t"), k.bc_d.res))
        sm = c["sm"] = smrot.next()
        ps = ppA.next()
        for kk in range(8):
            S.mm(ps[:, 0:64], hT[:, kk, i * 128:(i + 1) * 128], wdt[:, kk, :], start=(kk == 0), stop=(kk == 7))
        S.tt(sm[:, 0, :], ps[:, dc:dc + 32], dtb[:, dc:dc + 32], ALU.add)
        S.act(sm[:, 1, :], sm[:, 0, :], AF.Exp)
        S.act(sm[:, 2, :], sm[:, 1, :], AF.Ln, bias=1.0)
        S.tt(sm[:, 3, :], sm[:, 2, :], Abc[:, dc:dc + 32], ALU.mult)
        lha = c["lha"] = lharot.next()
        S.tt(lha.all(), LM.all().bc(1, 32), sm[:, 3, :].bc(2, 128), ALU.mult)
        ps2 = ppA.next()
        S.mm(ps2[:, 0:32], Uf.all(), sm[:, 3, :])
        S.mm(ps2[:, 32:64], k.ones_f.all(), sm[:, 3, :])
        S.copy(sm[:, 4:6, :], ps2[:, 0:64].rearrange("p (a h) -> p a h", a=2))
        S.act(sm[:, 6, :], sm[:, 4, :], AF.Exp)
        S.tt(sm[:, 0, :], sm[:, 5, :], sm[:, 4, :], ALU.subtract)
        S.act(sm[:, 7, :], sm[:, 0, :], AF.Exp)
        S.act(sm[:, 1, :], sm[:, 5, :], AF.Exp)
        S.tt(sm[:, 0, :], sm[:, 2, :], sm[:, 7, :], ALU.mult)
        xdt = c["xdt"] = xdtrot.next()
        xdd = c["xdd"] = xddrot.next()
        x3 = c["x3"] = xB[:, 0:2048].rearrange("p (h j) -> p h j", h=32)
        S.tt(xdt.all().rearrange("p (h j) -> p h j", h=32), x3, sm[:, 2, :].bc(2, 64), ALU.mult)
        S.tt(xdd.all().rearrange("p (h j) -> p h j", h=32), x3, sm[:, 0, :].bc(2, 64), ALU.mult)
        c["ydir"] = ydrot.next()
        return c

    def stA(c, g):
        bct = c["bct"]
        BT = bct[:, g, :]
        CT = bct[:, 8 + g, :]
        pc = ppA.next()
        S.mm(pc[:, 0:128], BT, CT)
        pD = ppA.next()
        for r in range(4):
            S.mm(pD[:, r * 128:(r + 1) * 128], c["lha"][:, 4 * g + r, :], U.all())
        c[("a", g)] = (pc, pD, CT)

    def stB(c, g):
        pc, pD, CT = c[("a", g)]
        cbm = cbrot.next()
        S.tt(cbm.all(), pc[:, 0:128], U.all(), ALU.mult)
        seg = sgrot.next()
        S.act(seg.all(), pD.all(), AF.Exp)
        c[("f", g)] = (cbm, seg, CT)

    def stC1(c, g):
        cbm, seg, CT = c[("f", g)]
        MT = mtrot.next()
        S.tt(MT.all(), seg.all().rearrange("p (r l) -> p r l", r=4), cbm.all().bc(1, 4), ALU.mult)
        c[("m", g)] = MT

    def stC2(c, g):
        cbm, seg, CT = c[("f", g)]
        MT = c[("m", g)]
        xB, xdt, xdd = c["xB"], c["xdt"], c["xdd"]
        py = ppY.next()
        for r in range(4):
            hh = 4 * g + r
            S.mm(py[:, r * 64:(r + 1) * 64], MT[:, r, :], xdt[:, hh * 64:(hh + 1) * 64])
        S.mm(py[:, 256:512], CT, hb.s(g))
        pst = ppY.next()
        S.mm(pst[:, 0:256], xB[:, 2048 + g * 128:2048 + (g + 1) * 128], xdd[:, g * 256:(g + 1) * 256])
        c[("p", g)] = (py, pst)

    def stE(c, g):
        py, pst = c[("p", g)]
        sm, ydir = c["sm"], c["ydir"]
        t3 = t3rot.next()
        S.tt(t3.all().rearrange("p (r j) -> p r j", r=4), py[:, 256:512].rearrange("p (r j) -> p r j", r=4),
             sm[:, 6, 4 * g:4 * g + 4].bc(2, 64), ALU.mult)
        S.tt(ydir[:, g * 256:(g + 1) * 256], t3.all(), py[:, 0:256], ALU.add)
        t4 = t3rot.next()
        S.tt(t4.all().rearrange("p (r j) -> p r j", r=4), h.s(g).rearrange("p (r j) -> p r j", r=4),
             sm[:, 1, 4 * g:4 * g + 4].bc(2, 64), ALU.mult)
        S.tt(h.s(g), t4.all(), pst[:, 0:256], ALU.add)
        S.copy(hb.s(g), h.s(g), eng="act")

    def epilogue(c):
        i = c["i"]
        ydir = c["ydir"]
        if fwd:
            S.dma(View(k.yf_d.t[i], k.yf_d.res), ydir.all())
            return
        yf = yfrot.next()
        S.dma(yf.all(), View(k.yf_d.t[i], k.yf_d.res))
        S.tt(ydir.all(), ydir.all(), yf.all(), ALU.add)
        S.tt(yf.all().rearrange("p (h j) -> p h j", h=32), c["x3"], Dbc.all().bc(2, 64), ALU.mult)
        S.tt(ydir.all(), ydir.all(), yf.all(), ALU.add)
        sz = szrot.next()
        for q in range(4):
            pz = ppB.next()
            for kk in range(8):
                S.mm(pz.all(), hT[:, kk, i * 128:(i + 1) * 128], wz[:, kk, q * 512:(q + 1) * 512],
                     start=(kk == 0), stop=(kk == 7))
            S.act(sz[:, q * 512:(q + 1) * 512], pz.all(), AF.Silu)
        S.tt(ydir.all(), ydir.all(), sz.all(), ALU.mult)
        rs = k.small.next()
        rms_rstd(S, k, rs[:, 2:3], ydir.all(), 2048, sz.all())
        gn = gnrot.next()
        S.stt(gn.all(), ydir.all(), rs[:, 2:3], nwbc.all(), ALU.mult, ALU.mult)
        S.dma(View(k.gn_d.t[i], k.gn_d.res), gn.all())

    ctxs = {0: prologue(order[0])}
    n = len(order) * NG
    for st_ in range(n + 2):
        if st_ < n:
            ci, g = divmod(st_, NG)
            if g == 3 and ci + 1 < len(order):
                ctxs[ci + 1] = prologue(order[ci + 1])
            stA(ctxs[ci], g)
        if 1 <= st_ <= n:
            ci, g = divmod(st_ - 1, NG)
            stC1(ctxs[ci], g)
        if st_ < n:
            ci, g = divmod(st_, NG)
            stB(ctxs[ci], g)
        if 1 <= st_ <= n:
            ci, g = divmod(st_ - 1, NG)
            stC2(ctxs[ci], g)
        if st_ >= 2:
            ci, g = divmod(st_ - 2, NG)
            stE(ctxs[ci], g)
            if g == NG - 1:
                epilogue(ctxs[ci])
                del ctxs[ci]


def ssd_phase_d(S, k, b, layer, src_fn, dst_fn):
    wo = S.sb("wo", [128, 16, 1024], BF16)
    for q in range(4):
        S.dma(wo[:, q * 4:(q + 1) * 4, :],
              View(k.ssd_out_w.t[0].rearrange("(kk p) n -> p kk n", p=128)[:, q * 4:(q + 1) * 4, :], k.ssd_out_w.res), eng="pool")
    g1 = {}
    for kind in (0, 1):
        g1[kind] = S.sb("g1", [128, 1024], F32)
        load_mod(S, k, layer, 2 if kind == 0 else b, 2, g1[kind].all())
    gnrot = S.rot("gnD", [128, 2048], BF16, 2)
    gTrot = S.rot("gT", [128, 16, 128], BF16, 2)
    xrrot = S.rot("xr", [128, 1024], F32, 2)
    xorot = S.rot("xo", [128, 1024], F32, 2)
    ppB = S.pool([0, 1, 2, 3])
    ppC = S.pool([4, 5, 6, 7])
    for i in range(NT):
        kind = 0 if i < 2 else 1
        gn = gnrot.next()
        S.dma(gn.all(), View(k.gn_d.t[i], k.gn_d.res))
        gT = gTrot.next()
        for hf in range(2):
            pt = ppB.next()
            pv = pbf(pt)
            for j in range(8):
                c = hf * 8 + j
                S.tr(pv[:, j * 128:(j + 1) * 128], gn[:, c * 128:(c + 1) * 128], k.identb.all())
            S.copy(gT[:, hf * 8:(hf + 1) * 8, :], pv.rearrange("p (j t) -> p j t", j=8), eng="act")
        xr = xrrot.next()
        S.dma(xr.all(), src_fn(i))
        xo = xorot.next()
        for hf in range(2):
            po = ppC.next()
            for c in range(16):
                S.mm(po.all(), gT[:, c, :], wo[:, c, hf * 512:(hf + 1) * 512], start=(c == 0), stop=(c == 15))
            S.tt(xo[:, hf * 512:(hf + 1) * 512], po.all(), g1[kind][:, hf * 512:(hf + 1) * 512], ALU.mult)
        S.tt(xo.all(), xo.all(), xr.all(), ALU.add)
        S.dma(dst_fn(i), xo.all())

NE = 16
U32 = mybir.dt.uint32


def moe_layer(S, k, layer, xs_fn):
    has_ctx = (layer == 0)
    ngrp = 2 if has_ctx else 1
    GNT = [16, 2]
    GCAP = [256, 32]
    GTI0 = [2, 0]
    NSLOT = 512 + (64 if has_ctx else 0)
    with ExitStack() as st_outer:
        S.stack = st_outer
        slT = [[S.sb(f"slT{g}{b}", [16, GNT[g] * 128], BF16) for b in range(NB)] for g in range(ngrp)]
        with ExitStack() as st_mid:
            S.stack = st_mid
            hn = [[S.sb(f"hn{g}{b}", [128, GNT[g], 1024], BF16) for b in range(NB)] for g in range(ngrp)]
            sl = [S.sb(f"sl{g}", [128, NB, GNT[g], NE], F32) for g in range(ngrp)]
            affhl = [S.sb(f"affhl{g}", [128, NB, GNT[g], NE, 2], BF16) for g in range(ngrp)]
            with ExitStack() as st:
                S.stack = st
                moe_route(S, k, layer, xs_fn, ngrp, GNT, GCAP, GTI0, hn, sl, affhl, slT)
                S.phase_end()
            with ExitStack() as st:
                S.stack = st
                moe_experts(S, k, layer, ngrp, GNT, hn, sl, affhl, NSLOT)
                S.phase_end()
        with ExitStack() as st:
            S.stack = st
            moe_scatter(S, k, layer, xs_fn, ngrp, GNT, GTI0, slT)
            S.phase_end()
    S.stack = None


def moe_route(S, k, layer, xs_fn, ngrp, GNT, GCAP, GTI0, hn, sl, affhl, slT):
    rw = S.sb("rw", [128, 8, NE], F32)
    S.dma(rw.all(), View(k.moe_router_w.t[layer].rearrange("(kk p) e -> p kk e", p=128), k.moe_router_w.res))
    aff = [S.sb(f"aff{g}", [128, NB, GNT[g], NE], F32) for g in range(ngrp)]
    xrot = S.rot("mx", [128, 1024], F32, 3)
    trot = S.rot("mtmp", [128, 1024], F32, 2)
    hfrot = S.rot("mhf", [128, 1024], F32, 2)
    hTrot = S.rot("mhfT", [128, 8, 128], F32, 2)
    erot = S.rot("mexp", [128, NE], F32, 3)
    ppA = S.pool([0, 1, 2, 3])
    ppB = S.pool([4, 5])
    ppC = S.pool([6, 7])
    for g in range(ngrp):
        for b in range(NB):
            A = S.sb("m2A", [128, 1024], F32)
            sh = S.sb("m2S", [128, 1024], F32)
            row = 2 if g == 1 else b
            load_mod(S, k, layer, row, 4, A.all())
            load_mod(S, k, layer, row, 3, sh.all())
            for j in range(GNT[g]):
                ti = GTI0[g] + j
                xt = xrot.next()
                S.dma(xt.all(), xs_fn(b)(ti))
                tmp = trot.next()
                rs = k.small.next()
                rms_rstd(S, k, rs[:, 2:3], xt.all(), 1024, tmp.all())
                S.stt(tmp.all(), xt.all(), rs[:, 2:3], A.all(), ALU.mult, ALU.mult)
                hf = hfrot.next()
                S.tt(hf.all(), tmp.all(), sh.all(), ALU.add)
                S.copy(hn[g][b][:, j, :], hf.all(), eng="act")
                hfT = hTrot.next()
                for q in range(2):
                    ps = ppA.next()
                    for c in range(4):
                        kk = q * 4 + c
                        S.tr(ps[:, c * 128:(c + 1) * 128], hf[:, kk * 128:(kk + 1) * 128], k.identf.all())
                    S.copy(hfT[:, q * 4:(q + 1) * 4, :], ps.all().rearrange("p (c t) -> p c t", c=4), eng="act")
                pl = ppB.next()
                for kk in range(8):
                    S.mm(pl[:, 0:NE], hfT[:, kk, :], rw[:, kk, :], start=(kk == 0), stop=(kk == 7))
                sm = k.small.next()
                S.reduce(sm[:, 0:1], pl[:, 0:NE], ALU.max)
                S.ts(sm[:, 1:2], sm[:, 0:1], -1.0, ALU.mult)
                ex = erot.next()
                S.act(ex.all(), pl[:, 0:NE], AF.Exp, bias=sm[:, 1:2], accum_out=sm[:, 2:3])
                S.recip(sm[:, 3:4], sm[:, 2:3])
                S.ts(aff[g][:, b, j, :], ex.all(), sm[:, 3:4], ALU.mult)
    lo = [S.sb(f"lo{g}", [128, NB, NE], F32) for g in range(ngrp)]
    hi = [S.sb(f"hi{g}", [128, NB, NE], F32) for g in range(ngrp)]
    mid = [S.sb(f"mid{g}", [128, NB, NE], F32) for g in range(ngrp)]
    cnt = [S.sb(f"cnt{g}", [128, NB, NE], F32) for g in range(ngrp)]
    selp = [S.sb(f"selp{g}", [128, NB, NE], F32) for g in range(ngrp)]
    seln = [S.sb(f"seln{g}", [128, NB, NE], F32) for g in range(ngrp)]
    ge = [S.sb(f"ge{g}", [128, NB, GNT[g], NE], BF16) for g in range(ngrp)]
    for g in range(ngrp):
        S.memset(lo[g].all(), 0.0)
        S.memset(hi[g].all(), 1.5)
    for it in range(31):
        for g in range(ngrp):
            nt = GNT[g]
            n = NB * nt * NE
            S.tt(mid[g].all(), lo[g].all(), hi[g].all(), ALU.add)
            S.ts(mid[g].all(), mid[g].all(), 0.5, ALU.mult)
            S.tt(ge[g].all(), aff[g].all(), mid[g].all().bc(2, nt), ALU.is_ge)
            pc = ppC.next()
            S.mm(pc[:, 0:n], k.ones_b.all(), ge[g].all().rearrange("p s t e -> p (s t e)"))
            S.reduce(cnt[g].all(), pc[:, 0:n].rearrange("p (s t e) -> p s e t", s=NB, t=nt), ALU.add)
            S.ts(selp[g].all(), cnt[g].all(), float(GCAP[g]), ALU.is_ge)
            S.ts(seln[g].all(), cnt[g].all(), float(GCAP[g]), ALU.is_lt)
            S.emit("dve", lambda e, g=g: e.copy_predicated(lo[g].t[:], selp[g].t[:].bitcast(U32), mid[g].t[:]),
                   reads=[selp[g].all(), mid[g].all()], writes=[lo[g].all()])
            S.emit("dve", lambda e, g=g: e.copy_predicated(hi[g].t[:], seln[g].t[:].bitcast(U32), mid[g].t[:]),
                   reads=[seln[g].all(), mid[g].all()], writes=[hi[g].all()])
    for g in range(ngrp):
        nt = GNT[g]
        n = NB * nt * NE
        cap = GCAP[g]
        S.tt(ge[g].all(), aff[g].all(), lo[g].all().bc(2, nt), ALU.is_ge)
        gef = ge[g].all().rearrange("p s t e -> p (s t e)")
        pr = ppC.next()
        S.mm(pr[:, 0:n], k.SUt.all(), gef)
        ptot = ppC.next()
        S.mm(ptot[:, 0:n], k.ones_b.all(), gef)
        tot = S.sb(f"tot{g}", [128, NB, nt, NE], F32)
        S.copy(tot.all(), ptot[:, 0:n].rearrange("p (s t e) -> p s t e", s=NB, t=nt))
        off = S.sb(f"off{g}", [128, NB, nt, NE], F32)
        S.memset(off[:, :, 0, :], 0.0)
        for j in range(1, nt):
            S.tt(off[:, :, j, :], off[:, :, j - 1, :], tot[:, :, j - 1, :], ALU.add)
        rank = S.sb(f"rank{g}", [128, NB, nt, NE], F32)
        S.tt(rank.all(), pr[:, 0:n].rearrange("p (s t e) -> p s t e", s=NB, t=nt), off.all(), ALU.add)
        ok = S.sb(f"ok{g}", [128, NB, nt, NE], F32)
        S.ts(ok.all(), rank.all(), float(cap), ALU.is_lt)
        S.tt(ok.all(), ok.all(), ge[g].all(), ALU.mult)
        for b in range(NB):
            offs = 1.0 + (32.0 * b if g == 1 else 0.0)
            S.stt(sl[g][:, b], rank[:, b], offs, ok[:, b], ALU.add, ALU.mult)
        S.ts(sl[g].all(), sl[g].all(), -1.0, ALU.add)
        for b in range(NB):
            for j0 in range(0, nt, 4):
                m = min(4, nt - j0)
                ps = ppA.next()
                for c in range(m):
                    S.tr(ps[0:NE, c * 128:(c + 1) * 128], sl[g][:, b, j0 + c, :], k.identf.all())
                S.copy(slT[g][b][:, j0 * 128:(j0 + m) * 128], ps[0:NE, 0:m * 128])
        S.copy(affhl[g][:, :, :, :, 0], aff[g].all())
        S.tt(affhl[g][:, :, :, :, 1], aff[g].all(), affhl[g][:, :, :, :, 0], ALU.subtract)


def moe_experts(S, k, layer, ngrp, GNT, hn, sl, affhl, NSLOT):
    has_ctx = ngrp == 2
    wrot = S.rot("mw", [128, 8, 1024], BF16, 4)
    xgT = S.sb("xgT", [128, 8, NSLOT], BF16)
    hT = S.sb("mhT", [128, 8, NSLOT], BF16)
    serot = S.rot("Se", [128, 16, 256], BF16, 2)
    secrot = S.rot("SeC", [128, NB, 2, 64], BF16, 2)
    sarot = S.rot("msa", [128, 512], F32, 2)
    yrot = S.rot("my", [128, 1024], BF16, 2)
    gates = S.rot("mgate", [128, 8], F32, 2)
    ppA = S.pool([0, 1, 2])
    ppB = S.pool([3, 4, 5])
    ppG = S.pool([6, 7])
    nchunks = [(0, 512)] + ([(512, 64)] if has_ctx else [])
    jchunks = [(0, 128), (128, 128), (256, 128), (384, 128)] + ([(512, 64)] if has_ctx else [])
    wsrc = {1: k.moe_w1, 3: k.moe_w3, 2: k.moe_w2}

    def loadw(which, e):
        w = wrot.next()
        src = wsrc[which]
        for q in range(2):
            S.dma(w[:, q * 4:(q + 1) * 4, :],
                  View(src.t[layer, e].rearrange("(kk p) n -> p kk n", p=128)[:, q * 4:(q + 1) * 4, :], src.res), eng="pool")
        return w

    for e in range(NE):
        w1 = loadw(1, e)
        w3 = loadw(3, e)
        gt = gates.next()
        pgt = ppG.next()
        for b in range(NB):
            Se = serot.next()
            for tt_ in range(16):
                S.ts(Se[:, tt_, :], k.iota[:, 0:256], sl[0][:, b, tt_, e:e + 1], ALU.is_equal)
            for kk0 in range(0, 8, 2):
                pg = ppA.next()
                for c in range(2):
                    kk = kk0 + c
                    for tt_ in range(16):
                        S.mm(pg[:, c * 256:(c + 1) * 256], hn[0][b][:, tt_, kk * 128:(kk + 1) * 128], Se[:, tt_, :],
                             start=(tt_ == 0), stop=(tt_ == 15))
                S.copy(xgT[:, kk0:kk0 + 2, b * 256:(b + 1) * 256], pg.all().rearrange("p (c j) -> p c j", c=2), eng="act")
            for jc in range(2):
                col = (b * 2 + jc) * 2
                for tt_ in range(16):
                    S.mm(pgt[:, col:col + 2], Se[:, tt_, jc * 128:(jc + 1) * 128], affhl[0][:, b, tt_, e, :],
                         start=(tt_ == 0), stop=(tt_ == 15))
        if has_ctx:
            SeC = secrot.next()
            for b in range(NB):
                for tt_ in range(2):
                    S.ts(SeC[:, b, tt_, :], k.iota[:, 0:64], sl[1][:, b, tt_, e:e + 1], ALU.is_equal)
            for kk0 in range(0, 8, 4):
                pg = ppA.next()
                for c in range(4):
                    kk = kk0 + c
                    n = 0
                    for b in range(NB):
                        for tt_ in range(2):
                            S.mm(pg[:, c * 64:(c + 1) * 64], hn[1][b][:, tt_, kk * 128:(kk + 1) * 128], SeC[:, b, tt_, :],
                                 start=(n == 0), stop=(n == 3))
                            n += 1
                S.copy(xgT[:, kk0:kk0 + 4, 512:576], pg[:, 0:256].rearrange("p (c j) -> p c j", c=4), eng="act")
            n = 0
            for b in range(NB):
                for tt_ in range(2):
                    S.mm(pgt[0:64, 8:10], SeC[:, b, tt_, :], affhl[1][:, b, tt_, e, :], start=(n == 0), stop=(n == 3))
                    n += 1
        ncol = 5 if has_ctx else 4
        S.reduce(gt[:, 0:ncol], pgt[:, 0:2 * ncol].rearrange("p (c two) -> p c two", two=2), ALU.add)
        for fc in range(8):
            for (n0, n) in nchunks:
                pa = ppA.next()
                for kk in range(8):
                    S.mm(pa[:, 0:n], w1[:, kk, fc * 128:(fc + 1) * 128], xgT[:, kk, n0:n0 + n], start=(kk == 0), stop=(kk == 7))
                pb = ppB.next()
                for kk in range(8):
                    S.mm(pb[:, 0:n], w3[:, kk, fc * 128:(fc + 1) * 128], xgT[:, kk, n0:n0 + n], start=(kk == 0), stop=(kk == 7))
                sa = sarot.next()
                S.act(sa[:, 0:n], pa[:, 0:n], AF.Silu)
                S.tt(hT[:, fc, n0:n0 + n], sa[:, 0:n], pb[:, 0:n], ALU.mult)
        w2 = loadw(2, e)
        for ji, (j0, m) in enumerate(jchunks):
            y = yrot.next()
            for hf in range(2):
                py = ppB.next()
                for fc in range(8):
                    S.mm(py[0:m, :], hT[:, fc, j0:j0 + m], w2[:, fc, hf * 512:(hf + 1) * 512], start=(fc == 0), stop=(fc == 7))
                S.act(y[0:m, hf * 512:(hf + 1) * 512], py[0:m, :], AF.Copy, scale=gt[0:m, ji:ji + 1])
            S.dma(View(k.y_d.t[e, j0:j0 + m, :], k.y_d.res), y[0:m, :])


def moe_scatter(S, k, layer, xs_fn, ngrp, GNT, GTI0, slT):
    sel16 = S.sb("sel16", [16, NE, 128], BF16)
    selio = S.sb("selio", [16, NE, 128], F32)
    S.emit("pool", lambda e: e.iota(selio.t[:], [[1, NE], [0, 128]], base=0, channel_multiplier=0,
                                    allow_small_or_imprecise_dtypes=True), writes=[selio.all()])
    S.ts(sel16.all(), selio.all(), k.pid[0:16, :], ALU.is_equal)
    pid32 = S.sb("pid32", [128, 1], F32)
    S.ts(pid32.all(), k.pid.all(), 32.0, ALU.add)
    yall = S.sb("yall", [128, 2 * NE, 1024], BF16)
    strot = S.rot("ST", [128, 2, NE, 128], BF16, 2)
    xrot = S.rot("sx", [128, 1024], F32, 2)
    xorot = S.rot("sxo", [128, 1024], F32, 2)
    ppA = S.pool([0, 1, 2, 3])
    ppB = S.pool([4, 5, 6, 7])
    for g in range(ngrp):
        for b in range(NB):
            g2 = S.sb("g2v", [128, 1024], F32)
            load_mod(S, k, layer, 2 if g == 1 else b, 5, g2.all())
            if g == 0:
                kr = 128
                njc = 2
                for e in range(NE):
                    S.dma(yall[:, 2 * e:2 * e + 2, :],
                          View(k.y_d.t[e, b * 256:(b + 1) * 256, :].rearrange("(jc j) d -> j jc d", jc=2), k.y_d.res))
                cmpv = [k.pid, k.pid128]
            else:
                kr = 32
                njc = 1
                S.dma(yall[0:32, 0:NE, :],
                      View(k.y_d.t[:, 512 + b * 32:512 + (b + 1) * 32, :].rearrange("e j d -> j e d"), k.y_d.res))
                cmpv = [k.pid if b == 0 else pid32]
            for j in range(GNT[g]):
                ti = GTI0[g] + j
                banks = [ppA.next() for _ in range(4)]
                for e in range(NE):
                    S.mm(banks[e // 4][:, (e % 4) * 128:(e % 4 + 1) * 128], sel16[:, e, :], slT[g][b][:, j * 128:(j + 1) * 128])
                ST = strot.next()
                for jc in range(njc):
                    for q in range(4):
                        S.ts(ST[:, jc, q * 4:(q + 1) * 4, :], banks[q].all().rearrange("p (e t) -> p e t", e=4),
                             cmpv[jc].all(), ALU.is_equal)
                xt = xrot.next()
                S.dma(xt.all(), xs_fn(b)(ti))
                xo = xorot.next()
                for hf in range(2):
                    po = ppB.next()
                    n = 0
                    tot = NE * njc
                    for e in range(NE):
                        for jc in range(njc):
                            yi = 2 * e + jc if g == 0 else e
                            S.mm(po.all(), ST[0:kr, jc, e, :], yall[0:kr, yi, hf * 512:(hf + 1) * 512],
                                 start=(n == 0), stop=(n == tot - 1))
                            n += 1
                    S.tt(xo[:, hf * 512:(hf + 1) * 512], po.all(), g2[:, hf * 512:(hf + 1) * 512], ALU.mult)
                S.tt(xo.all(), xo.all(), xt.all(), ALU.add)
                S.dma(xs_fn(b)(ti), xo.all())

def attn_mixer(S, k, b, layer, xs):
    NLT = NT - 2
    with ExitStack() as st_outer:
        S.stack = st_outer
        qT = S.sb("qT", [128, 8, LAT], BF16)
        kTd = S.sb("kTd", [128, 4, TOK], BF16)
        vaug = S.sb("vaug", [128, NT, 4, 72], BF16)
        with ExitStack() as st_h:
            S.stack = st_h
            hT = S.sb("ahT", [128, 8, TOK], BF16)
            with ExitStack() as st:
                S.stack = st
                norm_T(S, k, b, layer, 1, xs, hT, [(i, i * 128) for i in range(NT)], S.pool([0, 1, 2, 3]))
                S.phase_end()
            import os
            if os.environ.get("ATT_STOP") != "norm":
                with ExitStack() as st:
                    S.stack = st
                    attn_qkv(S, k, hT, qT, kTd, vaug)
                    S.phase_end()
        import os
        if os.environ.get("ATT_STOP") not in ("qkv", "norm"):
            with ExitStack() as st:
                S.stack = st
                attn_core(S, k, b, layer, xs, qT, kTd, vaug)
                S.phase_end()
    S.stack = None


def attn_qkv(S, k, hT, qT, kTd, vaug):
    w = S.sb("wqkv", [128, 8, 1536], BF16)
    for q in range(3):
        S.dma(w[:, :, q * 512:(q + 1) * 512],
              View(k.attn_qkv_w.t[0].rearrange("(kk p) n -> p kk n", p=128)[:, :, q * 512:(q + 1) * 512], k.attn_qkv_w.res), eng="pool")
    qw = S.sb("qwbc", [128, 64], F32)
    kw = S.sb("kwbc", [128, 64], F32)
    load_bc(S, qw.all(), k.attn_q_norm_w, k.attn_q_norm_w.t[0])
    load_bc(S, kw.all(), k.attn_k_norm_w, k.attn_k_norm_w.t[0])
    S.memset(vaug.all(), 1.0)
    qkrot = S.rot("qk", [128, 20, 64], F32, 2)
    sqrot = S.rot("qsq", [128, 20, 64], F32, 1)
    qnrot = S.rot("qn", [128, 20, 64], F32, 2)
    qrrot = S.rot("qr", [128, 20, 64], BF16, 2)
    kdrot = S.rot("kd", [128, 4, 2, 64], BF16, 2)
    csrot = S.rot("cs", [128, 2, 32], F32, 2)
    r1rot = S.rot("rp1", [128, 20, 32], F32, 2)
    r2rot = S.rot("rp2", [128, 20, 32], F32, 2)
    ssrot = S.rot("qss", [128, 20], F32, 3)
    ppA = S.pool([0, 1, 2, 3, 4, 5])
    ppB = S.pool([6, 7])
    import os
    LVL = int(os.environ.get("QKV_LVL", "9"))
    if LVL < 1:
        return
    for i in range(NT):
        banks = []
        for nb_ in range(3):
            ps = ppA.next()
            for kk in range(8):
                S.mm(ps.all(), hT[:, kk, i * 128:(i + 1) * 128], w[:, kk, nb_ * 512:(nb_ + 1) * 512], start=(kk == 0), stop=(kk == 7))
            banks.append(ps)
        VAR = os.environ.get("QKV_VAR", "Z")
        if VAR == "A":
            continue
        qk = qkrot.next()
        S.copy(qk[:, 0:8, :], banks[0].all().rearrange("p (h d) -> p h d", h=8), eng="act")
        S.copy(qk[:, 8:16, :], banks[1].all().rearrange("p (h d) -> p h d", h=8), eng="act")
        S.copy(qk[:, 16:20, :], banks[2][:, 0:256].rearrange("p (h d) -> p h d", h=4), eng="act")
        if VAR == "B":
            continue
        S.copy(vaug[:, i, :, 0:64], banks[2][:, 256:512].rearrange("p (h d) -> p h d", h=4), eng="act")
        if LVL < 2:
            continue
        sq = sqrot.next()
        S.tt(sq.all(), qk.all(), qk.all(), ALU.mult)
        ss = ssrot.next()
        S.reduce(ss.all(), sq.all(), ALU.add)
        S.act(ss.all(), ss.all(), AF.Ln, bias=k.eps.all(), scale=1.0 / 64)
        S.act(ss.all(), ss.all(), AF.Exp, scale=-0.5)
        qn = qnrot.next()
        S.tt(qn.all(), qk.all(), ss.all().bc(2, 64), ALU.mult)
        S.tt(qn[:, 0:16, :], qn[:, 0:16, :], qw.all().bc(1, 16), ALU.mult)
        S.tt(qn[:, 16:20, :], qn[:, 16:20, :], kw.all().bc(1, 4), ALU.mult)
        if LVL < 3:
            continue
        qr = qrrot.next()
        if i >= 2:
            cs = csrot.next()
            t0 = (i - 2) * 128
            S.dma(cs[:, 0, :], View(k.cos.t[t0:t0 + 128, :], k.cos.res))
            S.dma(cs[:, 1, :], View(k.sin.t[t0:t0 + 128, :], k.sin.res))
            cosb = cs[:, 0, :].bc(1, 20)
            sinb = cs[:, 1, :].bc(1, 20)
            t1 = qn[:, :, 0:32]
            t2 = qn[:, :, 32:64]
            a1 = r1rot.next()
            a2 = r2rot.next()
            S.tt(a1.all(), t1, cosb, ALU.mult)
            S.tt(a2.all(), t2, sinb, ALU.mult)
            S.tt(qr[:, :, 0:32], a1.all(), a2.all(), ALU.subtract)
            a3 = r1rot.next()
            a4 = r2rot.next()
            S.tt(a3.all(), t2, cosb, ALU.mult)
            S.tt(a4.all(), t1, sinb, ALU.mult)
            S.tt(qr[:, :, 32:64], a3.all(), a4.all(), ALU.add)
        else:
            S.copy(qr.all(), qn.all())
        if LVL < 4:
            continue
        if i >= 2:
            ps = ppB.next()
            pv = pbf(ps)
            for pr in range(8):
                S.tr(pv[:, pr * 128:(pr + 1) * 128], qr[:, 2 * pr:2 * pr + 2, :].rearrange("p h d -> p (h d)"), k.identb.all())
            S.copy(qT[:, :, (i - 2) * 128:(i - 1) * 128], pv.rearrange("p (c t) -> p c t", c=8), eng="act")
        if LVL < 5:
            continue
        kd = kdrot.next()
        S.copy(kd[:, :, 0, :], qr[:, 16:20, :])
        S.copy(kd[:, :, 1, :], qr[:, 16:20, :], eng="act")
        ps = ppB.next()
        pv = pbf(ps)
        for kv in range(4):
            S.tr(pv[:, kv * 128:(kv + 1) * 128], kd[:, kv].rearrange("p a d -> p (a d)"), k.identb.all())
        S.copy(kTd[:, :, i * 128:(i + 1) * 128], pv[:, 0:512].rearrange("p (c t) -> p c t", c=4))


def attn_core(S, k, b, layer, xs, qT, kTd, vaug):
    wo = S.sb("awo", [128, 8, 1024], BF16)
    for q in range(2):
        S.dma(wo[:, q * 4:(q + 1) * 4, :],
              View(k.attn_out_w.t[0].rearrange("(kk p) n -> p kk n", p=128)[:, q * 4:(q + 1) * 4, :], k.attn_out_w.res), eng="pool")
    g1 = S.sb("ag1", [128, 1024], F32)
    load_mod(S, k, layer, b, 2, g1.all())
    ptrot = S.rot("PT", [128, 512], BF16, 3)
    otrot = S.rot("otm", [128, 4, 1024], BF16, 2)
    oTrot = S.rot("aoT", [128, 8, 128], BF16, 2)
    xrot = S.rot("ax", [128, 1024], F32, 2)
    xorot = S.rot("axo", [128, 1024], F32, 2)
    ppS = S.pool([0, 1, 2, 3])
    ppAcc = S.pool([4, 5])
    ppO = S.pool([6, 7])
    for tqb in range(4):
        tq0 = tqb * 512
        otm = otrot.next()
        steps = [(h, tk) for h in range(16) for tk in range(NT)]

        def emit_scores(h, tk):
            pair, e = h // 2, h % 2
            kv = h // 4
            p0, p1 = e * 64, (e + 1) * 64
            psc = ppS.next()
            S.mm(psc.all(), kTd[p0:p1, kv, tk * 128:(tk + 1) * 128], qT[p0:p1, pair, tq0:tq0 + 512])
            return psc

        pending = emit_scores(*steps[0])
        acc = None
        for si, (h, tk) in enumerate(steps):
            kv = h // 4
            psc = pending
            if si + 1 < len(steps):
                pending = emit_scores(*steps[si + 1])
            if tk == 0:
                acc = ppAcc.next()
            PT = ptrot.next()
            S.act(PT.all(), psc.all(), AF.Exp, scale=0.125)
            for r in range(4):
                S.mm(acc[:, r * 128:r * 128 + 65], PT[:, r * 128:(r + 1) * 128], vaug[:, tk, kv, 0:65],
                     start=(tk == 0 and r == 0), stop=(tk == NT - 1 and r == 3))
            if tk == NT - 1:
                rc = k.small.next()
                a3 = acc.all().rearrange("p (r c) -> p r c", r=4)
                S.recip(rc[:, 0:4], a3[:, :, 64])
                S.tt(otm[:, :, h * 64:(h + 1) * 64], a3[:, :, 0:64], rc[:, 0:4].bc(2, 64), ALU.mult)
        for r in range(4):
            ti = 2 + tqb * 4 + r
            ps = ppO.next()
            pv = pbf(ps)
            for kk in range(8):
                S.tr(pv[:, kk * 128:(kk + 1) * 128], otm[:, r, kk * 128:(kk + 1) * 128], k.identb.all())
            oT = oTrot.next()
            S.copy(oT.all(), pv.rearrange("p (c t) -> p c t", c=8), eng="act")
            xt = xrot.next()
            S.dma(xt.all(), xs(ti))
            xo = xorot.next()
            for hf in range(2):
                po = ppO.next()
                for kk in range(8):
                    S.mm(po.all(), oT[:, kk, :], wo[:, kk, hf * 512:(hf + 1) * 512], start=(kk == 0), stop=(kk == 7))
                S.tt(xo[:, hf * 512:(hf + 1) * 512], po.all(), g1[:, hf * 512:(hf + 1) * 512], ALU.mult)
            S.tt(xo.all(), xo.all(), xt.all(), ALU.add)
            S.dma(xs(ti), xo.all())


def final_norm(S, k, xs_fn):
    with ExitStack() as st:
        S.stack = st
        fw = S.sb("fnw", [128, 1024], F32)
        load_bc(S, fw.all(), k.final_norm_w, k.final_norm_w.t)
        xrot = S.rot("fx", [128, 1024], F32, 3)
        jrot = S.rot("fj", [128, 1024], F32, 2)
        orot = S.rot("fo", [128, 1024], F32, 3)
        for b in range(NB):
            for j in range(NT - 2):
                xt = xrot.next()
                S.dma(xt.all(), xs_fn(b)(2 + j))
                rs = k.small.next()
                rms_rstd(S, k, rs[:, 2:3], xt.all(), 1024, jrot.next().all())
                xo = orot.next()
                S.stt(xo.all(), xt.all(), rs[:, 2:3], fw.all(), ALU.mult, ALU.mult)
                S.dma(View(k.out.t[b, j * 128:(j + 1) * 128, :], k.out.res), xo.all())
        S.phase_end()
    S.stack = None


WEIGHT_SPECS = [
    ("ada_w", [2, 1024, 6144]), ("ada_b", [2, 6144]), ("norm1_w", [2, 1024]), ("norm2_w", [2, 1024]),
    ("ssd_in_w", [1, 1024, 6208]), ("ssd_conv_w", [1, 5, 4096]), ("ssd_conv_b", [1, 4096]),
    ("ssd_dt_bias", [1, 2, 32]), ("ssd_A_log", [1, 2, 32]), ("ssd_D", [1, 32]), ("ssd_norm_w", [1, 2048]),
    ("ssd_out_w", [1, 2048, 1024]), ("attn_qkv_w", [1, 1024, 1536]), ("attn_q_norm_w", [1, 64]),
    ("attn_k_norm_w", [1, 64]), ("attn_out_w", [1, 1024, 1024]), ("moe_router_w", [2, 1024, 16]),
    ("moe_w1", [2, 16, 1024, 1024]), ("moe_w3", [2, 16, 1024, 1024]), ("moe_w2", [2, 16, 1024, 1024]),
    ("final_norm_w", [1024]),
]


def build_program(stop=None, dbg=False, only=None):
    nc = bass.Bass("TRN2", target_bir_lowering=False)
    k = K()
    with ExitStack() as st:
        S = Sched(nc, st)
        S.init_psum(8)

        def din(name, shape):
            t = nc.dram_tensor(name, list(shape), F32, kind="ExternalInput")
            return Tl(t.ap(), 1, name)

        k.x = din("x", [NB, LAT, D])
        k.c = din("c", [NB, D])
        k.ctx = din("ctx", [NB, CTXL, D])
        k.c_ctx = din("c_ctx", [D])
        for name, shape in WEIGHT_SPECS:
            setattr(k, name, din(name, shape))
        k.cos = din("rope_cos", [LAT, 32])
        k.sin = din("rope_sin", [LAT, 32])
        k.out = Tl(nc.dram_tensor("out", [NB, LAT, D], F32, kind="ExternalOutput").ap(), 1, "out")
        xkind = "ExternalOutput"
        k.xs = [Tl(nc.dram_tensor(f"xs{b}", [NT, 128, D], F32, kind=xkind).ap(), NT, f"xs{b}") for b in range(NB)]
        k.modv = [S.dram(f"modv{l}", [3, 6144], F32) for l in range(2)]
        k.xtm_d = S.dram("xtm_d", [NT, 128, 3072], BF16)
        k.bc_d = S.dram("bc_d", [16, 128, TOK], BF16)
        k.yf_d = S.dram("yf_d", [NT, 128, 2048], F32)
        k.gn_d = S.dram("gn_d", [NT, 128, 2048], BF16)
        k.y_d = S.dram("y_d", [16, 576, 1024], BF16)
        k.small = S.rot("small", [128, 4], F32, 12)
        setup_consts(S, k)
        S.phase_end()

        def in_src(b):
            def f(i):
                if i < 2:
                    return View(k.ctx.t[b, i * 128:(i + 1) * 128, :], k.ctx.res)
                return View(k.x.t[b, (i - 2) * 128:(i - 1) * 128, :], k.x.res)
            return f

        def xs_fn(b):
            def f(i):
                return View(k.xs[b].t[i], [k.xs[b].res[i]])
            return f

        stages = []
        stages.append(("mod0", lambda: phase_mod(S, k, 0)))
        for b in range(NB):
            stages.append((f"ssd{b}", lambda b=b: ssd_mixer(S, k, b, 0, in_src(b), xs_fn(b))))
        stages.append(("moe0", lambda: moe_layer(S, k, 0, xs_fn)))
        stages.append(("mod1", lambda: phase_mod(S, k, 1)))
        for b in range(NB):
            stages.append((f"attn{b}", lambda b=b: attn_mixer(S, k, b, 1, xs_fn(b))))
        stages.append(("moe1", lambda: moe_layer(S, k, 1, xs_fn)))
        stages.append(("final", lambda: final_norm(S, k, xs_fn)))
        if only is not None:
            with ExitStack() as st2:
                S.stack = st2
                rot = S.rot("initx", [128, D], F32, 3)
                for b in range(NB):
                    xi = din(f"xs_init{b}", [NT, 128, D])
                    for i in range(NT):
                        t = rot.next()
                        S.dma(t.all(), View(xi.t[i], xi.res))
                        S.dma(xs_fn(b)(i), t.all())
                S.phase_end()
        for name, fn in stages:
            if only is not None and name not in only:
                continue
            S.stack = st
            fn()
            if stop == name:
                break
        S.stack = st
        outs = [k.out.all()] + [x.all() for x in k.xs]
        S.finish(outs)
        k.ninst = S.ninst
    return nc, k


_ROPE = None


def rope_tables():
    global _ROPE
    if _ROPE is None:
        S_ = LAT
        row = np.repeat(np.arange(S_ // 64), 64).astype(np.float32)
        col = np.tile(np.arange(64), S_ // 64).astype(np.float32)
        inv = (np.float32(10000.0) ** (-np.arange(0, 32, 2, dtype=np.float32) / np.float32(32))).astype(np.float32)
        ang = np.concatenate([row[:, None] * inv, col[:, None] * inv], axis=-1).astype(np.float32)
        _ROPE = (np.cos(ang).astype(np.float32), np.sin(ang).astype(np.float32))
    return _ROPE


_PROG = {}


def kernel(**inputs):
    n_cores = 8
    if "full" not in _PROG:
        _PROG["full"] = build_program()[0]
    nc = _PROG["full"]
    cos, sin = rope_tables()
    shared = {name: np.ascontiguousarray(inputs[name], dtype=np.float32) for name, _ in WEIGHT_SPECS}
    shared["c_ctx"] = np.ascontiguousarray(inputs["c_ctx"], dtype=np.float32)
    shared["rope_cos"] = cos
    shared["rope_sin"] = sin
    in_maps = []
    for cid in range(n_cores):
        m = dict(shared)
        sl = slice(cid * NB, (cid + 1) * NB)
        m["x"] = np.ascontiguousarray(inputs["x"][sl], dtype=np.float32)
        m["c"] = np.ascontiguousarray(inputs["c"][sl], dtype=np.float32)
        m["ctx"] = np.ascontiguousarray(inputs["ctx"][sl], dtype=np.float32)
        in_maps.append(m)
    res = run_bass_kernel_spmd(nc, in_maps, core_ids=list(range(n_cores)))
    return np.concatenate([np.asarray(r["out"]) for r in res.results], axis=0).astype(np.float32)
```

```python
from contextlib import ExitStack
import math
import numpy as np
import concourse.bass as bass
import concourse.mybir as mybir
from concourse.bass_utils import run_bass_kernel_spmd

F32 = mybir.dt.float32
BF16 = mybir.dt.bfloat16
AF = mybir.ActivationFunctionType
ALU = mybir.AluOpType
AX = mybir.AxisListType

ENGS = ["pe", "dve", "act", "pool", "sp"]
NLANES = 28
NLANES_HW = 16


class Res:
    __slots__ = ("w", "r", "name")

    def __init__(self, name=""):
        self.w = None
        self.r = []
        self.name = name


class View:
    __slots__ = ("ap", "res")

    def __init__(self, ap, res):
        self.ap = ap
        self.res = res

    def __getitem__(self, idx):
        return View(self.ap[idx], self.res)

    def rearrange(self, *a, **k):
        return View(self.ap.rearrange(*a, **k), self.res)

    def bitcast(self, dt):
        return View(self.ap.bitcast(dt), self.res)

    def bc(self, axis, n):
        a = self.ap.unsqueeze(axis)
        shp = list(a.shape)
        shp[axis] = n
        return View(a.to_broadcast(shp), self.res)


class Tl:
    def __init__(self, t, nslots=1, name=""):
        self.t = t
        self.nslots = nslots
        self.res = [Res(f"{name}[{i}]") for i in range(nslots)]
        self.name = name

    def __getitem__(self, idx):
        return View(self.t[idx], self.res)

    def all(self):
        return View(self.t[:], self.res)

    def s(self, i, j=None):
        if j is None:
            return View(self.t[:, i], [self.res[i]])
        return View(self.t[:, i:j], self.res[i:j])


class Rot:
    def __init__(self, tiles):
        self.tiles = tiles
        self.i = 0

    def next(self):
        t = self.tiles[self.i % len(self.tiles)]
        self.i += 1
        return t


class Sched:
    def __init__(self, nc, stack):
        self.nc = nc
        self.stack = stack
        self.q = {e: [] for e in ENGS}
        self.sem = {e: stack.enter_context(nc.semaphore(f"s_{e}")) for e in ENGS}
        self.cnt = {e: 0 for e in ENGS}
        self.lane_sem = [stack.enter_context(nc.semaphore(f"l_{i}")) for i in range(NLANES)]
        self.lane_cnt = [0] * NLANES
        self.lane_next = 0
        self.lane_next_sw = 0
        self.seen = {e: {} for e in ENGS}
        self.ninst = 0
        self.psum_banks = []
        self.uid = 0

    def sb(self, name, shape, dtype, nslots=1):
        self.uid += 1
        nm = f"{name}_{self.uid}"
        t = self.stack.enter_context(self.nc.sbuf_tensor(nm, list(shape), dtype))
        return Tl(t, nslots, nm)

    def rot(self, name, shape, dtype, n):
        return Rot([self.sb(f"{name}{i}", shape, dtype) for i in range(n)])

    def dram(self, name, shape, dtype, kind="Internal", nslots=1):
        t = self.nc.dram_tensor(name, list(shape), dtype, kind=kind)
        return Tl(t.ap(), nslots, name)

    def init_psum(self, n=8):
        for i in range(n):
            t = self.stack.enter_context(self.nc.psum_tensor(f"ps{i}", [128, 512], F32))
            self.psum_banks.append(Tl(t, 1, f"ps{i}"))

    def pool(self, idx):
        return Rot([self.psum_banks[i] for i in idx])

    def _collect(self, eng, reads, writes):
        waits = {}

        def need(tok):
            if tok is None:
                return
            kind, key, val = tok
            if kind == "e" and key == eng and eng == "pe":
                return
            k = (kind, key)
            if waits.get(k, 0) < val:
                waits[k] = val

        for v in reads:
            for r in v.res:
                need(r.w)
        for v in writes:
            for r in v.res:
                need(r.w)
                for tok in r.r:
                    need(tok)
        out = []
        seen = self.seen[eng]
        for k, val in waits.items():
            if seen.get(k, 0) >= val:
                continue
            seen[k] = val
            out.append((k, val))
        return out

    def _mark(self, tok, reads, writes):
        for v in writes:
            for r in v.res:
                r.w = tok
                r.r = []
        for v in reads:
            for r in v.res:
                r.r = [t for t in r.r if (t[0], t[1]) != (tok[0], tok[1])]
                r.r.append(tok)

    def _semof(self, k):
        kind, key = k
        return self.sem[key] if kind == "e" else self.lane_sem[key]

    def emit(self, eng, fn, reads=(), writes=()):
        reads = list(reads)
        writes = list(writes)
        waits = self._collect(eng, reads, writes)
        self.cnt[eng] += 1
        val = self.cnt[eng]
        sem = self.sem[eng]
        wl = [(self._semof(k), v) for k, v in waits]

        def run(e, wl=wl, fn=fn, sem=sem):
            for s, v in wl:
                e.wait_ge(s, v)
            fn(e).then_inc(sem, 1)

        self.q[eng].append(run)
        self._mark(("e", eng, val), reads, writes)
        self.ninst += 1

    def dma(self, out, in_, eng="sp", **kw):
        if eng == "pool":
            lane = NLANES_HW + self.lane_next_sw % (NLANES - NLANES_HW)
            self.lane_next_sw += 1
        else:
            lane = self.lane_next % NLANES_HW
            self.lane_next += 1
        reads = [in_]
        writes = [out]
        waits = self._collect(eng, reads, writes)
        prev = self.lane_cnt[lane]
        k = ("d", lane)
        if prev > 0 and self.seen[eng].get(k, 0) < prev:
            self.seen[eng][k] = prev
            waits = [w for w in waits if w[0] != k] + [(k, prev)]
        self.lane_cnt[lane] += 16
        val = self.lane_cnt[lane]
        sem = self.lane_sem[lane]
        wl = [(self._semof(k2), v) for k2, v in waits]
        oap, iap = out.ap, in_.ap

        def run(e, wl=wl, sem=sem, oap=oap, iap=iap, kw=kw):
            for s, v in wl:
                e.wait_ge(s, v)
            e.dma_start(out=oap, in_=iap, **kw).then_inc(sem, 16)

        self.q[eng].append(run)
        self._mark(("d", lane, val), reads, writes)
        self.ninst += 1

    def wait_all(self, eng, views):
        waits = self._collect(eng, list(views), [])
        wl = [(self._semof(k), v) for k, v in waits]

        def run(e, wl=wl):
            for s, v in wl:
                e.wait_ge(s, v)

        self.q[eng].append(run)

    def barrier(self):
        snap_e = dict(self.cnt)
        snap_l = list(self.lane_cnt)
        for eng in ENGS:
            wl = []
            seen = self.seen[eng]
            for e2, v in snap_e.items():
                if v > 0 and seen.get(("e", e2), 0) < v and not (e2 == eng and eng == "pe"):
                    seen[("e", e2)] = v
                    wl.append((self.sem[e2], v))
            for i, v in enumerate(snap_l):
                if v > 0 and seen.get(("d", i), 0) < v:
                    seen[("d", i)] = v
                    wl.append((self.lane_sem[i], v))

            def run(e, wl=wl):
                for s, v in wl:
                    e.wait_ge(s, v)

            self.q[eng].append(run)

    def flush(self):
        nc = self.nc
        q = self.q
        self.q = {e: [] for e in ENGS}
        with nc.Block() as block:
            @block.sync
            def _(e):
                for f in q["sp"]:
                    f(e)

            @block.tensor
            def _(e):
                for f in q["pe"]:
                    f(e)

            @block.vector
            def _(e):
                for f in q["dve"]:
                    f(e)

            @block.scalar
            def _(e):
                for f in q["act"]:
                    f(e)

            @block.gpsimd
            def _(e):
                for f in q["pool"]:
                    f(e)

    def phase_end(self):
        self.barrier()
        self.flush()

    def finish(self, final_views):
        self.wait_all("sp", final_views)
        self.flush()

    def mm(self, out, lhsT, rhs, start=True, stop=True):
        self.emit("pe", lambda e: e.matmul(out.ap, lhsT.ap, rhs.ap, start=start, stop=stop),
                  reads=[lhsT, rhs], writes=[out])

    def tr(self, out, in_, ident):
        self.emit("pe", lambda e: e.transpose(out.ap, in_.ap, ident.ap), reads=[in_, ident], writes=[out])

    def act(self, out, in_, func, bias=None, scale=None, accum_out=None):
        reads = [in_]
        writes = [out]
        kw = {}
        if bias is not None:
            if isinstance(bias, View):
                reads.append(bias)
                kw["bias"] = bias.ap
            else:
                kw["bias"] = bias
        if scale is not None:
            if isinstance(scale, View):
                reads.append(scale)
                kw["scale"] = scale.ap
            else:
                kw["scale"] = scale
        if accum_out is not None:
            writes.append(accum_out)
            kw["accum_out"] = accum_out.ap
        self.emit("act", lambda e: e.activation(out.ap, in_.ap, func, **kw), reads=reads, writes=writes)

    def tt(self, out, a, b, op, eng="dve"):
        self.emit(eng, lambda e: e.tensor_tensor(out=out.ap, in0=a.ap, in1=b.ap, op=op), reads=[a, b], writes=[out])

    def ts(self, out, a, s1, op0, s2=None, op1=None, accum_out=None, eng="dve"):
        reads = [a]
        writes = [out]
        s1v = s1.ap if isinstance(s1, View) else s1
        s2v = s2.ap if isinstance(s2, View) else s2
        if isinstance(s1, View):
            reads.append(s1)
        if isinstance(s2, View):
            reads.append(s2)
        kw = {}
        if op1 is not None:
            kw["op1"] = op1
        if accum_out is not None:
            kw["accum_out"] = accum_out.ap
            writes.append(accum_out)
        self.emit(eng, lambda e: e.tensor_scalar(out=out.ap, in0=a.ap, scalar1=s1v, scalar2=s2v, op0=op0, **kw),
                  reads=reads, writes=writes)

    def stt(self, out, a, s, b, op0, op1, eng="dve"):
        reads = [a, b]
        sv = s.ap if isinstance(s, View) else s
        if isinstance(s, View):
            reads.append(s)
        self.emit(eng, lambda e: e.scalar_tensor_tensor(out=out.ap, in0=a.ap, scalar=sv, in1=b.ap, op0=op0, op1=op1),
                  reads=reads, writes=[out])

    def copy(self, out, in_, eng="dve"):
        if eng == "act":
            self.emit("act", lambda e: e.activation(out.ap, in_.ap, AF.Copy), reads=[in_], writes=[out])
        else:
            self.emit(eng, lambda e: e.tensor_copy(out=out.ap, in_=in_.ap), reads=[in_], writes=[out])

    def memset(self, out, val, eng="dve"):
        self.emit(eng, lambda e: e.memset(out.ap, val), reads=[], writes=[out])

    def reduce(self, out, in_, op, axis=None):
        axis = axis or AX.X
        self.emit("dve", lambda e: e.tensor_reduce(out=out.ap, in_=in_.ap, axis=axis, op=op), reads=[in_], writes=[out])

    def recip(self, out, in_):
        self.emit("dve", lambda e: e.reciprocal(out=out.ap, in_=in_.ap), reads=[in_], writes=[out])


def pbf(bank):
    return View(bank.t[:].bitcast(BF16), bank.res)

D = 1024
LAT = 2048
CTXL = 256
NT = 18
TOK = NT * 128
DI = 2048
NH = 32
NG = 8
EPS = 1e-6
NB = 2


class K:
    pass


def setup_consts(S, k):
    nc = S.nc
    k.iota = S.sb("iota", [128, 256], F32)
    k.pid = S.sb("pid", [128, 1], F32)
    k.pid128 = S.sb("pid128", [128, 1], F32)
    k.eps = S.sb("epsc", [128, 1], F32)
    S.emit("pool", lambda e: e.iota(k.iota.t[:], [[1, 256]], base=0, channel_multiplier=0,
                                    allow_small_or_imprecise_dtypes=True), writes=[k.iota.all()])
    S.emit("pool", lambda e: e.iota(k.pid.t[:], [[0, 1]], base=0, channel_multiplier=1,
                                    allow_small_or_imprecise_dtypes=True), writes=[k.pid.all()])
    S.ts(k.pid128.all(), k.pid.all(), 128.0, ALU.add)
    S.memset(k.eps.all(), EPS)
    io = k.iota[:, 0:128]

    def mk(name, dt, op):
        t = S.sb(name, [128, 128], dt)
        S.ts(t.all(), io, k.pid.all(), op)
        return t

    k.identb = mk("identb", BF16, ALU.is_equal)
    k.identf = mk("identf", F32, ALU.is_equal)
    k.U_le = mk("U_le", BF16, ALU.is_ge)
    k.L_ge = mk("L_ge", BF16, ALU.is_le)
    k.SLt = mk("SLt", BF16, ALU.is_lt)
    k.SUt = mk("SUt", BF16, ALU.is_gt)
    k.U_le_f = mk("U_le_f", F32, ALU.is_ge)
    k.L_ge_f = mk("L_ge_f", F32, ALU.is_le)
    k.ones_f = S.sb("ones_f", [128, 128], F32)
    S.memset(k.ones_f.all(), 1.0)
    k.ones_b = S.sb("ones_b", [128, 128], BF16)
    S.memset(k.ones_b.all(), 1.0)


def load_bc(S, dst, dram_tl, ap):
    S.dma(dst, View(ap.partition_broadcast(dst.ap.shape[0]), dram_tl.res))


def rms_rstd(S, k, rs, src, n, junk):
    ss = k.small.next()
    S.act(junk, src, AF.Square, accum_out=ss[:, 0:1])
    S.act(ss[:, 1:2], ss[:, 0:1], AF.Sqrt, bias=k.eps.all(), scale=1.0 / n)
    S.recip(rs, ss[:, 1:2])


def phase_mod(S, k, layer):
    with ExitStack() as st:
        S.stack = st
        cin = S.sb("cin", [128, 3, 8], F32)
        for b in range(NB):
            S.dma(cin[:, b, :], View(k.c.t[b].rearrange("(p kk) -> p kk", kk=8), k.c.res))
        S.dma(cin[:, 2, :], View(k.c_ctx.t.rearrange("(p kk) -> p kk", kk=8), k.c_ctx.res))
        sc = S.sb("silc", [128, 8, 3], BF16)
        S.act(sc.all().rearrange("p kk v -> p v kk"), cin.all(), AF.Silu)
        bias = S.sb("adab", [3, 6144], F32)
        load_bc(S, bias.all(), k.ada_b, k.ada_b.t[layer])
        nw = S.sb("nw12", [3, 2, 1024], F32)
        load_bc(S, nw[:, 0, :], k.norm1_w, k.norm1_w.t[layer])
        load_bc(S, nw[:, 1, :], k.norm2_w, k.norm2_w.t[layer])
        res = S.sb("modres", [3, 6144], F32)
        wrot = S.rot("adaw", [128, 8, 1024], BF16, 2)
        pp = S.pool([0, 1, 2, 3])
        for blk in range(6):
            w = wrot.next()
            S.dma(w.all(), View(k.ada_w.t[layer].rearrange("(p kk) n -> p kk n", kk=8)[:, :, blk * 1024:(blk + 1) * 1024],
                                k.ada_w.res), eng="pool")
            for half in range(2):
                ps = pp.next()
                for kk in range(8):
                    S.mm(ps[0:3, :], sc[:, kk, :], w[:, kk, half * 512:(half + 1) * 512], start=(kk == 0), stop=(kk == 7))
                c0 = blk * 1024 + half * 512
                S.tt(res[:, c0:c0 + 512], ps[0:3, :], bias[:, c0:c0 + 512], ALU.add)
        for j, blk in enumerate((1, 4)):
            S.stt(res[:, blk * 1024:(blk + 1) * 1024], res[:, blk * 1024:(blk + 1) * 1024], 1.0, nw[:, j, :], ALU.add, ALU.mult)
        S.dma(k.modv[layer].all(), res.all())
        S.phase_end()


def load_mod(S, k, layer, row, blk, dst):
    load_bc(S, dst, k.modv[layer], k.modv[layer].t[row, blk * 1024:(blk + 1) * 1024])


def norm_T(S, k, b, layer, which, src_fn, hT, tiles, pp):
    blkA, blkS = (1, 0) if which == 1 else (4, 3)
    mods = {}
    for kind in (0, 1):
        if not any((ti < 2) == (kind == 0) for ti, _ in tiles):
            continue
        A = S.sb("modA", [128, 1024], F32)
        sh = S.sb("modS", [128, 1024], F32)
        row = 2 if kind == 0 else b
        load_mod(S, k, layer, row, blkA, A.all())
        load_mod(S, k, layer, row, blkS, sh.all())
        mods[kind] = (A, sh)
    xrot = S.rot("nx", [128, 1024], F32, 3)
    trot = S.rot("ntmp", [128, 1024], F32, 2)
    hrot = S.rot("nhb", [128, 1024], BF16, 2)
    for ti, col in tiles:
        A, sh = mods[0 if ti < 2 else 1]
        xt = xrot.next()
        S.dma(xt.all(), src_fn(ti))
        tmp = trot.next()
        rs = k.small.next()
        rms_rstd(S, k, rs[:, 2:3], xt.all(), 1024, tmp.all())
        S.stt(tmp.all(), xt.all(), rs[:, 2:3], A.all(), ALU.mult, ALU.mult)
        hb = hrot.next()
        S.tt(hb.all(), tmp.all(), sh.all(), ALU.add)
        ps = pp.next()
        pv = pbf(ps)
        for kk in range(8):
            S.tr(pv[:, kk * 128:(kk + 1) * 128], hb[:, kk * 128:(kk + 1) * 128], k.identb.all())
        S.copy(hT[:, :, col:col + 128], pv.rearrange("p (kk t) -> p kk t", kk=8), eng="act")

def ssd_mixer(S, k, b, layer, src_fn, dst_fn):
    with ExitStack() as st_outer:
        S.stack = st_outer
        hT = S.sb("hT", [128, 8, TOK], BF16)
        with ExitStack() as st:
            S.stack = st
            norm_T(S, k, b, layer, 1, src_fn, hT, [(i, i * 128) for i in range(NT)], S.pool([0, 1, 2, 3]))
            S.phase_end()
        with ExitStack() as st:
            S.stack = st
            ssd_phase_a(S, k, hT)
            S.phase_end()
        for d in (0, 1):
            with ExitStack() as st:
                S.stack = st
                ssd_scan(S, k, b, layer, hT, d, src_fn, dst_fn)
                S.phase_end()
        S.stack = st_outer
    with ExitStack() as st:
        S.stack = st
        ssd_phase_d(S, k, b, layer, src_fn, dst_fn)
        S.phase_end()
    S.stack = None


def ssd_phase_a(S, k, hT):
    stg = S.sb("cstg", [6, 4096], F32)
    S.dma(stg[0:5, :], k.ssd_conv_w[0])
    S.dma(stg[5:6, :], View(k.ssd_conv_b.t[0:1, :], k.ssd_conv_b.res))
    cw = S.sb("cw", [128, 32, 8], F32)
    pp = S.pool([0, 1, 2, 3])
    for cc0 in range(0, 32, 8):
        ps = pp.next()
        for j in range(8):
            cc = cc0 + j
            S.tr(ps[:, j * 8:j * 8 + 6], stg[0:6, cc * 128:(cc + 1) * 128], k.identf[0:6, 0:6])
        S.copy(cw[:, cc0:cc0 + 8, :], ps[:, 0:64].rearrange("p (j e) -> p j e", e=8))
    wrot = S.rot("wA", [128, 8, 512], BF16, 2)
    urot = S.rot("uA", [128, TOK], F32, 2)
    arot = S.rot("accA", [128, TOK], F32, 2)
    crot = S.rot("cvA", [128, TOK], BF16, 2)
    trot = S.rot("tmA", [128, 8, 128], BF16, 3)
    pp2 = S.pool([4, 5, 6, 7])
    blocks = [(0, 256), (256, 512), (768, 512), (1280, 512), (1792, 512)]
    segs = [(0, 256), (256, TOK)]
    w = None
    for cc in range(32):
        if cc % 4 == 0:
            w = wrot.next()
            c0 = 2048 + cc * 128
            S.dma(w.all(), View(k.ssd_in_w.t[0].rearrange("(kk p) n -> p kk n", p=128)[:, :, c0:c0 + 512], k.ssd_in_w.res),
                  eng="pool")
        u = urot.next()
        for (t0, n) in blocks:
            ps = pp.next()
            for kk in range(8):
                S.mm(ps[:, 0:n], w[:, kk, (cc % 4) * 128:(cc % 4 + 1) * 128], hT[:, kk, t0:t0 + n],
                     start=(kk == 0), stop=(kk == 7))
            S.copy(u[:, t0:t0 + n], ps[:, 0:n], eng="act")
        acc = arot.next()
        for (a, e) in segs:
            S.ts(acc[:, a:e], u[:, a:e], cw[:, cc, 2:3], ALU.mult, s2=cw[:, cc, 5:6], op1=ALU.add)
            for tap in (0, 1, 3, 4):
                dl = tap - 2
                lo = a + max(0, -dl)
                hi = e - max(0, dl)
                S.stt(acc[:, lo:hi], u[:, lo + dl:hi + dl], cw[:, cc, tap:tap + 1], acc[:, lo:hi], ALU.mult, ALU.add)
        cv = crot.next()
        S.act(cv.all(), acc.all(), AF.Silu)
        if cc >= 16:
            S.dma(View(k.bc_d.t[cc - 16], k.bc_d.res), cv.all())
        if cc < 24:
            for i0 in range(0, NT, 8):
                n = min(8, NT - i0)
                ps = pp2.next()
                pv = pbf(ps)
                for j in range(n):
                    S.tr(pv[:, j * 128:(j + 1) * 128], cv[:, (i0 + j) * 128:(i0 + j + 1) * 128], k.identb.all())
                tm = trot.next()
                S.copy(tm[:, 0:n, :], pv[:, 0:n * 128].rearrange("p (j c) -> p j c", j=n))
                S.dma(View(k.xtm_d.t[i0:i0 + n, :, cc * 128:(cc + 1) * 128].rearrange("i t c -> t i c"), k.xtm_d.res),
                      tm[:, 0:n, :])


def ssd_scan(S, k, b, layer, hT, d, src_fn, dst_fn):
    fwd = (d == 0)
    dc = d * 32
    wdt = S.sb("wdt", [128, 8, 64], BF16)
    S.dma(wdt.all(), View(k.ssd_in_w.t[0].rearrange("(kk p) n -> p kk n", p=128)[:, :, 6144:6208], k.ssd_in_w.res), eng="pool")
    dtb = S.sb("dtb", [128, 64], F32)
    load_bc(S, dtb.all(), k.ssd_dt_bias, k.ssd_dt_bias.t[0].rearrange("a h -> (a h)"))
    Abc = S.sb("Abc", [128, 64], F32)
    load_bc(S, Abc.all(), k.ssd_A_log, k.ssd_A_log.t[0].rearrange("a h -> (a h)"))
    S.act(Abc.all(), Abc.all(), AF.Exp)
    S.ts(Abc.all(), Abc.all(), -1.0, ALU.mult)
    h = S.sb("hst", [128, NG, 256], F32, nslots=NG)
    hb = S.sb("hstb", [128, NG, 256], BF16, nslots=NG)
    S.memset(h.all(), 0.0)
    S.memset(hb.all(), 0.0)
    if not fwd:
        wz = S.sb("wz", [128, 8, 2048], BF16)
        for q in range(4):
            S.dma(wz[:, :, q * 512:(q + 1) * 512],
                  View(k.ssd_in_w.t[0].rearrange("(kk p) n -> p kk n", p=128)[:, :, q * 512:(q + 1) * 512], k.ssd_in_w.res), eng="pool")
        Dbc = S.sb("Dbc", [128, 32], F32)
        load_bc(S, Dbc.all(), k.ssd_D, k.ssd_D.t[0])
        nwbc = S.sb("snw", [128, 2048], F32)
        load_bc(S, nwbc.all(), k.ssd_norm_w, k.ssd_norm_w.t[0])
        yfrot = S.rot("yf", [128, 2048], F32, 1)
        szrot = S.rot("sz", [128, 2048], F32, 1)
        gnrot = S.rot("gn", [128, 2048], BF16, 1)
    xbrot = S.rot("xB", [128, 3072], BF16, 2)
    bcrot = S.rot("bct", [128, 16, 128], BF16, 2)
    ydrot = S.rot("ydir", [128, 2048], F32, 2)
    xdtrot = S.rot("xdt", [128, 2048], BF16, 2)
    xddrot = S.rot("xdd", [128, 2048], BF16, 2)
    smrot = S.rot("ssm", [128, 8, 32], F32, 2)
    lharot = S.rot("lha", [128, 32, 128], BF16, 2)
    cbrot = S.rot("cbm", [128, 128], BF16, 4)
    sgrot = S.rot("seg", [128, 512], BF16, 4)
    mtrot = S.rot("MT", [128, 4, 128], BF16, 4)
    t3rot = S.rot("t3", [128, 256], F32, 6)
    ppA = S.pool([0, 1, 2, 3] if fwd else [0, 1, 2])
    ppB = S.pool([3])
    ppY = S.pool([4, 5, 6, 7])
    U = k.U_le if fwd else k.L_ge
    Uf = k.U_le_f if fwd else k.L_ge_f
    LM = k.SLt if fwd else k.SUt
    order = list(range(NT)) if fwd else [1, 0] + list(range(NT - 1, 1, -1))

    def prologue(i):
        c = {}
        c["i"] = i
        xB = c["xB"] = xbrot.next()
        S.dma(xB.all(), View(k.xtm_d.t[i], k.xtm_d.res))
        bct = c["bct"] = bcrot.next()
        S.dma(bct.all(), View(k.bc_d.t[:, :, i * 128:(i + 1) * 128].rearrange("c n t -> n c t"), k.bc_d.res))
        sm = c["sm"] = smrot.next()
        ps = ppA.next()
        for kk in range(8):
            S.mm(ps[:, 0:64], hT[:, kk, i * 128:(i + 1) * 128], wdt[:, kk, :], start=(kk == 0), stop=(kk == 7))
        S.tt(sm[:, 0, :], ps[:, dc:dc + 32], dtb[:, dc:dc + 32], ALU.add)
        S.act(sm[:, 1, :], sm[:, 0, :], AF.Exp)
        S.act(sm[:, 2, :], sm[:, 1, :], AF.Ln, bias=1.0)
        S.tt(sm[:, 3, :], sm[:, 2, :], Abc[:, dc:dc + 32], ALU.mult)
        lha = c["lha"] = lharot.next()
        S.tt(lha.all(), LM.all().bc(1, 32), sm[:, 3, :].bc(2, 128), ALU.mult)
        ps2 = ppA.next()
        S.mm(ps2[:, 0:32], Uf.all(), sm[:, 3, :])
        S.mm(ps2[:, 32:64], k.ones_f.all(), sm[:, 3, :])
        S.copy(sm[:, 4:6, :], ps2[:, 0:64].rearrange("p (a h) -> p a h", a=2))
        S.act(sm[:, 6, :], sm[:, 4, :], AF.Exp)
        S.tt(sm[:, 0, :], sm[:, 5, :], sm[:, 4, :], ALU.subtract)
        S.act(sm[:, 7, :], sm[:, 0, :], AF.Exp)
        S.act(sm[:, 1, :], sm[:, 5, :], AF.Exp)
        S.tt(sm[:, 0, :], sm[:, 2, :], sm[:, 7, :], ALU.mult)
        xdt = c["xdt"] = xdtrot.next()
        xdd = c["xdd"] = xddrot.next()
        x3 = c["x3"] = xB[:, 0:2048].rearrange("p (h j) -> p h j", h=32)
        S.tt(xdt.all().rearrange("p (h j) -> p h j", h=32), x3, sm[:, 2, :].bc(2, 64), ALU.mult)
        S.tt(xdd.all().rearrange("p (h j) -> p h j", h=32), x3, sm[:, 0, :].bc(2, 64), ALU.mult)
        c["ydir"] = ydrot.next()
        return c

    def stA(c, g):
        bct = c["bct"]
        BT = bct[:, g, :]
        CT = bct[:, 8 + g, :]
        pc = ppA.next()
        S.mm(pc[:, 0:128], BT, CT)
        pD = ppA.next()
        for r in range(4):
            S.mm(pD[:, r * 128:(r + 1) * 128], c["lha"][:, 4 * g + r, :], U.all())
        c[("a", g)] = (pc, pD, CT)

    def stB(c, g):
        pc, pD, CT = c[("a", g)]
        cbm = cbrot.next()
        S.tt(cbm.all(), pc[:, 0:128], U.all(), ALU.mult)
        seg = sgrot.next()
        S.act(seg.all(), pD.all(), AF.Exp)
        c[("f", g)] = (cbm, seg, CT)

    def stC1(c, g):
        cbm, seg, CT = c[("f", g)]
        MT = mtrot.next()
        S.tt(MT.all(), seg.all().rearrange("p (r l) -> p r l", r=4), cbm.all().bc(1, 4), ALU.mult)
        c[("m", g)] = MT

    def stC2(c, g):
        cbm, seg, CT = c[("f", g)]
        MT = c[("m", g)]
        xB, xdt, xdd = c["xB"], c["xdt"], c["xdd"]
        py = ppY.next()
        for r in range(4):
            hh = 4 * g + r
            S.mm(py[:, r * 64:(r + 1) * 64], MT[:, r, :], xdt[:, hh * 64:(hh + 1) * 64])
        S.mm(py[:, 256:512], CT, hb.s(g))
        pst = ppY.next()
        S.mm(pst[:, 0:256], xB[:, 2048 + g * 128:2048 + (g + 1) * 128], xdd[:, g * 256:(g + 1) * 256])
        c[("p", g)] = (py, pst)

    def stE(c, g):
        py, pst = c[("p", g)]
        sm, ydir = c["sm"], c["ydir"]
        t3 = t3rot.next()
        S.tt(t3.all().rearrange("p (r j) -> p r j", r=4), py[:, 256:512].rearrange("p (r j) -> p r j", r=4),
             sm[:, 6, 4 * g:4 * g + 4].bc(2, 64), ALU.mult)
        S.tt(ydir[:, g * 256:(g + 1) * 256], t3.all(), py[:, 0:256], ALU.add)
        t4 = t3rot.next()
        S.tt(t4.all().rearrange("p (r j) -> p r j", r=4), h.s(g).rearrange("p (r j) -> p r j", r=4),
             sm[:, 1, 4 * g:4 * g + 4].bc(2, 64), ALU.mult)
        S.tt(h.s(g), t4.all(), pst[:, 0:256], ALU.add)
        S.copy(hb.s(g), h.s(g), eng="act")

    def epilogue(c):
        i = c["i"]
        ydir = c["ydir"]
        if fwd:
            S.dma(View(k.yf_d.t[i], k.yf_d.res), ydir.all())
            return
        yf = yfrot.next()
        S.dma(yf.all(), View(k.yf_d.t[i], k.yf_d.res))
        S.tt(ydir.all(), ydir.all(), yf.all(), ALU.add)
        S.tt(yf.all().rearrange("p (h j) -> p h j", h=32), c["x3"], Dbc.all().bc(2, 64), ALU.mult)
        S.tt(ydir.all(), ydir.all(), yf.all(), ALU.add)
        sz = szrot.next()
        for q in range(4):
            pz = ppB.next()
            for kk in range(8):
                S.mm(pz.all(), hT[:, kk, i * 128:(i + 1) * 128], wz[:, kk, q * 512:(q + 1) * 512],
                     start=(kk == 0), stop=(kk == 7))
            S.act(sz[:, q * 512:(q + 1) * 512], pz.all(), AF.Silu)
        S.tt(ydir.all(), ydir.all(), sz.all(), ALU.mult)
        rs = k.small.next()
        rms_rstd(S, k, rs[:, 2:3], ydir.all(), 2048, sz.all())
        gn = gnrot.next()
        S.stt(gn.all(), ydir.all(), rs[:, 2:3], nwbc.all(), ALU.mult, ALU.mult)
        S.dma(View(k.gn_d.t[i], k.gn_d.res), gn.all())

    ctxs = {0: prologue(order[0])}
    n = len(order) * NG
    for st_ in range(n + 2):
        if st_ < n:
            ci, g = divmod(st_, NG)
            if g == 3 and ci + 1 < len(order):
                ctxs[ci + 1] = prologue(order[ci + 1])
            stA(ctxs[ci], g)
        if 1 <= st_ <= n:
            ci, g = divmod(st_ - 1, NG)
            stC1(ctxs[ci], g)
        if st_ < n:
            ci, g = divmod(st_, NG)
            stB(ctxs[ci], g)
        if 1 <= st_ <= n:
            ci, g = divmod(st_ - 1, NG)
            stC2(ctxs[ci], g)
        if st_ >= 2:
            ci, g = divmod(st_ - 2, NG)
            stE(ctxs[ci], g)
            if g == NG - 1:
                epilogue(ctxs[ci])
                del ctxs[ci]


def ssd_phase_d(S, k, b, layer, src_fn, dst_fn):
    wo = S.sb("wo", [128, 16, 1024], BF16)
    for q in range(4):
        S.dma(wo[:, q * 4:(q + 1) * 4, :],
              View(k.ssd_out_w.t[0].rearrange("(kk p) n -> p kk n", p=128)[:, q * 4:(q + 1) * 4, :], k.ssd_out_w.res), eng="pool")
    g1 = {}
    for kind in (0, 1):
        g1[kind] = S.sb("g1", [128, 1024], F32)
        load_mod(S, k, layer, 2 if kind == 0 else b, 2, g1[kind].all())
    gnrot = S.rot("gnD", [128, 2048], BF16, 2)
    gTrot = S.rot("gT", [128, 16, 128], BF16, 2)
    xrrot = S.rot("xr", [128, 1024], F32, 2)
    xorot = S.rot("xo", [128, 1024], F32, 2)
    ppB = S.pool([0, 1, 2, 3])
    ppC = S.pool([4, 5, 6, 7])
    for i in range(NT):
        kind = 0 if i < 2 else 1
        gn = gnrot.next()
        S.dma(gn.all(), View(k.gn_d.t[i], k.gn_d.res))
        gT = gTrot.next()
        for hf in range(2):
            pt = ppB.next()
            pv = pbf(pt)
            for j in range(8):
                c = hf * 8 + j
                S.tr(pv[:, j * 128:(j + 1) * 128], gn[:, c * 128:(c + 1) * 128], k.identb.all())
            S.copy(gT[:, hf * 8:(hf + 1) * 8, :], pv.rearrange("p (j t) -> p j t", j=8), eng="act")
        xr = xrrot.next()
        S.dma(xr.all(), src_fn(i))
        xo = xorot.next()
        for hf in range(2):
            po = ppC.next()
            for c in range(16):
                S.mm(po.all(), gT[:, c, :], wo[:, c, hf * 512:(hf + 1) * 512], start=(c == 0), stop=(c == 15))
            S.tt(xo[:, hf * 512:(hf + 1) * 512], po.all(), g1[kind][:, hf * 512:(hf + 1) * 512], ALU.mult)
        S.tt(xo.all(), xo.all(), xr.all(), ALU.add)
        S.dma(dst_fn(i), xo.all())

NE = 16
U32 = mybir.dt.uint32


def moe_layer(S, k, layer, xs_fn):
    has_ctx = (layer == 0)
    ngrp = 2 if has_ctx else 1
    GNT = [16, 2]
    GCAP = [256, 32]
    GTI0 = [2, 0]
    NSLOT = 512 + (64 if has_ctx else 0)
    with ExitStack() as st_outer:
        S.stack = st_outer
        slT = [[S.sb(f"slT{g}{b}", [16, GNT[g] * 128], BF16) for b in range(NB)] for g in range(ngrp)]
        with ExitStack() as st_mid:
            S.stack = st_mid
            hn = [[S.sb(f"hn{g}{b}", [128, GNT[g], 1024], BF16) for b in range(NB)] for g in range(ngrp)]
            sl = [S.sb(f"sl{g}", [128, NB, GNT[g], NE], F32) for g in range(ngrp)]
            affhl = [S.sb(f"affhl{g}", [128, NB, GNT[g], NE, 2], BF16) for g in range(ngrp)]
            with ExitStack() as st:
                S.stack = st
                moe_route(S, k, layer, xs_fn, ngrp, GNT, GCAP, GTI0, hn, sl, affhl, slT)
                S.phase_end()
            with ExitStack() as st:
                S.stack = st
                moe_experts(S, k, layer, ngrp, GNT, hn, sl, affhl, NSLOT)
                S.phase_end()
        with ExitStack() as st:
            S.stack = st
            moe_scatter(S, k, layer, xs_fn, ngrp, GNT, GTI0, slT)
            S.phase_end()
    S.stack = None


def moe_route(S, k, layer, xs_fn, ngrp, GNT, GCAP, GTI0, hn, sl, affhl, slT):
    rw = S.sb("rw", [128, 8, NE], F32)
    S.dma(rw.all(), View(k.moe_router_w.t[layer].rearrange("(kk p) e -> p kk e", p=128), k.moe_router_w.res))
    aff = [S.sb(f"aff{g}", [128, NB, GNT[g], NE], F32) for g in range(ngrp)]
    xrot = S.rot("mx", [128, 1024], F32, 3)
    trot = S.rot("mtmp", [128, 1024], F32, 2)
    hfrot = S.rot("mhf", [128, 1024], F32, 2)
    hTrot = S.rot("mhfT", [128, 8, 128], F32, 2)
    erot = S.rot("mexp", [128, NE], F32, 3)
    ppA = S.pool([0, 1, 2, 3])
    ppB = S.pool([4, 5])
    ppC = S.pool([6, 7])
    for g in range(ngrp):
        for b in range(NB):
            A = S.sb("m2A", [128, 1024], F32)
            sh = S.sb("m2S", [128, 1024], F32)
            row = 2 if g == 1 else b
            load_mod(S, k, layer, row, 4, A.all())
            load_mod(S, k, layer, row, 3, sh.all())
            for j in range(GNT[g]):
                ti = GTI0[g] + j
                xt = xrot.next()
                S.dma(xt.all(), xs_fn(b)(ti))
                tmp = trot.next()
                rs = k.small.next()
                rms_rstd(S, k, rs[:, 2:3], xt.all(), 1024, tmp.all())
                S.stt(tmp.all(), xt.all(), rs[:, 2:3], A.all(), ALU.mult, ALU.mult)
                hf = hfrot.next()
                S.tt(hf.all(), tmp.all(), sh.all(), ALU.add)
                S.copy(hn[g][b][:, j, :], hf.all(), eng="act")
                hfT = hTrot.next()
                for q in range(2):
                    ps = ppA.next()
                    for c in range(4):
                        kk = q * 4 + c
                        S.tr(ps[:, c * 128:(c + 1) * 128], hf[:, kk * 128:(kk + 1) * 128], k.identf.all())
                    S.copy(hfT[:, q * 4:(q + 1) * 4, :], ps.all().rearrange("p (c t) -> p c t", c=4), eng="act")
                pl = ppB.next()
                for kk in range(8):
                    S.mm(pl[:, 0:NE], hfT[:, kk, :], rw[:, kk, :], start=(kk == 0), stop=(kk == 7))
                sm = k.small.next()
                S.reduce(sm[:, 0:1], pl[:, 0:NE], ALU.max)
                S.ts(sm[:, 1:2], sm[:, 0:1], -1.0, ALU.mult)
                ex = erot.next()
                S.act(ex.all(), pl[:, 0:NE], AF.Exp, bias=sm[:, 1:2], accum_out=sm[:, 2:3])
                S.recip(sm[:, 3:4], sm[:, 2:3])
                S.ts(aff[g][:, b, j, :], ex.all(), sm[:, 3:4], ALU.mult)
    lo = [S.sb(f"lo{g}", [128, NB, NE], F32) for g in range(ngrp)]
    hi = [S.sb(f"hi{g}", [128, NB, NE], F32) for g in range(ngrp)]
    mid = [S.sb(f"mid{g}", [128, NB, NE], F32) for g in range(ngrp)]
    cnt = [S.sb(f"cnt{g}", [128, NB, NE], F32) for g in range(ngrp)]
    selp = [S.sb(f"selp{g}", [128, NB, NE], F32) for g in range(ngrp)]
    seln = [S.sb(f"seln{g}", [128, NB, NE], F32) for g in range(ngrp)]
    ge = [S.sb(f"ge{g}", [128, NB, GNT[g], NE], BF16) for g in range(ngrp)]
    for g in range(ngrp):
        S.memset(lo[g].all(), 0.0)
        S.memset(hi[g].all(), 1.5)
    for it in range(31):
        for g in range(ngrp):
            nt = GNT[g]
            n = NB * nt * NE
            S.tt(mid[g].all(), lo[g].all(), hi[g].all(), ALU.add)
            S.ts(mid[g].all(), mid[g].all(), 0.5, ALU.mult)
            S.tt(ge[g].all(), aff[g].all(), mid[g].all().bc(2, nt), ALU.is_ge)
            pc = ppC.next()
            S.mm(pc[:, 0:n], k.ones_b.all(), ge[g].all().rearrange("p s t e -> p (s t e)"))
            S.reduce(cnt[g].all(), pc[:, 0:n].rearrange("p (s t e) -> p s e t", s=NB, t=nt), ALU.add)
            S.ts(selp[g].all(), cnt[g].all(), float(GCAP[g]), ALU.is_ge)
            S.ts(seln[g].all(), cnt[g].all(), float(GCAP[g]), ALU.is_lt)
            S.emit("dve", lambda e, g=g: e.copy_predicated(lo[g].t[:], selp[g].t[:].bitcast(U32), mid[g].t[:]),
                   reads=[selp[g].all(), mid[g].all()], writes=[lo[g].all()])
            S.emit("dve", lambda e, g=g: e.copy_predicated(hi[g].t[:], seln[g].t[:].bitcast(U32), mid[g].t[:]),
                   reads=[seln[g].all(), mid[g].all()], writes=[hi[g].all()])
    for g in range(ngrp):
        nt = GNT[g]
        n = NB * nt * NE
        cap = GCAP[g]
        S.tt(ge[g].all(), aff[g].all(), lo[g].all().bc(2, nt), ALU.is_ge)
        gef = ge[g].all().rearrange("p s t e -> p (s t e)")
        pr = ppC.next()
        S.mm(pr[:, 0:n], k.SUt.all(), gef)
        ptot = ppC.next()
        S.mm(ptot[:, 0:n], k.ones_b.all(), gef)
        tot = S.sb(f"tot{g}", [128, NB, nt, NE], F32)
        S.copy(tot.all(), ptot[:, 0:n].rearrange("p (s t e) -> p s t e", s=NB, t=nt))
        off = S.sb(f"off{g}", [128, NB, nt, NE], F32)
        S.memset(off[:, :, 0, :], 0.0)
        for j in range(1, nt):
            S.tt(off[:, :, j, :], off[:, :, j - 1, :], tot[:, :, j - 1, :], ALU.add)
        rank = S.sb(f"rank{g}", [128, NB, nt, NE], F32)
        S.tt(rank.all(), pr[:, 0:n].rearrange("p (s t e) -> p s t e", s=NB, t=nt), off.all(), ALU.add)
        ok = S.sb(f"ok{g}", [128, NB, nt, NE], F32)
        S.ts(ok.all(), rank.all(), float(cap), ALU.is_lt)
        S.tt(ok.all(), ok.all(), ge[g].all(), ALU.mult)
        for b in range(NB):
            offs = 1.0 + (32.0 * b if g == 1 else 0.0)
            S.stt(sl[g][:, b], rank[:, b], offs, ok[:, b], ALU.add, ALU.mult)
        S.ts(sl[g].all(), sl[g].all(), -1.0, ALU.add)
        for b in range(NB):
            for j0 in range(0, nt, 4):
                m = min(4, nt - j0)
                ps = ppA.next()
                for c in range(m):
                    S.tr(ps[0:NE, c * 128:(c + 1) * 128], sl[g][:, b, j0 + c, :], k.identf.all())
                S.copy(slT[g][b][:, j0 * 128:(j0 + m) * 128], ps[0:NE, 0:m * 128])
        S.copy(affhl[g][:, :, :, :, 0], aff[g].all())
        S.tt(affhl[g][:, :, :, :, 1], aff[g].all(), affhl[g][:, :, :, :, 0], ALU.subtract)


def moe_experts(S, k, layer, ngrp, GNT, hn, sl, affhl, NSLOT):
    has_ctx = ngrp == 2
    wrot = S.rot("mw", [128, 8, 1024], BF16, 4)
    xgT = S.sb("xgT", [128, 8, NSLOT], BF16)
    hT = S.sb("mhT", [128, 8, NSLOT], BF16)
    serot = S.rot("Se", [128, 16, 256], BF16, 2)
    secrot = S.rot("SeC", [128, NB, 2, 64], BF16, 2)
    sarot = S.rot("msa", [128, 512], F32, 2)
    yrot = S.rot("my", [128, 1024], BF16, 2)
    gates = S.rot("mgate", [128, 8], F32, 2)
    ppA = S.pool([0, 1, 2])
    ppB = S.pool([3, 4, 5])
    ppG = S.pool([6, 7])
    nchunks = [(0, 512)] + ([(512, 64)] if has_ctx else [])
    jchunks = [(0, 128), (128, 128), (256, 128), (384, 128)] + ([(512, 64)] if has_ctx else [])
    wsrc = {1: k.moe_w1, 3: k.moe_w3, 2: k.moe_w2}

    def loadw(which, e):
        w = wrot.next()
        src = wsrc[which]
        for q in range(2):
            S.dma(w[:, q * 4:(q + 1) * 4, :],
                  View(src.t[layer, e].rearrange("(kk p) n -> p kk n", p=128)[:, q * 4:(q + 1) * 4, :], src.res), eng="pool")
        return w

    for e in range(NE):
        w1 = loadw(1, e)
        w3 = loadw(3, e)
        gt = gates.next()
        pgt = ppG.next()
        for b in range(NB):
            Se = serot.next()
            for tt_ in range(16):
                S.ts(Se[:, tt_, :], k.iota[:, 0:256], sl[0][:, b, tt_, e:e + 1], ALU.is_equal)
            for kk0 in range(0, 8, 2):
                pg = ppA.next()
                for c in range(2):
                    kk = kk0 + c
                    for tt_ in range(16):
                        S.mm(pg[:, c * 256:(c + 1) * 256], hn[0][b][:, tt_, kk * 128:(kk + 1) * 128], Se[:, tt_, :],
                             start=(tt_ == 0), stop=(tt_ == 15))
                S.copy(xgT[:, kk0:kk0 + 2, b * 256:(b + 1) * 256], pg.all().rearrange("p (c j) -> p c j", c=2), eng="act")
            for jc in range(2):
                col = (b * 2 + jc) * 2
                for tt_ in range(16):
                    S.mm(pgt[:, col:col + 2], Se[:, tt_, jc * 128:(jc + 1) * 128], affhl[0][:, b, tt_, e, :],
                         start=(tt_ == 0), stop=(tt_ == 15))
        if has_ctx:
            SeC = secrot.next()
            for b in range(NB):
                for tt_ in range(2):
                    S.ts(SeC[:, b, tt_, :], k.iota[:, 0:64], sl[1][:, b, tt_, e:e + 1], ALU.is_equal)
            for kk0 in range(0, 8, 4):
                pg = ppA.next()
                for c in range(4):
                    kk = kk0 + c
                    n = 0
                    for b in range(NB):
                        for tt_ in range(2):
                            S.mm(pg[:, c * 64:(c + 1) * 64], hn[1][b][:, tt_, kk * 128:(kk + 1) * 128], SeC[:, b, tt_, :],
                                 start=(n == 0), stop=(n == 3))
                            n += 1
                S.copy(xgT[:, kk0:kk0 + 4, 512:576], pg[:, 0:256].rearrange("p (c j) -> p c j", c=4), eng="act")
            n = 0
            for b in range(NB):
                for tt_ in range(2):
                    S.mm(pgt[0:64, 8:10], SeC[:, b, tt_, :], affhl[1][:, b, tt_, e, :], start=(n == 0), stop=(n == 3))
                    n += 1
        ncol = 5 if has_ctx else 4
        S.reduce(gt[:, 0:ncol], pgt[:, 0:2 * ncol].rearrange("p (c two) -> p c two", two=2), ALU.add)
        for fc in range(8):
            for (n0, n) in nchunks:
                pa = ppA.next()
                for kk in range(8):
                    S.mm(pa[:, 0:n], w1[:, kk, fc * 128:(fc + 1) * 128], xgT[:, kk, n0:n0 + n], start=(kk == 0), stop=(kk == 7))
                pb = ppB.next()
                for kk in range(8):
                    S.mm(pb[:, 0:n], w3[:, kk, fc * 128:(fc + 1) * 128], xgT[:, kk, n0:n0 + n], start=(kk == 0), stop=(kk == 7))
                sa = sarot.next()
                S.act(sa[:, 0:n], pa[:, 0:n], AF.Silu)
                S.tt(hT[:, fc, n0:n0 + n], sa[:, 0:n], pb[:, 0:n], ALU.mult)
        w2 = loadw(2, e)
        for ji, (j0, m) in enumerate(jchunks):
            y = yrot.next()
            for hf in range(2):
                py = ppB.next()
                for fc in range(8):
                    S.mm(py[0:m, :], hT[:, fc, j0:j0 + m], w2[:, fc, hf * 512:(hf + 1) * 512], start=(fc == 0), stop=(fc == 7))
                S.act(y[0:m, hf * 512:(hf + 1) * 512], py[0:m, :], AF.Copy, scale=gt[0:m, ji:ji + 1])
            S.dma(View(k.y_d.t[e, j0:j0 + m, :], k.y_d.res), y[0:m, :])


def moe_scatter(S, k, layer, xs_fn, ngrp, GNT, GTI0, slT):
    sel16 = S.sb("sel16", [16, NE, 128], BF16)
    selio = S.sb("selio", [16, NE, 128], F32)
    S.emit("pool", lambda e: e.iota(selio.t[:], [[1, NE], [0, 128]], base=0, channel_multiplier=0,
                                    allow_small_or_imprecise_dtypes=True), writes=[selio.all()])
    S.ts(sel16.all(), selio.all(), k.pid[0:16, :], ALU.is_equal)
    pid32 = S.sb("pid32", [128, 1], F32)
    S.ts(pid32.all(), k.pid.all(), 32.0, ALU.add)
    yall = S.sb("yall", [128, 2 * NE, 1024], BF16)
    strot = S.rot("ST", [128, 2, NE, 128], BF16, 2)
    xrot = S.rot("sx", [128, 1024], F32, 2)
    xorot = S.rot("sxo", [128, 1024], F32, 2)
    ppA = S.pool([0, 1, 2, 3])
    ppB = S.pool([4, 5, 6, 7])
    for g in range(ngrp):
        for b in range(NB):
            g2 = S.sb("g2v", [128, 1024], F32)
            load_mod(S, k, layer, 2 if g == 1 else b, 5, g2.all())
            if g == 0:
                kr = 128
                njc = 2
                for e in range(NE):
                    S.dma(yall[:, 2 * e:2 * e + 2, :],
                          View(k.y_d.t[e, b * 256:(b + 1) * 256, :].rearrange("(jc j) d -> j jc d", jc=2), k.y_d.res))
                cmpv = [k.pid, k.pid128]
            else:
                kr = 32
                njc = 1
                S.dma(yall[0:32, 0:NE, :],
                      View(k.y_d.t[:, 512 + b * 32:512 + (b + 1) * 32, :].rearrange("e j d -> j e d"), k.y_d.res))
                cmpv = [k.pid if b == 0 else pid32]
            def make_ST(j):
                banks = [ppA.next() for _ in range(4)]
                for e in range(NE):
                    S.mm(banks[e // 4][:, (e % 4) * 128:(e % 4 + 1) * 128], sel16[:, e, :], slT[g][b][:, j * 128:(j + 1) * 128])
                ST_ = strot.next()
                for jc in range(njc):
                    for q in range(4):
                        S.ts(ST_[:, jc, q * 4:(q + 1) * 4, :], banks[q].all().rearrange("p (e t) -> p e t", e=4),
                             cmpv[jc].all(), ALU.is_equal)
                return ST_

            ST_next = make_ST(0)
            for j in range(GNT[g]):
                ti = GTI0[g] + j
                ST = ST_next
                if j + 1 < GNT[g]:
                    ST_next = make_ST(j + 1)
                xt = xrot.next()
                S.dma(xt.all(), xs_fn(b)(ti))
                xo = xorot.next()
                for hf in range(2):
                    po = ppB.next()
                    n = 0
                    tot = NE * njc
                    for e in range(NE):
                        for jc in range(njc):
                            yi = 2 * e + jc if g == 0 else e
                            S.mm(po.all(), ST[0:kr, jc, e, :], yall[0:kr, yi, hf * 512:(hf + 1) * 512],
                                 start=(n == 0), stop=(n == tot - 1))
                            n += 1
                    S.tt(xo[:, hf * 512:(hf + 1) * 512], po.all(), g2[:, hf * 512:(hf + 1) * 512], ALU.mult)
                S.tt(xo.all(), xo.all(), xt.all(), ALU.add)
                S.dma(xs_fn(b)(ti), xo.all())

def attn_mixer(S, k, b, layer, xs):
    NLT = NT - 2
    with ExitStack() as st_outer:
        S.stack = st_outer
        qT = S.sb("qT", [128, 8, LAT], BF16)
        kTd = S.sb("kTd", [128, 4, TOK], BF16)
        vaug = S.sb("vaug", [128, NT, 4, 72], BF16)
        with ExitStack() as st_h:
            S.stack = st_h
            hT = S.sb("ahT", [128, 8, TOK], BF16)
            with ExitStack() as st:
                S.stack = st
                norm_T(S, k, b, layer, 1, xs, hT, [(i, i * 128) for i in range(NT)], S.pool([0, 1, 2, 3]))
                S.phase_end()
            import os
            if os.environ.get("ATT_STOP") != "norm":
                with ExitStack() as st:
                    S.stack = st
                    attn_qkv(S, k, hT, qT, kTd, vaug)
                    S.phase_end()
        import os
        if os.environ.get("ATT_STOP") not in ("qkv", "norm"):
            with ExitStack() as st:
                S.stack = st
                attn_core(S, k, b, layer, xs, qT, kTd, vaug)
                S.phase_end()
    S.stack = None


def attn_qkv(S, k, hT, qT, kTd, vaug):
    w = S.sb("wqkv", [128, 8, 1536], BF16)
    for q in range(3):
        S.dma(w[:, :, q * 512:(q + 1) * 512],
              View(k.attn_qkv_w.t[0].rearrange("(kk p) n -> p kk n", p=128)[:, :, q * 512:(q + 1) * 512], k.attn_qkv_w.res), eng="pool")
    qw = S.sb("qwbc", [128, 64], F32)
    kw = S.sb("kwbc", [128, 64], F32)
    load_bc(S, qw.all(), k.attn_q_norm_w, k.attn_q_norm_w.t[0])
    load_bc(S, kw.all(), k.attn_k_norm_w, k.attn_k_norm_w.t[0])
    S.memset(vaug.all(), 1.0)
    qkrot = S.rot("qk", [128, 20, 64], F32, 2)
    sqrot = S.rot("qsq", [128, 20, 64], F32, 1)
    qnrot = S.rot("qn", [128, 20, 64], F32, 2)
    qrrot = S.rot("qr", [128, 20, 64], BF16, 2)
    kdrot = S.rot("kd", [128, 4, 2, 64], BF16, 2)
    csrot = S.rot("cs", [128, 2, 32], F32, 2)
    r1rot = S.rot("rp1", [128, 20, 32], F32, 2)
    r2rot = S.rot("rp2", [128, 20, 32], F32, 2)
    ssrot = S.rot("qss", [128, 20], F32, 3)
    ppA = S.pool([0, 1, 2, 3, 4, 5])
    ppB = S.pool([6, 7])
    import os
    LVL = int(os.environ.get("QKV_LVL", "9"))
    if LVL < 1:
        return
    for i in range(NT):
        banks = []
        for nb_ in range(3):
            ps = ppA.next()
            for kk in range(8):
                S.mm(ps.all(), hT[:, kk, i * 128:(i + 1) * 128], w[:, kk, nb_ * 512:(nb_ + 1) * 512], start=(kk == 0), stop=(kk == 7))
            banks.append(ps)
        VAR = os.environ.get("QKV_VAR", "Z")
        if VAR == "A":
            continue
        qk = qkrot.next()
        S.copy(qk[:, 0:8, :], banks[0].all().rearrange("p (h d) -> p h d", h=8), eng="act")
        S.copy(qk[:, 8:16, :], banks[1].all().rearrange("p (h d) -> p h d", h=8), eng="act")
        S.copy(qk[:, 16:20, :], banks[2][:, 0:256].rearrange("p (h d) -> p h d", h=4), eng="act")
        if VAR == "B":
            continue
        S.copy(vaug[:, i, :, 0:64], banks[2][:, 256:512].rearrange("p (h d) -> p h d", h=4), eng="act")
        if LVL < 2:
            continue
        sq = sqrot.next()
        S.tt(sq.all(), qk.all(), qk.all(), ALU.mult)
        ss = ssrot.next()
        S.reduce(ss.all(), sq.all(), ALU.add)
        S.act(ss.all(), ss.all(), AF.Ln, bias=k.eps.all(), scale=1.0 / 64)
        S.act(ss.all(), ss.all(), AF.Exp, scale=-0.5)
        qn = qnrot.next()
        S.tt(qn.all(), qk.all(), ss.all().bc(2, 64), ALU.mult)
        S.tt(qn[:, 0:16, :], qn[:, 0:16, :], qw.all().bc(1, 16), ALU.mult)
        S.tt(qn[:, 16:20, :], qn[:, 16:20, :], kw.all().bc(1, 4), ALU.mult)
        if LVL < 3:
            continue
        qr = qrrot.next()
        if i >= 2:
            cs = csrot.next()
            t0 = (i - 2) * 128
            S.dma(cs[:, 0, :], View(k.cos.t[t0:t0 + 128, :], k.cos.res))
            S.dma(cs[:, 1, :], View(k.sin.t[t0:t0 + 128, :], k.sin.res))
            cosb = cs[:, 0, :].bc(1, 20)
            sinb = cs[:, 1, :].bc(1, 20)
            t1 = qn[:, :, 0:32]
            t2 = qn[:, :, 32:64]
            a1 = r1rot.next()
            a2 = r2rot.next()
            S.tt(a1.all(), t1, cosb, ALU.mult)
            S.tt(a2.all(), t2, sinb, ALU.mult)
            S.tt(qr[:, :, 0:32], a1.all(), a2.all(), ALU.subtract)
            a3 = r1rot.next()
            a4 = r2rot.next()
            S.tt(a3.all(), t2, cosb, ALU.mult)
            S.tt(a4.all(), t1, sinb, ALU.mult)
            S.tt(qr[:, :, 32:64], a3.all(), a4.all(), ALU.add)
        else:
            S.copy(qr.all(), qn.all())
        if LVL < 4:
            continue
        if i >= 2:
            ps = ppB.next()
            pv = pbf(ps)
            for pr in range(8):
                S.tr(pv[:, pr * 128:(pr + 1) * 128], qr[:, 2 * pr:2 * pr + 2, :].rearrange("p h d -> p (h d)"), k.identb.all())
            S.copy(qT[:, :, (i - 2) * 128:(i - 1) * 128], pv.rearrange("p (c t) -> p c t", c=8), eng="act")
        if LVL < 5:
            continue
        kd = kdrot.next()
        S.copy(kd[:, :, 0, :], qr[:, 16:20, :])
        S.copy(kd[:, :, 1, :], qr[:, 16:20, :], eng="act")
        ps = ppB.next()
        pv = pbf(ps)
        for kv in range(4):
            S.tr(pv[:, kv * 128:(kv + 1) * 128], kd[:, kv].rearrange("p a d -> p (a d)"), k.identb.all())
        S.copy(kTd[:, :, i * 128:(i + 1) * 128], pv[:, 0:512].rearrange("p (c t) -> p c t", c=4))


def attn_core(S, k, b, layer, xs, qT, kTd, vaug):
    wo = S.sb("awo", [128, 8, 1024], BF16)
    for q in range(2):
        S.dma(wo[:, q * 4:(q + 1) * 4, :],
              View(k.attn_out_w.t[0].rearrange("(kk p) n -> p kk n", p=128)[:, q * 4:(q + 1) * 4, :], k.attn_out_w.res), eng="pool")
    g1 = S.sb("ag1", [128, 1024], F32)
    load_mod(S, k, layer, b, 2, g1.all())
    ptrot = S.rot("PT", [128, 512], BF16, 3)
    otrot = S.rot("otm", [128, 4, 1024], BF16, 2)
    oTrot = S.rot("aoT", [128, 8, 128], BF16, 2)
    xrot = S.rot("ax", [128, 1024], F32, 2)
    xorot = S.rot("axo", [128, 1024], F32, 2)
    ppS = S.pool([0, 1, 2, 3])
    ppAcc = S.pool([4, 5])
    ppO = S.pool([6, 7])
    for tqb in range(4):
        tq0 = tqb * 512
        otm = otrot.next()
        steps = [(h, tk) for h in range(16) for tk in range(NT)]

        def emit_scores(h, tk):
            pair, e = h // 2, h % 2
            kv = h // 4
            p0, p1 = e * 64, (e + 1) * 64
            psc = ppS.next()
            S.mm(psc.all(), kTd[p0:p1, kv, tk * 128:(tk + 1) * 128], qT[p0:p1, pair, tq0:tq0 + 512])
            return psc

        pending = emit_scores(*steps[0])
        acc = None
        for si, (h, tk) in enumerate(steps):
            kv = h // 4
            psc = pending
            if si + 1 < len(steps):
                pending = emit_scores(*steps[si + 1])
            if tk == 0:
                acc = ppAcc.next()
            PT = ptrot.next()
            S.act(PT.all(), psc.all(), AF.Exp, scale=0.125)
            for r in range(4):
                S.mm(acc[:, r * 128:r * 128 + 65], PT[:, r * 128:(r + 1) * 128], vaug[:, tk, kv, 0:65],
                     start=(tk == 0 and r == 0), stop=(tk == NT - 1 and r == 3))
            if tk == NT - 1:
                rc = k.small.next()
                a3 = acc.all().rearrange("p (r c) -> p r c", r=4)
                S.recip(rc[:, 0:4], a3[:, :, 64])
                S.tt(otm[:, :, h * 64:(h + 1) * 64], a3[:, :, 0:64], rc[:, 0:4].bc(2, 64), ALU.mult)
        for r in range(4):
            ti = 2 + tqb * 4 + r
            ps = ppO.next()
            pv = pbf(ps)
            for kk in range(8):
                S.tr(pv[:, kk * 128:(kk + 1) * 128], otm[:, r, kk * 128:(kk + 1) * 128], k.identb.all())
            oT = oTrot.next()
            S.copy(oT.all(), pv.rearrange("p (c t) -> p c t", c=8), eng="act")
            xt = xrot.next()
            S.dma(xt.all(), xs(ti))
            xo = xorot.next()
            for hf in range(2):
                po = ppO.next()
                for kk in range(8):
                    S.mm(po.all(), oT[:, kk, :], wo[:, kk, hf * 512:(hf + 1) * 512], start=(kk == 0), stop=(kk == 7))
                S.tt(xo[:, hf * 512:(hf + 1) * 512], po.all(), g1[:, hf * 512:(hf + 1) * 512], ALU.mult)
            S.tt(xo.all(), xo.all(), xt.all(), ALU.add)
            S.dma(xs(ti), xo.all())


def final_norm(S, k, xs_fn):
    with ExitStack() as st:
        S.stack = st
        fw = S.sb("fnw", [128, 1024], F32)
        load_bc(S, fw.all(), k.final_norm_w, k.final_norm_w.t)
        xrot = S.rot("fx", [128, 1024], F32, 3)
        jrot = S.rot("fj", [128, 1024], F32, 2)
        orot = S.rot("fo", [128, 1024], F32, 3)
        for b in range(NB):
            for j in range(NT - 2):
                xt = xrot.next()
                S.dma(xt.all(), xs_fn(b)(2 + j))
                rs = k.small.next()
                rms_rstd(S, k, rs[:, 2:3], xt.all(), 1024, jrot.next().all())
                xo = orot.next()
                S.stt(xo.all(), xt.all(), rs[:, 2:3], fw.all(), ALU.mult, ALU.mult)
                S.dma(View(k.out.t[b, j * 128:(j + 1) * 128, :], k.out.res), xo.all())
        S.phase_end()
    S.stack = None


WEIGHT_SPECS = [
    ("ada_w", [2, 1024, 6144]), ("ada_b", [2, 6144]), ("norm1_w", [2, 1024]), ("norm2_w", [2, 1024]),
    ("ssd_in_w", [1, 1024, 6208]), ("ssd_conv_w", [1, 5, 4096]), ("ssd_conv_b", [1, 4096]),
    ("ssd_dt_bias", [1, 2, 32]), ("ssd_A_log", [1, 2, 32]), ("ssd_D", [1, 32]), ("ssd_norm_w", [1, 2048]),
    ("ssd_out_w", [1, 2048, 1024]), ("attn_qkv_w", [1, 1024, 1536]), ("attn_q_norm_w", [1, 64]),
    ("attn_k_norm_w", [1, 64]), ("attn_out_w", [1, 1024, 1024]), ("moe_router_w", [2, 1024, 16]),
    ("moe_w1", [2, 16, 1024, 1024]), ("moe_w3", [2, 16, 1024, 1024]), ("moe_w2", [2, 16, 1024, 1024]),
    ("final_norm_w", [1024]),
]


def build_program(stop=None, dbg=False, only=None):
    nc = bass.Bass("TRN2", target_bir_lowering=False)
    k = K()
    with ExitStack() as st:
        S = Sched(nc, st)
        S.init_psum(8)

        def din(name, shape):
            t = nc.dram_tensor(name, list(shape), F32, kind="ExternalInput")
            return Tl(t.ap(), 1, name)

        k.x = din("x", [NB, LAT, D])
        k.c = din("c", [NB, D])
        k.ctx = din("ctx", [NB, CTXL, D])
        k.c_ctx = din("c_ctx", [D])
        for name, shape in WEIGHT_SPECS:
            setattr(k, name, din(name, shape))
        k.cos = din("rope_cos", [LAT, 32])
        k.sin = din("rope_sin", [LAT, 32])
        k.out = Tl(nc.dram_tensor("out", [NB, LAT, D], F32, kind="ExternalOutput").ap(), 1, "out")
        xkind = "ExternalOutput"
        k.xs = [Tl(nc.dram_tensor(f"xs{b}", [NT, 128, D], F32, kind=xkind).ap(), NT, f"xs{b}") for b in range(NB)]
        k.modv = [S.dram(f"modv{l}", [3, 6144], F32) for l in range(2)]
        k.xtm_d = S.dram("xtm_d", [NT, 128, 3072], BF16)
        k.bc_d = S.dram("bc_d", [16, 128, TOK], BF16)
        k.yf_d = S.dram("yf_d", [NT, 128, 2048], F32)
        k.gn_d = S.dram("gn_d", [NT, 128, 2048], BF16)
        k.y_d = S.dram("y_d", [16, 576, 1024], BF16)
        k.small = S.rot("small", [128, 4], F32, 12)
        setup_consts(S, k)
        S.phase_end()

        def in_src(b):
            def f(i):
                if i < 2:
                    return View(k.ctx.t[b, i * 128:(i + 1) * 128, :], k.ctx.res)
                return View(k.x.t[b, (i - 2) * 128:(i - 1) * 128, :], k.x.res)
            return f

        def xs_fn(b):
            def f(i):
                return View(k.xs[b].t[i], [k.xs[b].res[i]])
            return f

        stages = []
        stages.append(("mod0", lambda: phase_mod(S, k, 0)))
        for b in range(NB):
            stages.append((f"ssd{b}", lambda b=b: ssd_mixer(S, k, b, 0, in_src(b), xs_fn(b))))
        stages.append(("moe0", lambda: moe_layer(S, k, 0, xs_fn)))
        stages.append(("mod1", lambda: phase_mod(S, k, 1)))
        for b in range(NB):
            stages.append((f"attn{b}", lambda b=b: attn_mixer(S, k, b, 1, xs_fn(b))))
        stages.append(("moe1", lambda: moe_layer(S, k, 1, xs_fn)))
        stages.append(("final", lambda: final_norm(S, k, xs_fn)))
        if only is not None:
            with ExitStack() as st2:
                S.stack = st2
                rot = S.rot("initx", [128, D], F32, 3)
                for b in range(NB):
                    xi = din(f"xs_init{b}", [NT, 128, D])
                    for i in range(NT):
                        t = rot.next()
                        S.dma(t.all(), View(xi.t[i], xi.res))
                        S.dma(xs_fn(b)(i), t.all())
                S.phase_end()
        for name, fn in stages:
            if only is not None and name not in only:
                continue
            S.stack = st
            fn()
            if stop == name:
                break
        S.stack = st
        outs = [k.out.all()] + [x.all() for x in k.xs]
        S.finish(outs)
        k.ninst = S.ninst
    return nc, k


_ROPE = None


def rope_tables():
    global _ROPE
    if _ROPE is None:
        S_ = LAT
        row = np.repeat(np.arange(S_ // 64), 64).astype(np.float32)
        col = np.tile(np.arange(64), S_ // 64).astype(np.float32)
        inv = (np.float32(10000.0) ** (-np.arange(0, 32, 2, dtype=np.float32) / np.float32(32))).astype(np.float32)
        ang = np.concatenate([row[:, None] * inv, col[:, None] * inv], axis=-1).astype(np.float32)
        _ROPE = (np.cos(ang).astype(np.float32), np.sin(ang).astype(np.float32))
    return _ROPE


_PROG = {}


def kernel(**inputs):
    n_cores = 8
    if "full" not in _PROG:
        _PROG["full"] = build_program()[0]
    nc = _PROG["full"]
    cos, sin = rope_tables()
    shared = {name: np.ascontiguousarray(inputs[name], dtype=np.float32) for name, _ in WEIGHT_SPECS}
    shared["c_ctx"] = np.ascontiguousarray(inputs["c_ctx"], dtype=np.float32)
    shared["rope_cos"] = cos
    shared["rope_sin"] = sin
    in_maps = []
    for cid in range(n_cores):
        m = dict(shared)
        sl = slice(cid * NB, (cid + 1) * NB)
        m["x"] = np.ascontiguousarray(inputs["x"][sl], dtype=np.float32)
        m["c"] = np.ascontiguousarray(inputs["c"][sl], dtype=np.float32)
        m["ctx"] = np.ascontiguousarray(inputs["ctx"][sl], dtype=np.float32)
        in_maps.append(m)
    res = run_bass_kernel_spmd(nc, in_maps, core_ids=list(range(n_cores)))
    return np.concatenate([np.asarray(r["out"]) for r in res.results], axis=0).astype(np.float32)
```

```python
from contextlib import ExitStack
import math
import numpy as np
import concourse.bass as bass
import concourse.mybir as mybir
from concourse.bass_utils import run_bass_kernel_spmd

F32 = mybir.dt.float32
BF16 = mybir.dt.bfloat16
AF = mybir.ActivationFunctionType
ALU = mybir.AluOpType
AX = mybir.AxisListType

ENGS = ["pe", "dve", "act", "pool", "sp"]
NLANES = 28
NLANES_HW = 16


class Res:
    __slots__ = ("w", "r", "name")

    def __init__(self, name=""):
        self.w = None
        self.r = []
        self.name = name


class View:
    __slots__ = ("ap", "res")

    def __init__(self, ap, res):
        self.ap = ap
        self.res = res

    def __getitem__(self, idx):
        return View(self.ap[idx], self.res)

    def rearrange(self, *a, **k):
        return View(self.ap.rearrange(*a, **k), self.res)

    def bitcast(self, dt):
        return View(self.ap.bitcast(dt), self.res)

    def bc(self, axis, n):
        a = self.ap.unsqueeze(axis)
        shp = list(a.shape)
        shp[axis] = n
        return View(a.to_broadcast(shp), self.res)


class Tl:
    def __init__(self, t, nslots=1, name=""):
        self.t = t
        self.nslots = nslots
        self.res = [Res(f"{name}[{i}]") for i in range(nslots)]
        self.name = name

    def __getitem__(self, idx):
        return View(self.t[idx], self.res)

    def all(self):
        return View(self.t[:], self.res)

    def s(self, i, j=None):
        if j is None:
            return View(self.t[:, i], [self.res[i]])
        return View(self.t[:, i:j], self.res[i:j])


class Rot:
    def __init__(self, tiles):
        self.tiles = tiles
        self.i = 0

    def next(self):
        t = self.tiles[self.i % len(self.tiles)]
        self.i += 1
        return t


class Sched:
    def __init__(self, nc, stack):
        self.nc = nc
        self.stack = stack
        self.q = {e: [] for e in ENGS}
        self.sem = {e: stack.enter_context(nc.semaphore(f"s_{e}")) for e in ENGS}
        self.cnt = {e: 0 for e in ENGS}
        self.lane_sem = [stack.enter_context(nc.semaphore(f"l_{i}")) for i in range(NLANES)]
        self.lane_cnt = [0] * NLANES
        self.lane_next = 0
        self.lane_next_sw = 0
        self.seen = {e: {} for e in ENGS}
        self.ninst = 0
        self.psum_banks = []
        self.uid = 0

    def sb(self, name, shape, dtype, nslots=1):
        self.uid += 1
        nm = f"{name}_{self.uid}"
        t = self.stack.enter_context(self.nc.sbuf_tensor(nm, list(shape), dtype))
        return Tl(t, nslots, nm)

    def rot(self, name, shape, dtype, n):
        return Rot([self.sb(f"{name}{i}", shape, dtype) for i in range(n)])

    def dram(self, name, shape, dtype, kind="Internal", nslots=1):
        t = self.nc.dram_tensor(name, list(shape), dtype, kind=kind)
        return Tl(t.ap(), nslots, name)

    def init_psum(self, n=8):
        for i in range(n):
            t = self.stack.enter_context(self.nc.psum_tensor(f"ps{i}", [128, 512], F32))
            self.psum_banks.append(Tl(t, 1, f"ps{i}"))

    def pool(self, idx):
        return Rot([self.psum_banks[i] for i in idx])

    def _collect(self, eng, reads, writes):
        waits = {}

        def need(tok):
            if tok is None:
                return
            kind, key, val = tok
            if kind == "e" and key == eng and eng == "pe":
                return
            k = (kind, key)
            if waits.get(k, 0) < val:
                waits[k] = val

        for v in reads:
            for r in v.res:
                need(r.w)
        for v in writes:
            for r in v.res:
                need(r.w)
                for tok in r.r:
                    need(tok)
        out = []
        seen = self.seen[eng]
        for k, val in waits.items():
            if seen.get(k, 0) >= val:
                continue
            seen[k] = val
            out.append((k, val))
        return out

    def _mark(self, tok, reads, writes):
        for v in writes:
            for r in v.res:
                r.w = tok
                r.r = []
        for v in reads:
            for r in v.res:
                r.r = [t for t in r.r if (t[0], t[1]) != (tok[0], tok[1])]
                r.r.append(tok)

    def _semof(self, k):
        kind, key = k
        return self.sem[key] if kind == "e" else self.lane_sem[key]

    def emit(self, eng, fn, reads=(), writes=()):
        reads = list(reads)
        writes = list(writes)
        waits = self._collect(eng, reads, writes)
        self.cnt[eng] += 1
        val = self.cnt[eng]
        sem = self.sem[eng]
        wl = [(self._semof(k), v) for k, v in waits]

        def run(e, wl=wl, fn=fn, sem=sem):
            for s, v in wl:
                e.wait_ge(s, v)
            fn(e).then_inc(sem, 1)

        self.q[eng].append(run)
        self._mark(("e", eng, val), reads, writes)
        self.ninst += 1

    def dma(self, out, in_, eng="sp", **kw):
        if eng == "pool":
            lane = NLANES_HW + self.lane_next_sw % (NLANES - NLANES_HW)
            self.lane_next_sw += 1
        else:
            lane = self.lane_next % NLANES_HW
            self.lane_next += 1
        reads = [in_]
        writes = [out]
        waits = self._collect(eng, reads, writes)
        prev = self.lane_cnt[lane]
        k = ("d", lane)
        if prev > 0 and self.seen[eng].get(k, 0) < prev:
            self.seen[eng][k] = prev
            waits = [w for w in waits if w[0] != k] + [(k, prev)]
        self.lane_cnt[lane] += 16
        val = self.lane_cnt[lane]
        sem = self.lane_sem[lane]
        wl = [(self._semof(k2), v) for k2, v in waits]
        oap, iap = out.ap, in_.ap

        def run(e, wl=wl, sem=sem, oap=oap, iap=iap, kw=kw):
            for s, v in wl:
                e.wait_ge(s, v)
            e.dma_start(out=oap, in_=iap, **kw).then_inc(sem, 16)

        self.q[eng].append(run)
        self._mark(("d", lane, val), reads, writes)
        self.ninst += 1

    def wait_all(self, eng, views):
        waits = self._collect(eng, list(views), [])
        wl = [(self._semof(k), v) for k, v in waits]

        def run(e, wl=wl):
            for s, v in wl:
                e.wait_ge(s, v)

        self.q[eng].append(run)

    def barrier(self):
        snap_e = dict(self.cnt)
        snap_l = list(self.lane_cnt)
        for eng in ENGS:
            wl = []
            seen = self.seen[eng]
            for e2, v in snap_e.items():
                if v > 0 and seen.get(("e", e2), 0) < v and not (e2 == eng and eng == "pe"):
                    seen[("e", e2)] = v
                    wl.append((self.sem[e2], v))
            for i, v in enumerate(snap_l):
                if v > 0 and seen.get(("d", i), 0) < v:
                    seen[("d", i)] = v
                    wl.append((self.lane_sem[i], v))

            def run(e, wl=wl):
                for s, v in wl:
                    e.wait_ge(s, v)

            self.q[eng].append(run)

    def flush(self):
        nc = self.nc
        q = self.q
        self.q = {e: [] for e in ENGS}
        with nc.Block() as block:
            @block.sync
            def _(e):
                for f in q["sp"]:
                    f(e)

            @block.tensor
            def _(e):
                for f in q["pe"]:
                    f(e)

            @block.vector
            def _(e):
                for f in q["dve"]:
                    f(e)

            @block.scalar
            def _(e):
                for f in q["act"]:
                    f(e)

            @block.gpsimd
            def _(e):
                for f in q["pool"]:
                    f(e)

    def phase_end(self):
        self.barrier()
        self.flush()

    def finish(self, final_views):
        self.wait_all("sp", final_views)
        self.flush()

    def mm(self, out, lhsT, rhs, start=True, stop=True):
        self.emit("pe", lambda e: e.matmul(out.ap, lhsT.ap, rhs.ap, start=start, stop=stop),
                  reads=[lhsT, rhs], writes=[out])

    def tr(self, out, in_, ident):
        self.emit("pe", lambda e: e.transpose(out.ap, in_.ap, ident.ap), reads=[in_, ident], writes=[out])

    def act(self, out, in_, func, bias=None, scale=None, accum_out=None):
        reads = [in_]
        writes = [out]
        kw = {}
        if bias is not None:
            if isinstance(bias, View):
                reads.append(bias)
                kw["bias"] = bias.ap
            else:
                kw["bias"] = bias
        if scale is not None:
            if isinstance(scale, View):
                reads.append(scale)
                kw["scale"] = scale.ap
            else:
                kw["scale"] = scale
        if accum_out is not None:
            writes.append(accum_out)
            kw["accum_out"] = accum_out.ap
        self.emit("act", lambda e: e.activation(out.ap, in_.ap, func, **kw), reads=reads, writes=writes)

    def tt(self, out, a, b, op, eng="dve"):
        self.emit(eng, lambda e: e.tensor_tensor(out=out.ap, in0=a.ap, in1=b.ap, op=op), reads=[a, b], writes=[out])

    def ts(self, out, a, s1, op0, s2=None, op1=None, accum_out=None, eng="dve"):
        reads = [a]
        writes = [out]
        s1v = s1.ap if isinstance(s1, View) else s1
        s2v = s2.ap if isinstance(s2, View) else s2
        if isinstance(s1, View):
            reads.append(s1)
        if isinstance(s2, View):
            reads.append(s2)
        kw = {}
        if op1 is not None:
            kw["op1"] = op1
        if accum_out is not None:
            kw["accum_out"] = accum_out.ap
            writes.append(accum_out)
        self.emit(eng, lambda e: e.tensor_scalar(out=out.ap, in0=a.ap, scalar1=s1v, scalar2=s2v, op0=op0, **kw),
                  reads=reads, writes=writes)

    def stt(self, out, a, s, b, op0, op1, eng="dve"):
        reads = [a, b]
        sv = s.ap if isinstance(s, View) else s
        if isinstance(s, View):
            reads.append(s)
        self.emit(eng, lambda e: e.scalar_tensor_tensor(out=out.ap, in0=a.ap, scalar=sv, in1=b.ap, op0=op0, op1=op1),
                  reads=reads, writes=[out])

    def copy(self, out, in_, eng="dve"):
        if eng == "act":
            self.emit("act", lambda e: e.activation(out.ap, in_.ap, AF.Copy), reads=[in_], writes=[out])
        else:
            self.emit(eng, lambda e: e.tensor_copy(out=out.ap, in_=in_.ap), reads=[in_], writes=[out])

    def memset(self, out, val, eng="dve"):
        self.emit(eng, lambda e: e.memset(out.ap, val), reads=[], writes=[out])

    def reduce(self, out, in_, op, axis=None):
        axis = axis or AX.X
        self.emit("dve", lambda e: e.tensor_reduce(out=out.ap, in_=in_.ap, axis=axis, op=op), reads=[in_], writes=[out])

    def recip(self, out, in_):
        self.emit("dve", lambda e: e.reciprocal(out=out.ap, in_=in_.ap), reads=[in_], writes=[out])


def pbf(bank):
    return View(bank.t[:].bitcast(BF16), bank.res)

D = 1024
LAT = 2048
CTXL = 256
NT = 18
TOK = NT * 128
DI = 2048
NH = 32
NG = 8
EPS = 1e-6
NB = 2


class K:
    pass


def setup_consts(S, k):
    nc = S.nc
    k.iota = S.sb("iota", [128, 256], F32)
    k.pid = S.sb("pid", [128, 1], F32)
    k.pid128 = S.sb("pid128", [128, 1], F32)
    k.eps = S.sb("epsc", [128, 1], F32)
    S.emit("pool", lambda e: e.iota(k.iota.t[:], [[1, 256]], base=0, channel_multiplier=0,
                                    allow_small_or_imprecise_dtypes=True), writes=[k.iota.all()])
    S.emit("pool", lambda e: e.iota(k.pid.t[:], [[0, 1]], base=0, channel_multiplier=1,
                                    allow_small_or_imprecise_dtypes=True), writes=[k.pid.all()])
    S.ts(k.pid128.all(), k.pid.all(), 128.0, ALU.add)
    S.memset(k.eps.all(), EPS)
    io = k.iota[:, 0:128]

    def mk(name, dt, op):
        t = S.sb(name, [128, 128], dt)
        S.ts(t.all(), io, k.pid.all(), op)
        return t

    k.identb = mk("identb", BF16, ALU.is_equal)
    k.identf = mk("identf", F32, ALU.is_equal)
    k.U_le = mk("U_le", BF16, ALU.is_ge)
    k.L_ge = mk("L_ge", BF16, ALU.is_le)
    k.SLt = mk("SLt", BF16, ALU.is_lt)
    k.SUt = mk("SUt", BF16, ALU.is_gt)
    k.U_le_f = mk("U_le_f", F32, ALU.is_ge)
    k.L_ge_f = mk("L_ge_f", F32, ALU.is_le)
    k.ones_f = S.sb("ones_f", [128, 128], F32)
    S.memset(k.ones_f.all(), 1.0)
    k.ones_b = S.sb("ones_b", [128, 128], BF16)
    S.memset(k.ones_b.all(), 1.0)


def load_bc(S, dst, dram_tl, ap):
    S.dma(dst, View(ap.partition_broadcast(dst.ap.shape[0]), dram_tl.res))


def rms_rstd(S, k, rs, src, n, junk):
    ss = k.small.next()
    S.act(junk, src, AF.Square, accum_out=ss[:, 0:1])
    S.act(ss[:, 1:2], ss[:, 0:1], AF.Sqrt, bias=k.eps.all(), scale=1.0 / n)
    S.recip(rs, ss[:, 1:2])


def phase_mod(S, k, layer):
    with ExitStack() as st:
        S.stack = st
        cin = S.sb("cin", [128, 3, 8], F32)
        for b in range(NB):
            S.dma(cin[:, b, :], View(k.c.t[b].rearrange("(p kk) -> p kk", kk=8), k.c.res))
        S.dma(cin[:, 2, :], View(k.c_ctx.t.rearrange("(p kk) -> p kk", kk=8), k.c_ctx.res))
        sc = S.sb("silc", [128, 8, 3], BF16)
        S.act(sc.all().rearrange("p kk v -> p v kk"), cin.all(), AF.Silu)
        bias = S.sb("adab", [3, 6144], F32)
        load_bc(S, bias.all(), k.ada_b, k.ada_b.t[layer])
        nw = S.sb("nw12", [3, 2, 1024], F32)
        load_bc(S, nw[:, 0, :], k.norm1_w, k.norm1_w.t[layer])
        load_bc(S, nw[:, 1, :], k.norm2_w, k.norm2_w.t[layer])
        res = S.sb("modres", [3, 6144], F32)
        wrot = S.rot("adaw", [128, 8, 1024], BF16, 2)
        pp = S.pool([0, 1, 2, 3])
        for blk in range(6):
            w = wrot.next()
            S.dma(w.all(), View(k.ada_w.t[layer].rearrange("(p kk) n -> p kk n", kk=8)[:, :, blk * 1024:(blk + 1) * 1024],
                                k.ada_w.res), eng="pool")
            for half in range(2):
                ps = pp.next()
                for kk in range(8):
                    S.mm(ps[0:3, :], sc[:, kk, :], w[:, kk, half * 512:(half + 1) * 512], start=(kk == 0), stop=(kk == 7))
                c0 = blk * 1024 + half * 512
                S.tt(res[:, c0:c0 + 512], ps[0:3, :], bias[:, c0:c0 + 512], ALU.add)
        for j, blk in enumerate((1, 4)):
            S.stt(res[:, blk * 1024:(blk + 1) * 1024], res[:, blk * 1024:(blk + 1) * 1024], 1.0, nw[:, j, :], ALU.add, ALU.mult)
        S.dma(k.modv[layer].all(), res.all())
        S.phase_end()


def load_mod(S, k, layer, row, blk, dst):
    load_bc(S, dst, k.modv[layer], k.modv[layer].t[row, blk * 1024:(blk + 1) * 1024])


def norm_T(S, k, b, layer, which, src_fn, hT, tiles, pp):
    blkA, blkS = (1, 0) if which == 1 else (4, 3)
    mods = {}
    for kind in (0, 1):
        if not any((ti < 2) == (kind == 0) for ti, _ in tiles):
            continue
        A = S.sb("modA", [128, 1024], F32)
        sh = S.sb("modS", [128, 1024], F32)
        row = 2 if kind == 0 else b
        load_mod(S, k, layer, row, blkA, A.all())
        load_mod(S, k, layer, row, blkS, sh.all())
        mods[kind] = (A, sh)
    xrot = S.rot("nx", [128, 1024], F32, 3)
    trot = S.rot("ntmp", [128, 1024], F32, 2)
    hrot = S.rot("nhb", [128, 1024], BF16, 2)
    for ti, col in tiles:
        A, sh = mods[0 if ti < 2 else 1]
        xt = xrot.next()
        S.dma(xt.all(), src_fn(ti))
        tmp = trot.next()
        rs = k.small.next()
        rms_rstd(S, k, rs[:, 2:3], xt.all(), 1024, tmp.all())
        S.stt(tmp.all(), xt.all(), rs[:, 2:3], A.all(), ALU.mult, ALU.mult)
        hb = hrot.next()
        S.tt(hb.all(), tmp.all(), sh.all(), ALU.add)
        ps = pp.next()
        pv = pbf(ps)
        for kk in range(8):
            S.tr(pv[:, kk * 128:(kk + 1) * 128], hb[:, kk * 128:(kk + 1) * 128], k.identb.all())
        S.copy(hT[:, :, col:col + 128], pv.rearrange("p (kk t) -> p kk t", kk=8), eng="act")

def ssd_mixer(S, k, b, layer, src_fn, dst_fn):
    with ExitStack() as st_outer:
        S.stack = st_outer
        hT = S.sb("hT", [128, 8, TOK], BF16)
        with ExitStack() as st:
            S.stack = st
            norm_T(S, k, b, layer, 1, src_fn, hT, [(i, i * 128) for i in range(NT)], S.pool([0, 1, 2, 3]))
            S.phase_end()
        with ExitStack() as st:
            S.stack = st
            ssd_phase_a(S, k, hT)
            S.phase_end()
        for d in (0, 1):
            with ExitStack() as st:
                S.stack = st
                ssd_scan(S, k, b, layer, hT, d, src_fn, dst_fn)
                S.phase_end()
        S.stack = st_outer
    with ExitStack() as st:
        S.stack = st
        ssd_phase_d(S, k, b, layer, src_fn, dst_fn)
        S.phase_end()
    S.stack = None


def ssd_phase_a(S, k, hT):
    stg = S.sb("cstg", [6, 4096], F32)
    S.dma(stg[0:5, :], k.ssd_conv_w[0])
    S.dma(stg[5:6, :], View(k.ssd_conv_b.t[0:1, :], k.ssd_conv_b.res))
    cw = S.sb("cw", [128, 32, 8], F32)
    pp = S.pool([0, 1, 2, 3])
    for cc0 in range(0, 32, 8):
        ps = pp.next()
        for j in range(8):
            cc = cc0 + j
            S.tr(ps[:, j * 8:j * 8 + 6], stg[0:6, cc * 128:(cc + 1) * 128], k.identf[0:6, 0:6])
        S.copy(cw[:, cc0:cc0 + 8, :], ps[:, 0:64].rearrange("p (j e) -> p j e", e=8))
    wrot = S.rot("wA", [128, 8, 512], BF16, 2)
    urot = S.rot("uA", [128, TOK], F32, 2)
    arot = S.rot("accA", [128, TOK], F32, 2)
    crot = S.rot("cvA", [128, TOK], BF16, 3)
    trot = S.rot("tmA", [128, 8, 128], BF16, 3)
    pp2 = S.pool([4, 5, 6, 7])
    blocks = [(0, 256), (256, 512), (768, 512), (1280, 512), (1792, 512)]
    segs = [(0, 256), (256, TOK)]
    w = None
    prev = None

    def tail(cc, cv):
        if cc >= 16:
            S.dma(View(k.bc_d.t[cc - 16], k.bc_d.res), cv.all())
        if cc < 24:
            for i0 in range(0, NT, 8):
                n = min(8, NT - i0)
                ps = pp2.next()
                pv = pbf(ps)
                for j in range(n):
                    S.tr(pv[:, j * 128:(j + 1) * 128], cv[:, (i0 + j) * 128:(i0 + j + 1) * 128], k.identb.all())
                tm = trot.next()
                S.copy(tm[:, 0:n, :], pv[:, 0:n * 128].rearrange("p (j c) -> p j c", j=n))
                S.dma(View(k.xtm_d.t[i0:i0 + n, :, cc * 128:(cc + 1) * 128].rearrange("i t c -> t i c"), k.xtm_d.res),
                      tm[:, 0:n, :])

    for cc in range(32):
        if cc % 4 == 0:
            w = wrot.next()
            c0 = 2048 + cc * 128
            S.dma(w.all(), View(k.ssd_in_w.t[0].rearrange("(kk p) n -> p kk n", p=128)[:, :, c0:c0 + 512], k.ssd_in_w.res),
                  eng="pool")
        u = urot.next()
        for (t0, n) in blocks:
            ps = pp.next()
            for kk in range(8):
                S.mm(ps[:, 0:n], w[:, kk, (cc % 4) * 128:(cc % 4 + 1) * 128], hT[:, kk, t0:t0 + n],
                     start=(kk == 0), stop=(kk == 7))
            S.copy(u[:, t0:t0 + n], ps[:, 0:n], eng="act")
        acc = arot.next()
        for (a, e) in segs:
            S.ts(acc[:, a:e], u[:, a:e], cw[:, cc, 2:3], ALU.mult, s2=cw[:, cc, 5:6], op1=ALU.add)
            for tap in (0, 1, 3, 4):
                dl = tap - 2
                lo = a + max(0, -dl)
                hi = e - max(0, dl)
                S.stt(acc[:, lo:hi], u[:, lo + dl:hi + dl], cw[:, cc, tap:tap + 1], acc[:, lo:hi], ALU.mult, ALU.add)
        cv = crot.next()
        S.act(cv.all(), acc.all(), AF.Silu)
        if prev is not None:
            tail(*prev)
        prev = (cc, cv)
    tail(*prev)


def ssd_scan(S, k, b, layer, hT, d, src_fn, dst_fn):
    fwd = (d == 0)
    dc = d * 32
    wdt = S.sb("wdt", [128, 8, 64], BF16)
    S.dma(wdt.all(), View(k.ssd_in_w.t[0].rearrange("(kk p) n -> p kk n", p=128)[:, :, 6144:6208], k.ssd_in_w.res), eng="pool")
    dtb = S.sb("dtb", [128, 64], F32)
    load_bc(S, dtb.all(), k.ssd_dt_bias, k.ssd_dt_bias.t[0].rearrange("a h -> (a h)"))
    Abc = S.sb("Abc", [128, 64], F32)
    load_bc(S, Abc.all(), k.ssd_A_log, k.ssd_A_log.t[0].rearrange("a h -> (a h)"))
    S.act(Abc.all(), Abc.all(), AF.Exp)
    S.ts(Abc.all(), Abc.all(), -1.0, ALU.mult)
    h = S.sb("hst", [128, NG, 256], F32, nslots=NG)
    hb = S.sb("hstb", [128, NG, 256], BF16, nslots=NG)
    S.memset(h.all(), 0.0)
    S.memset(hb.all(), 0.0)
    if not fwd:
        wz = S.sb("wz", [128, 8, 2048], BF16)
        for q in range(4):
            S.dma(wz[:, :, q * 512:(q + 1) * 512],
                  View(k.ssd_in_w.t[0].rearrange("(kk p) n -> p kk n", p=128)[:, :, q * 512:(q + 1) * 512], k.ssd_in_w.res), eng="pool")
        Dbc = S.sb("Dbc", [128, 32], F32)
        load_bc(S, Dbc.all(), k.ssd_D, k.ssd_D.t[0])
        nwbc = S.sb("snw", [128, 2048], F32)
        load_bc(S, nwbc.all(), k.ssd_norm_w, k.ssd_norm_w.t[0])
        yfrot = S.rot("yf", [128, 2048], F32, 1)
        szrot = S.rot("sz", [128, 2048], F32, 1)
        gnrot = S.rot("gn", [128, 2048], BF16, 1)
    xbrot = S.rot("xB", [128, 3072], BF16, 2)
    bcrot = S.rot("bct", [128, 16, 128], BF16, 2)
    ydrot = S.rot("ydir", [128, 2048], F32, 2)
    xdtrot = S.rot("xdt", [128, 2048], BF16, 2)
    xddrot = S.rot("xdd", [128, 2048], BF16, 2)
    smrot = S.rot("ssm", [128, 8, 32], F32, 2)
    lharot = S.rot("lha", [128, 32, 128], BF16, 2)
    cbrot = S.rot("cbm", [128, 128], BF16, 4)
    sgrot = S.rot("seg", [128, 512], BF16, 4)
    mtrot = S.rot("MT", [128, 4, 128], BF16, 4)
    t3rot = S.rot("t3", [128, 256], F32, 6)
    ppA = S.pool([0, 1, 2, 3] if fwd else [0, 1, 2])
    ppB = S.pool([3])
    ppY = S.pool([4, 5, 6, 7])
    U = k.U_le if fwd else k.L_ge
    Uf = k.U_le_f if fwd else k.L_ge_f
    LM = k.SLt if fwd else k.SUt
    order = list(range(NT)) if fwd else [1, 0] + list(range(NT - 1, 1, -1))

    def prologue(i):
        c = {}
        c["i"] = i
        xB = c["xB"] = xbrot.next()
        S.dma(xB.all(), View(k.xtm_d.t[i], k.xtm_d.res))
        bct = c["bct"] = bcrot.next()
        S.dma(bct.all(), View(k.bc_d.t[:, :, i * 128:(i + 1) * 128].rearrange("c n t -> n c t"), k.bc_d.res))
        sm = c["sm"] = smrot.next()
        ps = ppA.next()
        for kk in range(8):
            S.mm(ps[:, 0:64], hT[:, kk, i * 128:(i + 1) * 128], wdt[:, kk, :], start=(kk == 0), stop=(kk == 7))
        S.tt(sm[:, 0, :], ps[:, dc:dc + 32], dtb[:, dc:dc + 32], ALU.add)
        S.act(sm[:, 1, :], sm[:, 0, :], AF.Exp)
        S.act(sm[:, 2, :], sm[:, 1, :], AF.Ln, bias=1.0)
        S.tt(sm[:, 3, :], sm[:, 2, :], Abc[:, dc:dc + 32], ALU.mult)
        lha = c["lha"] = lharot.next()
        S.tt(lha.all(), LM.all().bc(1, 32), sm[:, 3, :].bc(2, 128), ALU.mult)
        ps2 = ppA.next()
        S.mm(ps2[:, 0:32], Uf.all(), sm[:, 3, :])
        S.mm(ps2[:, 32:64], k.ones_f.all(), sm[:, 3, :])
        S.copy(sm[:, 4:6, :], ps2[:, 0:64].rearrange("p (a h) -> p a h", a=2))
        S.act(sm[:, 6, :], sm[:, 4, :], AF.Exp)
        S.tt(sm[:, 0, :], sm[:, 5, :], sm[:, 4, :], ALU.subtract)
        S.act(sm[:, 7, :], sm[:, 0, :], AF.Exp)
        S.act(sm[:, 1, :], sm[:, 5, :], AF.Exp)
        S.tt(sm[:, 0, :], sm[:, 2, :], sm[:, 7, :], ALU.mult)
        xdt = c["xdt"] = xdtrot.next()
        xdd = c["xdd"] = xddrot.next()
        x3 = c["x3"] = xB[:, 0:2048].rearrange("p (h j) -> p h j", h=32)
        S.tt(xdt.all().rearrange("p (h j) -> p h j", h=32), x3, sm[:, 2, :].bc(2, 64), ALU.mult)
        S.tt(xdd.all().rearrange("p (h j) -> p h j", h=32), x3, sm[:, 0, :].bc(2, 64), ALU.mult)
        c["ydir"] = ydrot.next()
        return c

    def stA(c, g):
        bct = c["bct"]
        BT = bct[:, g, :]
        CT = bct[:, 8 + g, :]
        pc = ppA.next()
        S.mm(pc[:, 0:128], BT, CT)
        pD = ppA.next()
        for r in range(4):
            S.mm(pD[:, r * 128:(r + 1) * 128], c["lha"][:, 4 * g + r, :], U.all())
        c[("a", g)] = (pc, pD, CT)

    def stB(c, g):
        pc, pD, CT = c[("a", g)]
        cbm = cbrot.next()
        S.tt(cbm.all(), pc[:, 0:128], U.all(), ALU.mult)
        seg = sgrot.next()
        S.act(seg.all(), pD.all(), AF.Exp)
        c[("f", g)] = (cbm, seg, CT)

    def stC1(c, g):
        cbm, seg, CT = c[("f", g)]
        MT = mtrot.next()
        S.tt(MT.all(), seg.all().rearrange("p (r l) -> p r l", r=4), cbm.all().bc(1, 4), ALU.mult)
        c[("m", g)] = MT

    def stC2(c, g):
        cbm, seg, CT = c[("f", g)]
        MT = c[("m", g)]
        xB, xdt, xdd = c["xB"], c["xdt"], c["xdd"]
        py = ppY.next()
        for r in range(4):
            hh = 4 * g + r
            S.mm(py[:, r * 64:(r + 1) * 64], MT[:, r, :], xdt[:, hh * 64:(hh + 1) * 64])
        S.mm(py[:, 256:512], CT, hb.s(g))
        pst = ppY.next()
        S.mm(pst[:, 0:256], xB[:, 2048 + g * 128:2048 + (g + 1) * 128], xdd[:, g * 256:(g + 1) * 256])
        c[("p", g)] = (py, pst)

    def stE(c, g):
        py, pst = c[("p", g)]
        sm, ydir = c["sm"], c["ydir"]
        t3 = t3rot.next()
        S.tt(t3.all().rearrange("p (r j) -> p r j", r=4), py[:, 256:512].rearrange("p (r j) -> p r j", r=4),
             sm[:, 6, 4 * g:4 * g + 4].bc(2, 64), ALU.mult)
        S.tt(ydir[:, g * 256:(g + 1) * 256], t3.all(), py[:, 0:256], ALU.add)
        t4 = t3rot.next()
        S.tt(t4.all().rearrange("p (r j) -> p r j", r=4), h.s(g).rearrange("p (r j) -> p r j", r=4),
             sm[:, 1, 4 * g:4 * g + 4].bc(2, 64), ALU.mult)
        S.tt(h.s(g), t4.all(), pst[:, 0:256], ALU.add)
        S.copy(hb.s(g), h.s(g), eng="act")

    def epilogue(c):
        i = c["i"]
        ydir = c["ydir"]
        if fwd:
            S.dma(View(k.yf_d.t[i], k.yf_d.res), ydir.all())
            return
        yf = yfrot.next()
        S.dma(yf.all(), View(k.yf_d.t[i], k.yf_d.res))
        S.tt(ydir.all(), ydir.all(), yf.all(), ALU.add)
        S.tt(yf.all().rearrange("p (h j) -> p h j", h=32), c["x3"], Dbc.all().bc(2, 64), ALU.mult)
        S.tt(ydir.all(), ydir.all(), yf.all(), ALU.add)
        sz = szrot.next()
        for q in range(4):
            pz = ppB.next()
            for kk in range(8):
                S.mm(pz.all(), hT[:, kk, i * 128:(i + 1) * 128], wz[:, kk, q * 512:(q + 1) * 512],
                     start=(kk == 0), stop=(kk == 7))
            S.act(sz[:, q * 512:(q + 1) * 512], pz.all(), AF.Silu)
        S.tt(ydir.all(), ydir.all(), sz.all(), ALU.mult)
        rs = k.small.next()
        rms_rstd(S, k, rs[:, 2:3], ydir.all(), 2048, sz.all())
        gn = gnrot.next()
        S.stt(gn.all(), ydir.all(), rs[:, 2:3], nwbc.all(), ALU.mult, ALU.mult)
        S.dma(View(k.gn_d.t[i], k.gn_d.res), gn.all())

    ctxs = {0: prologue(order[0])}
    n = len(order) * NG
    for st_ in range(n + 2):
        if st_ < n:
            ci, g = divmod(st_, NG)
            if g == 3 and ci + 1 < len(order):
                ctxs[ci + 1] = prologue(order[ci + 1])
            stA(ctxs[ci], g)
        if 1 <= st_ <= n:
            ci, g = divmod(st_ - 1, NG)
            stC1(ctxs[ci], g)
        if st_ < n:
            ci, g = divmod(st_, NG)
            stB(ctxs[ci], g)
        if 1 <= st_ <= n:
            ci, g = divmod(st_ - 1, NG)
            stC2(ctxs[ci], g)
        if st_ >= 2:
            ci, g = divmod(st_ - 2, NG)
            stE(ctxs[ci], g)
            if g == NG - 1:
                epilogue(ctxs[ci])
                del ctxs[ci]


def ssd_phase_d(S, k, b, layer, src_fn, dst_fn):
    wo = S.sb("wo", [128, 16, 1024], BF16)
    for q in range(4):
        S.dma(wo[:, q * 4:(q + 1) * 4, :],
              View(k.ssd_out_w.t[0].rearrange("(kk p) n -> p kk n", p=128)[:, q * 4:(q + 1) * 4, :], k.ssd_out_w.res), eng="pool")
    g1 = {}
    for kind in (0, 1):
        g1[kind] = S.sb("g1", [128, 1024], F32)
        load_mod(S, k, layer, 2 if kind == 0 else b, 2, g1[kind].all())
    gnrot = S.rot("gnD", [128, 2048], BF16, 2)
    gTrot = S.rot("gT", [128, 16, 128], BF16, 2)
    xrrot = S.rot("xr", [128, 1024], F32, 2)
    xorot = S.rot("xo", [128, 1024], F32, 2)
    ppB = S.pool([0, 1, 2, 3])
    ppC = S.pool([4, 5, 6, 7])
    for i in range(NT):
        kind = 0 if i < 2 else 1
        gn = gnrot.next()
        S.dma(gn.all(), View(k.gn_d.t[i], k.gn_d.res))
        gT = gTrot.next()
        for hf in range(2):
            pt = ppB.next()
            pv = pbf(pt)
            for j in range(8):
                c = hf * 8 + j
                S.tr(pv[:, j * 128:(j + 1) * 128], gn[:, c * 128:(c + 1) * 128], k.identb.all())
            S.copy(gT[:, hf * 8:(hf + 1) * 8, :], pv.rearrange("p (j t) -> p j t", j=8), eng="act")
        xr = xrrot.next()
        S.dma(xr.all(), src_fn(i))
        xo = xorot.next()
        for hf in range(2):
            po = ppC.next()
            for c in range(16):
                S.mm(po.all(), gT[:, c, :], wo[:, c, hf * 512:(hf + 1) * 512], start=(c == 0), stop=(c == 15))
            S.tt(xo[:, hf * 512:(hf + 1) * 512], po.all(), g1[kind][:, hf * 512:(hf + 1) * 512], ALU.mult)
        S.tt(xo.all(), xo.all(), xr.all(), ALU.add)
        S.dma(dst_fn(i), xo.all())

NE = 16
U32 = mybir.dt.uint32


def moe_layer(S, k, layer, xs_fn):
    has_ctx = (layer == 0)
    ngrp = 2 if has_ctx else 1
    GNT = [16, 2]
    GCAP = [256, 32]
    GTI0 = [2, 0]
    NSLOT = 512 + (64 if has_ctx else 0)
    with ExitStack() as st_outer:
        S.stack = st_outer
        slT = [[S.sb(f"slT{g}{b}", [16, GNT[g] * 128], BF16) for b in range(NB)] for g in range(ngrp)]
        with ExitStack() as st_mid:
            S.stack = st_mid
            hn = [[S.sb(f"hn{g}{b}", [128, GNT[g], 1024], BF16) for b in range(NB)] for g in range(ngrp)]
            sl = [S.sb(f"sl{g}", [128, NB, GNT[g], NE], F32) for g in range(ngrp)]
            affhl = [S.sb(f"affhl{g}", [128, NB, GNT[g], NE, 2], BF16) for g in range(ngrp)]
            with ExitStack() as st:
                S.stack = st
                moe_route(S, k, layer, xs_fn, ngrp, GNT, GCAP, GTI0, hn, sl, affhl, slT)
                S.phase_end()
            with ExitStack() as st:
                S.stack = st
                moe_experts(S, k, layer, ngrp, GNT, hn, sl, affhl, NSLOT)
                S.phase_end()
        with ExitStack() as st:
            S.stack = st
            moe_scatter(S, k, layer, xs_fn, ngrp, GNT, GTI0, slT)
            S.phase_end()
    S.stack = None


def moe_route(S, k, layer, xs_fn, ngrp, GNT, GCAP, GTI0, hn, sl, affhl, slT):
    rw = S.sb("rw", [128, 8, NE], F32)
    S.dma(rw.all(), View(k.moe_router_w.t[layer].rearrange("(kk p) e -> p kk e", p=128), k.moe_router_w.res))
    aff = [S.sb(f"aff{g}", [128, NB, GNT[g], NE], F32) for g in range(ngrp)]
    xrot = S.rot("mx", [128, 1024], F32, 3)
    trot = S.rot("mtmp", [128, 1024], F32, 2)
    hfrot = S.rot("mhf", [128, 1024], F32, 2)
    hTrot = S.rot("mhfT", [128, 8, 128], F32, 2)
    erot = S.rot("mexp", [128, NE], F32, 3)
    ppA = S.pool([0, 1, 2, 3])
    ppB = S.pool([4, 5])
    ppC = S.pool([6, 7])
    for g in range(ngrp):
        for b in range(NB):
            A = S.sb("m2A", [128, 1024], F32)
            sh = S.sb("m2S", [128, 1024], F32)
            row = 2 if g == 1 else b
            load_mod(S, k, layer, row, 4, A.all())
            load_mod(S, k, layer, row, 3, sh.all())
            for j in range(GNT[g]):
                ti = GTI0[g] + j
                xt = xrot.next()
                S.dma(xt.all(), xs_fn(b)(ti))
                tmp = trot.next()
                rs = k.small.next()
                rms_rstd(S, k, rs[:, 2:3], xt.all(), 1024, tmp.all())
                S.stt(tmp.all(), xt.all(), rs[:, 2:3], A.all(), ALU.mult, ALU.mult)
                hf = hfrot.next()
                S.tt(hf.all(), tmp.all(), sh.all(), ALU.add)
                S.copy(hn[g][b][:, j, :], hf.all(), eng="act")
                hfT = hTrot.next()
                for q in range(2):
                    ps = ppA.next()
                    for c in range(4):
                        kk = q * 4 + c
                        S.tr(ps[:, c * 128:(c + 1) * 128], hf[:, kk * 128:(kk + 1) * 128], k.identf.all())
                    S.copy(hfT[:, q * 4:(q + 1) * 4, :], ps.all().rearrange("p (c t) -> p c t", c=4), eng="act")
                pl = ppB.next()
                for kk in range(8):
                    S.mm(pl[:, 0:NE], hfT[:, kk, :], rw[:, kk, :], start=(kk == 0), stop=(kk == 7))
                sm = k.small.next()
                S.reduce(sm[:, 0:1], pl[:, 0:NE], ALU.max)
                S.ts(sm[:, 1:2], sm[:, 0:1], -1.0, ALU.mult)
                ex = erot.next()
                S.act(ex.all(), pl[:, 0:NE], AF.Exp, bias=sm[:, 1:2], accum_out=sm[:, 2:3])
                S.recip(sm[:, 3:4], sm[:, 2:3])
                S.ts(aff[g][:, b, j, :], ex.all(), sm[:, 3:4], ALU.mult)
    lo = [S.sb(f"lo{g}", [128, NB, NE], F32) for g in range(ngrp)]
    hi = [S.sb(f"hi{g}", [128, NB, NE], F32) for g in range(ngrp)]
    mid = [S.sb(f"mid{g}", [128, NB, NE], F32) for g in range(ngrp)]
    cnt = [S.sb(f"cnt{g}", [128, NB, NE], F32) for g in range(ngrp)]
    selp = [S.sb(f"selp{g}", [128, NB, NE], F32) for g in range(ngrp)]
    seln = [S.sb(f"seln{g}", [128, NB, NE], F32) for g in range(ngrp)]
    ge = [S.sb(f"ge{g}", [128, NB, GNT[g], NE], BF16) for g in range(ngrp)]
    for g in range(ngrp):
        S.memset(lo[g].all(), 0.0)
        S.memset(hi[g].all(), 1.5)
    for it in range(31):
        for g in range(ngrp):
            nt = GNT[g]
            n = NB * nt * NE
            S.tt(mid[g].all(), lo[g].all(), hi[g].all(), ALU.add)
            S.ts(mid[g].all(), mid[g].all(), 0.5, ALU.mult)
            S.tt(ge[g].all(), aff[g].all(), mid[g].all().bc(2, nt), ALU.is_ge)
            pc = ppC.next()
            S.mm(pc[:, 0:n], k.ones_b.all(), ge[g].all().rearrange("p s t e -> p (s t e)"))
            S.reduce(cnt[g].all(), pc[:, 0:n].rearrange("p (s t e) -> p s e t", s=NB, t=nt), ALU.add)
            S.ts(selp[g].all(), cnt[g].all(), float(GCAP[g]), ALU.is_ge)
            S.ts(seln[g].all(), cnt[g].all(), float(GCAP[g]), ALU.is_lt)
            S.emit("dve", lambda e, g=g: e.copy_predicated(lo[g].t[:], selp[g].t[:].bitcast(U32), mid[g].t[:]),
                   reads=[selp[g].all(), mid[g].all()], writes=[lo[g].all()])
            S.emit("dve", lambda e, g=g: e.copy_predicated(hi[g].t[:], seln[g].t[:].bitcast(U32), mid[g].t[:]),
                   reads=[seln[g].all(), mid[g].all()], writes=[hi[g].all()])
    for g in range(ngrp):
        nt = GNT[g]
        n = NB * nt * NE
        cap = GCAP[g]
        S.tt(ge[g].all(), aff[g].all(), lo[g].all().bc(2, nt), ALU.is_ge)
        gef = ge[g].all().rearrange("p s t e -> p (s t e)")
        pr = ppC.next()
        S.mm(pr[:, 0:n], k.SUt.all(), gef)
        ptot = ppC.next()
        S.mm(ptot[:, 0:n], k.ones_b.all(), gef)
        tot = S.sb(f"tot{g}", [128, NB, nt, NE], F32)
        S.copy(tot.all(), ptot[:, 0:n].rearrange("p (s t e) -> p s t e", s=NB, t=nt))
        off = S.sb(f"off{g}", [128, NB, nt, NE], F32)
        S.memset(off[:, :, 0, :], 0.0)
        for j in range(1, nt):
            S.tt(off[:, :, j, :], off[:, :, j - 1, :], tot[:, :, j - 1, :], ALU.add)
        rank = S.sb(f"rank{g}", [128, NB, nt, NE], F32)
        S.tt(rank.all(), pr[:, 0:n].rearrange("p (s t e) -> p s t e", s=NB, t=nt), off.all(), ALU.add)
        ok = S.sb(f"ok{g}", [128, NB, nt, NE], F32)
        S.ts(ok.all(), rank.all(), float(cap), ALU.is_lt)
        S.tt(ok.all(), ok.all(), ge[g].all(), ALU.mult)
        for b in range(NB):
            offs = 1.0 + (32.0 * b if g == 1 else 0.0)
            S.stt(sl[g][:, b], rank[:, b], offs, ok[:, b], ALU.add, ALU.mult)
        S.ts(sl[g].all(), sl[g].all(), -1.0, ALU.add)
        for b in range(NB):
            for j0 in range(0, nt, 4):
                m = min(4, nt - j0)
                ps = ppA.next()
                for c in range(m):
                    S.tr(ps[0:NE, c * 128:(c + 1) * 128], sl[g][:, b, j0 + c, :], k.identf.all())
                S.copy(slT[g][b][:, j0 * 128:(j0 + m) * 128], ps[0:NE, 0:m * 128])
        S.copy(affhl[g][:, :, :, :, 0], aff[g].all())
        S.tt(affhl[g][:, :, :, :, 1], aff[g].all(), affhl[g][:, :, :, :, 0], ALU.subtract)


def moe_experts(S, k, layer, ngrp, GNT, hn, sl, affhl, NSLOT):
    has_ctx = ngrp == 2
    wrot = S.rot("mw", [128, 8, 1024], BF16, 4)
    xgT = S.sb("xgT", [128, 8, NSLOT], BF16)
    hT = S.sb("mhT", [128, 8, NSLOT], BF16)
    serot = S.rot("Se", [128, 16, 256], BF16, 2)
    secrot = S.rot("SeC", [128, NB, 2, 64], BF16, 2)
    sarot = S.rot("msa", [128, 512], F32, 2)
    yrot = S.rot("my", [128, 1024], BF16, 2)
    gates = S.rot("mgate", [128, 8], F32, 2)
    ppA = S.pool([0, 1, 2])
    ppB = S.pool([3, 4, 5])
    ppG = S.pool([6, 7])
    nchunks = [(0, 512)] + ([(512, 64)] if has_ctx else [])
    jchunks = [(0, 128), (128, 128), (256, 128), (384, 128)] + ([(512, 64)] if has_ctx else [])
    wsrc = {1: k.moe_w1, 3: k.moe_w3, 2: k.moe_w2}

    def loadw(which, e):
        w = wrot.next()
        src = wsrc[which]
        for q in range(2):
            S.dma(w[:, q * 4:(q + 1) * 4, :],
                  View(src.t[layer, e].rearrange("(kk p) n -> p kk n", p=128)[:, q * 4:(q + 1) * 4, :], src.res), eng="pool")
        return w

    for e in range(NE):
        w1 = loadw(1, e)
        w3 = loadw(3, e)
        gt = gates.next()
        pgt = ppG.next()
        for b in range(NB):
            Se = serot.next()
            for tt_ in range(16):
                S.ts(Se[:, tt_, :], k.iota[:, 0:256], sl[0][:, b, tt_, e:e + 1], ALU.is_equal)
            for kk0 in range(0, 8, 2):
                pg = ppA.next()
                for c in range(2):
                    kk = kk0 + c
                    for tt_ in range(16):
                        S.mm(pg[:, c * 256:(c + 1) * 256], hn[0][b][:, tt_, kk * 128:(kk + 1) * 128], Se[:, tt_, :],
                             start=(tt_ == 0), stop=(tt_ == 15))
                S.copy(xgT[:, kk0:kk0 + 2, b * 256:(b + 1) * 256], pg.all().rearrange("p (c j) -> p c j", c=2), eng="act")
            for jc in range(2):
                col = (b * 2 + jc) * 2
                for tt_ in range(16):
                    S.mm(pgt[:, col:col + 2], Se[:, tt_, jc * 128:(jc + 1) * 128], affhl[0][:, b, tt_, e, :],
                         start=(tt_ == 0), stop=(tt_ == 15))
        if has_ctx:
            SeC = secrot.next()
            for b in range(NB):
                for tt_ in range(2):
                    S.ts(SeC[:, b, tt_, :], k.iota[:, 0:64], sl[1][:, b, tt_, e:e + 1], ALU.is_equal)
            for kk0 in range(0, 8, 4):
                pg = ppA.next()
                for c in range(4):
                    kk = kk0 + c
                    n = 0
                    for b in range(NB):
                        for tt_ in range(2):
                            S.mm(pg[:, c * 64:(c + 1) * 64], hn[1][b][:, tt_, kk * 128:(kk + 1) * 128], SeC[:, b, tt_, :],
                                 start=(n == 0), stop=(n == 3))
                            n += 1
                S.copy(xgT[:, kk0:kk0 + 4, 512:576], pg[:, 0:256].rearrange("p (c j) -> p c j", c=4), eng="act")
            n = 0
            for b in range(NB):
                for tt_ in range(2):
                    S.mm(pgt[0:64, 8:10], SeC[:, b, tt_, :], affhl[1][:, b, tt_, e, :], start=(n == 0), stop=(n == 3))
                    n += 1
        ncol = 5 if has_ctx else 4
        S.reduce(gt[:, 0:ncol], pgt[:, 0:2 * ncol].rearrange("p (c two) -> p c two", two=2), ALU.add)
        for fc in range(8):
            for (n0, n) in nchunks:
                pa = ppA.next()
                for kk in range(8):
                    S.mm(pa[:, 0:n], w1[:, kk, fc * 128:(fc + 1) * 128], xgT[:, kk, n0:n0 + n], start=(kk == 0), stop=(kk == 7))
                pb = ppB.next()
                for kk in range(8):
                    S.mm(pb[:, 0:n], w3[:, kk, fc * 128:(fc + 1) * 128], xgT[:, kk, n0:n0 + n], start=(kk == 0), stop=(kk == 7))
                sa = sarot.next()
                S.act(sa[:, 0:n], pa[:, 0:n], AF.Silu)
                S.tt(hT[:, fc, n0:n0 + n], sa[:, 0:n], pb[:, 0:n], ALU.mult)
        w2 = loadw(2, e)
        for ji, (j0, m) in enumerate(jchunks):
            y = yrot.next()
            for hf in range(2):
                py = ppB.next()
                for fc in range(8):
                    S.mm(py[0:m, :], hT[:, fc, j0:j0 + m], w2[:, fc, hf * 512:(hf + 1) * 512], start=(fc == 0), stop=(fc == 7))
                S.act(y[0:m, hf * 512:(hf + 1) * 512], py[0:m, :], AF.Copy, scale=gt[0:m, ji:ji + 1])
            S.dma(View(k.y_d.t[e, j0:j0 + m, :], k.y_d.res), y[0:m, :])


def moe_scatter(S, k, layer, xs_fn, ngrp, GNT, GTI0, slT):
    sel16 = S.sb("sel16", [16, NE, 128], BF16)
    selio = S.sb("selio", [16, NE, 128], F32)
    S.emit("pool", lambda e: e.iota(selio.t[:], [[1, NE], [0, 128]], base=0, channel_multiplier=0,
                                    allow_small_or_imprecise_dtypes=True), writes=[selio.all()])
    S.ts(sel16.all(), selio.all(), k.pid[0:16, :], ALU.is_equal)
    pid32 = S.sb("pid32", [128, 1], F32)
    S.ts(pid32.all(), k.pid.all(), 32.0, ALU.add)
    yall = S.sb("yall", [128, 2 * NE, 1024], BF16)
    strot = S.rot("ST", [128, 2, NE, 128], BF16, 2)
    xrot = S.rot("sx", [128, 1024], F32, 2)
    xorot = S.rot("sxo", [128, 1024], F32, 2)
    ppA = S.pool([0, 1, 2, 3])
    ppB = S.pool([4, 5, 6, 7])
    for g in range(ngrp):
        for b in range(NB):
            g2 = S.sb("g2v", [128, 1024], F32)
            load_mod(S, k, layer, 2 if g == 1 else b, 5, g2.all())
            if g == 0:
                kr = 128
                njc = 2
                for e in range(NE):
                    S.dma(yall[:, 2 * e:2 * e + 2, :],
                          View(k.y_d.t[e, b * 256:(b + 1) * 256, :].rearrange("(jc j) d -> j jc d", jc=2), k.y_d.res))
                cmpv = [k.pid, k.pid128]
            else:
                kr = 32
                njc = 1
                S.dma(yall[0:32, 0:NE, :],
                      View(k.y_d.t[:, 512 + b * 32:512 + (b + 1) * 32, :].rearrange("e j d -> j e d"), k.y_d.res))
                cmpv = [k.pid if b == 0 else pid32]
            def make_ST(j):
                banks = [ppA.next() for _ in range(4)]
                for e in range(NE):
                    S.mm(banks[e // 4][:, (e % 4) * 128:(e % 4 + 1) * 128], sel16[:, e, :], slT[g][b][:, j * 128:(j + 1) * 128])
                ST_ = strot.next()
                for jc in range(njc):
                    for q in range(4):
                        S.ts(ST_[:, jc, q * 4:(q + 1) * 4, :], banks[q].all().rearrange("p (e t) -> p e t", e=4),
                             cmpv[jc].all(), ALU.is_equal)
                return ST_

            ST_next = make_ST(0)
            for j in range(GNT[g]):
                ti = GTI0[g] + j
                ST = ST_next
                if j + 1 < GNT[g]:
                    ST_next = make_ST(j + 1)
                xt = xrot.next()
                S.dma(xt.all(), xs_fn(b)(ti))
                xo = xorot.next()
                for hf in range(2):
                    po = ppB.next()
                    n = 0
                    tot = NE * njc
                    for e in range(NE):
                        for jc in range(njc):
                            yi = 2 * e + jc if g == 0 else e
                            S.mm(po.all(), ST[0:kr, jc, e, :], yall[0:kr, yi, hf * 512:(hf + 1) * 512],
                                 start=(n == 0), stop=(n == tot - 1))
                            n += 1
                    S.tt(xo[:, hf * 512:(hf + 1) * 512], po.all(), g2[:, hf * 512:(hf + 1) * 512], ALU.mult)
                S.tt(xo.all(), xo.all(), xt.all(), ALU.add)
                S.dma(xs_fn(b)(ti), xo.all())

def attn_mixer(S, k, b, layer, xs):
    NLT = NT - 2
    with ExitStack() as st_outer:
        S.stack = st_outer
        qT = S.sb("qT", [128, 8, LAT], BF16)
        kTd = S.sb("kTd", [128, 4, TOK], BF16)
        vaug = S.sb("vaug", [128, NT, 4, 72], BF16)
        with ExitStack() as st_h:
            S.stack = st_h
            hT = S.sb("ahT", [128, 8, TOK], BF16)
            with ExitStack() as st:
                S.stack = st
                norm_T(S, k, b, layer, 1, xs, hT, [(i, i * 128) for i in range(NT)], S.pool([0, 1, 2, 3]))
                S.phase_end()
            import os
            if os.environ.get("ATT_STOP") != "norm":
                with ExitStack() as st:
                    S.stack = st
                    attn_qkv(S, k, hT, qT, kTd, vaug)
                    S.phase_end()
        import os
        if os.environ.get("ATT_STOP") not in ("qkv", "norm"):
            with ExitStack() as st:
                S.stack = st
                attn_core(S, k, b, layer, xs, qT, kTd, vaug)
                S.phase_end()
    S.stack = None


def attn_qkv(S, k, hT, qT, kTd, vaug):
    w = S.sb("wqkv", [128, 8, 1536], BF16)
    for q in range(3):
        S.dma(w[:, :, q * 512:(q + 1) * 512],
              View(k.attn_qkv_w.t[0].rearrange("(kk p) n -> p kk n", p=128)[:, :, q * 512:(q + 1) * 512], k.attn_qkv_w.res), eng="pool")
    qw = S.sb("qwbc", [128, 64], F32)
    kw = S.sb("kwbc", [128, 64], F32)
    load_bc(S, qw.all(), k.attn_q_norm_w, k.attn_q_norm_w.t[0])
    load_bc(S, kw.all(), k.attn_k_norm_w, k.attn_k_norm_w.t[0])
    S.memset(vaug.all(), 1.0)
    qkrot = S.rot("qk", [128, 20, 64], F32, 2)
    sqrot = S.rot("qsq", [128, 20, 64], F32, 1)
    qnrot = S.rot("qn", [128, 20, 64], F32, 2)
    qrrot = S.rot("qr", [128, 20, 64], BF16, 2)
    kdrot = S.rot("kd", [128, 4, 2, 64], BF16, 2)
    csrot = S.rot("cs", [128, 2, 32], F32, 2)
    r1rot = S.rot("rp1", [128, 20, 32], F32, 2)
    r2rot = S.rot("rp2", [128, 20, 32], F32, 2)
    ssrot = S.rot("qss", [128, 20], F32, 3)
    ppA = S.pool([0, 1, 2, 3, 4, 5])
    ppB = S.pool([6, 7])
    import os
    LVL = int(os.environ.get("QKV_LVL", "9"))
    if LVL < 1:
        return
    for i in range(NT):
        banks = []
        for nb_ in range(3):
            ps = ppA.next()
            for kk in range(8):
                S.mm(ps.all(), hT[:, kk, i * 128:(i + 1) * 128], w[:, kk, nb_ * 512:(nb_ + 1) * 512], start=(kk == 0), stop=(kk == 7))
            banks.append(ps)
        VAR = os.environ.get("QKV_VAR", "Z")
        if VAR == "A":
            continue
        qk = qkrot.next()
        S.copy(qk[:, 0:8, :], banks[0].all().rearrange("p (h d) -> p h d", h=8), eng="act")
        S.copy(qk[:, 8:16, :], banks[1].all().rearrange("p (h d) -> p h d", h=8), eng="act")
        S.copy(qk[:, 16:20, :], banks[2][:, 0:256].rearrange("p (h d) -> p h d", h=4), eng="act")
        if VAR == "B":
            continue
        S.copy(vaug[:, i, :, 0:64], banks[2][:, 256:512].rearrange("p (h d) -> p h d", h=4), eng="act")
        if LVL < 2:
            continue
        sq = sqrot.next()
        S.tt(sq.all(), qk.all(), qk.all(), ALU.mult)
        ss = ssrot.next()
        S.reduce(ss.all(), sq.all(), ALU.add)
        S.act(ss.all(), ss.all(), AF.Ln, bias=k.eps.all(), scale=1.0 / 64)
        S.act(ss.all(), ss.all(), AF.Exp, scale=-0.5)
        qn = qnrot.next()
        S.tt(qn.all(), qk.all(), ss.all().bc(2, 64), ALU.mult)
        S.tt(qn[:, 0:16, :], qn[:, 0:16, :], qw.all().bc(1, 16), ALU.mult)
        S.tt(qn[:, 16:20, :], qn[:, 16:20, :], kw.all().bc(1, 4), ALU.mult)
        if LVL < 3:
            continue
        qr = qrrot.next()
        if i >= 2:
            cs = csrot.next()
            t0 = (i - 2) * 128
            S.dma(cs[:, 0, :], View(k.cos.t[t0:t0 + 128, :], k.cos.res))
            S.dma(cs[:, 1, :], View(k.sin.t[t0:t0 + 128, :], k.sin.res))
            cosb = cs[:, 0, :].bc(1, 20)
            sinb = cs[:, 1, :].bc(1, 20)
            t1 = qn[:, :, 0:32]
            t2 = qn[:, :, 32:64]
            a1 = r1rot.next()
            a2 = r2rot.next()
            S.tt(a1.all(), t1, cosb, ALU.mult)
            S.tt(a2.all(), t2, sinb, ALU.mult)
            S.tt(qr[:, :, 0:32], a1.all(), a2.all(), ALU.subtract)
            a3 = r1rot.next()
            a4 = r2rot.next()
            S.tt(a3.all(), t2, cosb, ALU.mult)
            S.tt(a4.all(), t1, sinb, ALU.mult)
            S.tt(qr[:, :, 32:64], a3.all(), a4.all(), ALU.add)
        else:
            S.copy(qr.all(), qn.all())
        if LVL < 4:
            continue
        if i >= 2:
            ps = ppB.next()
            pv = pbf(ps)
            for pr in range(8):
                S.tr(pv[:, pr * 128:(pr + 1) * 128], qr[:, 2 * pr:2 * pr + 2, :].rearrange("p h d -> p (h d)"), k.identb.all())
            S.copy(qT[:, :, (i - 2) * 128:(i - 1) * 128], pv.rearrange("p (c t) -> p c t", c=8), eng="act")
        if LVL < 5:
            continue
        kd = kdrot.next()
        S.copy(kd[:, :, 0, :], qr[:, 16:20, :])
        S.copy(kd[:, :, 1, :], qr[:, 16:20, :], eng="act")
        ps = ppB.next()
        pv = pbf(ps)
        for kv in range(4):
            S.tr(pv[:, kv * 128:(kv + 1) * 128], kd[:, kv].rearrange("p a d -> p (a d)"), k.identb.all())
        S.copy(kTd[:, :, i * 128:(i + 1) * 128], pv[:, 0:512].rearrange("p (c t) -> p c t", c=4))


def attn_core(S, k, b, layer, xs, qT, kTd, vaug):
    wo = S.sb("awo", [128, 8, 1024], BF16)
    for q in range(2):
        S.dma(wo[:, q * 4:(q + 1) * 4, :],
              View(k.attn_out_w.t[0].rearrange("(kk p) n -> p kk n", p=128)[:, q * 4:(q + 1) * 4, :], k.attn_out_w.res), eng="pool")
    g1 = S.sb("ag1", [128, 1024], F32)
    load_mod(S, k, layer, b, 2, g1.all())
    ptrot = S.rot("PT", [128, 512], BF16, 3)
    otrot = S.rot("otm", [128, 4, 1024], BF16, 2)
    oTrot = S.rot("aoT", [128, 8, 128], BF16, 2)
    xrot = S.rot("ax", [128, 1024], F32, 2)
    xorot = S.rot("axo", [128, 1024], F32, 2)
    ppS = S.pool([0, 1, 2, 3])
    ppAcc = S.pool([4, 5])
    ppO = S.pool([6, 7])
    for tqb in range(4):
        tq0 = tqb * 512
        otm = otrot.next()
        steps = [(h, tk) for h in range(16) for tk in range(NT)]

        def emit_scores(h, tk):
            pair, e = h // 2, h % 2
            kv = h // 4
            p0, p1 = e * 64, (e + 1) * 64
            psc = ppS.next()
            S.mm(psc.all(), kTd[p0:p1, kv, tk * 128:(tk + 1) * 128], qT[p0:p1, pair, tq0:tq0 + 512])
            return psc

        pending = emit_scores(*steps[0])
        acc = None
        for si, (h, tk) in enumerate(steps):
            kv = h // 4
            psc = pending
            if si + 1 < len(steps):
                pending = emit_scores(*steps[si + 1])
            if tk == 0:
                acc = ppAcc.next()
            PT = ptrot.next()
            S.act(PT.all(), psc.all(), AF.Exp, scale=0.125)
            for r in range(4):
                S.mm(acc[:, r * 128:r * 128 + 65], PT[:, r * 128:(r + 1) * 128], vaug[:, tk, kv, 0:65],
                     start=(tk == 0 and r == 0), stop=(tk == NT - 1 and r == 3))
            if tk == NT - 1:
                rc = k.small.next()
                a3 = acc.all().rearrange("p (r c) -> p r c", r=4)
                S.recip(rc[:, 0:4], a3[:, :, 64])
                S.tt(otm[:, :, h * 64:(h + 1) * 64], a3[:, :, 0:64], rc[:, 0:4].bc(2, 64), ALU.mult)
        for r in range(4):
            ti = 2 + tqb * 4 + r
            ps = ppO.next()
            pv = pbf(ps)
            for kk in range(8):
                S.tr(pv[:, kk * 128:(kk + 1) * 128], otm[:, r, kk * 128:(kk + 1) * 128], k.identb.all())
            oT = oTrot.next()
            S.copy(oT.all(), pv.rearrange("p (c t) -> p c t", c=8), eng="act")
            xt = xrot.next()
            S.dma(xt.all(), xs(ti))
            xo = xorot.next()
            for hf in range(2):
                po = ppO.next()
                for kk in range(8):
                    S.mm(po.all(), oT[:, kk, :], wo[:, kk, hf * 512:(hf + 1) * 512], start=(kk == 0), stop=(kk == 7))
                S.tt(xo[:, hf * 512:(hf + 1) * 512], po.all(), g1[:, hf * 512:(hf + 1) * 512], ALU.mult)
            S.tt(xo.all(), xo.all(), xt.all(), ALU.add)
            S.dma(xs(ti), xo.all())


def final_norm(S, k, xs_fn):
    with ExitStack() as st:
        S.stack = st
        fw = S.sb("fnw", [128, 1024], F32)
        load_bc(S, fw.all(), k.final_norm_w, k.final_norm_w.t)
        xrot = S.rot("fx", [128, 1024], F32, 3)
        jrot = S.rot("fj", [128, 1024], F32, 2)
        orot = S.rot("fo", [128, 1024], F32, 3)
        for b in range(NB):
            for j in range(NT - 2):
                xt = xrot.next()
                S.dma(xt.all(), xs_fn(b)(2 + j))
                rs = k.small.next()
                rms_rstd(S, k, rs[:, 2:3], xt.all(), 1024, jrot.next().all())
                xo = orot.next()
                S.stt(xo.all(), xt.all(), rs[:, 2:3], fw.all(), ALU.mult, ALU.mult)
                S.dma(View(k.out.t[b, j * 128:(j + 1) * 128, :], k.out.res), xo.all())
        S.phase_end()
    S.stack = None


WEIGHT_SPECS = [
    ("ada_w", [2, 1024, 6144]), ("ada_b", [2, 6144]), ("norm1_w", [2, 1024]), ("norm2_w", [2, 1024]),
    ("ssd_in_w", [1, 1024, 6208]), ("ssd_conv_w", [1, 5, 4096]), ("ssd_conv_b", [1, 4096]),
    ("ssd_dt_bias", [1, 2, 32]), ("ssd_A_log", [1, 2, 32]), ("ssd_D", [1, 32]), ("ssd_norm_w", [1, 2048]),
    ("ssd_out_w", [1, 2048, 1024]), ("attn_qkv_w", [1, 1024, 1536]), ("attn_q_norm_w", [1, 64]),
    ("attn_k_norm_w", [1, 64]), ("attn_out_w", [1, 1024, 1024]), ("moe_router_w", [2, 1024, 16]),
    ("moe_w1", [2, 16, 1024, 1024]), ("moe_w3", [2, 16, 1024, 1024]), ("moe_w2", [2, 16, 1024, 1024]),
    ("final_norm_w", [1024]),
]


def build_program(stop=None, dbg=False, only=None):
    nc = bass.Bass("TRN2", target_bir_lowering=False)
    k = K()
    with ExitStack() as st:
        S = Sched(nc, st)
        S.init_psum(8)

        def din(name, shape):
            t = nc.dram_tensor(name, list(shape), F32, kind="ExternalInput")
            return Tl(t.ap(), 1, name)

        k.x = din("x", [NB, LAT, D])
        k.c = din("c", [NB, D])
        k.ctx = din("ctx", [NB, CTXL, D])
        k.c_ctx = din("c_ctx", [D])
        for name, shape in WEIGHT_SPECS:
            setattr(k, name, din(name, shape))
        k.cos = din("rope_cos", [LAT, 32])
        k.sin = din("rope_sin", [LAT, 32])
        k.out = Tl(nc.dram_tensor("out", [NB, LAT, D], F32, kind="ExternalOutput").ap(), 1, "out")
        xkind = "ExternalOutput"
        k.xs = [Tl(nc.dram_tensor(f"xs{b}", [NT, 128, D], F32, kind=xkind).ap(), NT, f"xs{b}") for b in range(NB)]
        k.modv = [S.dram(f"modv{l}", [3, 6144], F32) for l in range(2)]
        k.xtm_d = S.dram("xtm_d", [NT, 128, 3072], BF16)
        k.bc_d = S.dram("bc_d", [16, 128, TOK], BF16)
        k.yf_d = S.dram("yf_d", [NT, 128, 2048], F32)
        k.gn_d = S.dram("gn_d", [NT, 128, 2048], BF16)
        k.y_d = S.dram("y_d", [16, 576, 1024], BF16)
        k.small = S.rot("small", [128, 4], F32, 12)
        setup_consts(S, k)
        S.phase_end()

        def in_src(b):
            def f(i):
                if i < 2:
                    return View(k.ctx.t[b, i * 128:(i + 1) * 128, :], k.ctx.res)
                return View(k.x.t[b, (i - 2) * 128:(i - 1) * 128, :], k.x.res)
            return f

        def xs_fn(b):
            def f(i):
                return View(k.xs[b].t[i], [k.xs[b].res[i]])
            return f

        stages = []
        stages.append(("mod0", lambda: phase_mod(S, k, 0)))
        for b in range(NB):
            stages.append((f"ssd{b}", lambda b=b: ssd_mixer(S, k, b, 0, in_src(b), xs_fn(b))))
        stages.append(("moe0", lambda: moe_layer(S, k, 0, xs_fn)))
        stages.append(("mod1", lambda: phase_mod(S, k, 1)))
        for b in range(NB):
            stages.append((f"attn{b}", lambda b=b: attn_mixer(S, k, b, 1, xs_fn(b))))
        stages.append(("moe1", lambda: moe_layer(S, k, 1, xs_fn)))
        stages.append(("final", lambda: final_norm(S, k, xs_fn)))
        if only is not None:
            with ExitStack() as st2:
                S.stack = st2
                rot = S.rot("initx", [128, D], F32, 3)
                for b in range(NB):
                    xi = din(f"xs_init{b}", [NT, 128, D])
                    for i in range(NT):
                        t = rot.next()
                        S.dma(t.all(), View(xi.t[i], xi.res))
                        S.dma(xs_fn(b)(i), t.all())
                S.phase_end()
        for name, fn in stages:
            if only is not None and name not in only:
                continue
            S.stack = st
            fn()
            if stop == name:
                break
        S.stack = st
        outs = [k.out.all()] + [x.all() for x in k.xs]
        S.finish(outs)
        k.ninst = S.ninst
    return nc, k


_ROPE = None


def rope_tables():
    global _ROPE
    if _ROPE is None:
        S_ = LAT
        row = np.repeat(np.arange(S_ // 64), 64).astype(np.float32)
        col = np.tile(np.arange(64), S_ // 64).astype(np.float32)
        inv = (np.float32(10000.0) ** (-np.arange(0, 32, 2, dtype=np.float32) / np.float32(32))).astype(np.float32)
        ang = np.concatenate([row[:, None] * inv, col[:, None] * inv], axis=-1).astype(np.float32)
        _ROPE = (np.cos(ang).astype(np.float32), np.sin(ang).astype(np.float32))
    return _ROPE


_PROG = {}


def kernel(**inputs):
    n_cores = 8
    if "full" not in _PROG:
        _PROG["full"] = build_program()[0]
    nc = _PROG["full"]
    cos, sin = rope_tables()
    shared = {name: np.ascontiguousarray(inputs[name], dtype=np.float32) for name, _ in WEIGHT_SPECS}
    shared["c_ctx"] = np.ascontiguousarray(inputs["c_ctx"], dtype=np.float32)
    shared["rope_cos"] = cos
    shared["rope_sin"] = sin
    in_maps = []
    for cid in range(n_cores):
        m = dict(shared)
        sl = slice(cid * NB, (cid + 1) * NB)
        m["x"] = np.ascontiguousarray(inputs["x"][sl], dtype=np.float32)
        m["c"] = np.ascontiguousarray(inputs["c"][sl], dtype=np.float32)
        m["ctx"] = np.ascontiguousarray(inputs["ctx"][sl], dtype=np.float32)
        in_maps.append(m)
    res = run_bass_kernel_spmd(nc, in_maps, core_ids=list(range(n_cores)))
    return np.concatenate([np.asarray(r["out"]) for r in res.results], axis=0).astype(np.float32)
```

```python
from contextlib import ExitStack
import math
import numpy as np
import concourse.bass as bass
import concourse.mybir as mybir
from concourse.bass_utils import run_bass_kernel_spmd

F32 = mybir.dt.float32
BF16 = mybir.dt.bfloat16
AF = mybir.ActivationFunctionType
ALU = mybir.AluOpType
AX = mybir.AxisListType

ENGS = ["pe", "dve", "act", "pool", "sp"]
NLANES = 28
NLANES_HW = 16


class Res:
    __slots__ = ("w", "r", "name")

    def __init__(self, name=""):
        self.w = None
        self.r = []
        self.name = name


class View:
    __slots__ = ("ap", "res")

    def __init__(self, ap, res):
        self.ap = ap
        self.res = res

    def __getitem__(self, idx):
        return View(self.ap[idx], self.res)

    def rearrange(self, *a, **k):
        return View(self.ap.rearrange(*a, **k), self.res)

    def bitcast(self, dt):
        return View(self.ap.bitcast(dt), self.res)

    def bc(self, axis, n):
        a = self.ap.unsqueeze(axis)
        shp = list(a.shape)
        shp[axis] = n
        return View(a.to_broadcast(shp), self.res)


class Tl:
    def __init__(self, t, nslots=1, name=""):
        self.t = t
        self.nslots = nslots
        self.res = [Res(f"{name}[{i}]") for i in range(nslots)]
        self.name = name

    def __getitem__(self, idx):
        return View(self.t[idx], self.res)

    def all(self):
        return View(self.t[:], self.res)

    def s(self, i, j=None):
        if j is None:
            return View(self.t[:, i], [self.res[i]])
        return View(self.t[:, i:j], self.res[i:j])


class Rot:
    def __init__(self, tiles):
        self.tiles = tiles
        self.i = 0

    def next(self):
        t = self.tiles[self.i % len(self.tiles)]
        self.i += 1
        return t


class Sched:
    def __init__(self, nc, stack):
        self.nc = nc
        self.stack = stack
        self.q = {e: [] for e in ENGS}
        self.sem = {e: stack.enter_context(nc.semaphore(f"s_{e}")) for e in ENGS}
        self.cnt = {e: 0 for e in ENGS}
        self.lane_sem = [stack.enter_context(nc.semaphore(f"l_{i}")) for i in range(NLANES)]
        self.lane_cnt = [0] * NLANES
        self.lane_next = 0
        self.lane_next_sw = 0
        self.seen = {e: {} for e in ENGS}
        self.ninst = 0
        self.psum_banks = []
        self.uid = 0

    def sb(self, name, shape, dtype, nslots=1):
        self.uid += 1
        nm = f"{name}_{self.uid}"
        t = self.stack.enter_context(self.nc.sbuf_tensor(nm, list(shape), dtype))
        return Tl(t, nslots, nm)

    def rot(self, name, shape, dtype, n):
        return Rot([self.sb(f"{name}{i}", shape, dtype) for i in range(n)])

    def dram(self, name, shape, dtype, kind="Internal", nslots=1):
        t = self.nc.dram_tensor(name, list(shape), dtype, kind=kind)
        return Tl(t.ap(), nslots, name)

    def init_psum(self, n=8):
        for i in range(n):
            t = self.stack.enter_context(self.nc.psum_tensor(f"ps{i}", [128, 512], F32))
            self.psum_banks.append(Tl(t, 1, f"ps{i}"))

    def pool(self, idx):
        return Rot([self.psum_banks[i] for i in idx])

    def _collect(self, eng, reads, writes):
        waits = {}

        def need(tok):
            if tok is None:
                return
            kind, key, val = tok
            if kind == "e" and key == eng and eng == "pe":
                return
            k = (kind, key)
            if waits.get(k, 0) < val:
                waits[k] = val

        for v in reads:
            for r in v.res:
                need(r.w)
        for v in writes:
            for r in v.res:
                need(r.w)
                for tok in r.r:
                    need(tok)
        out = []
        seen = self.seen[eng]
        for k, val in waits.items():
            if seen.get(k, 0) >= val:
                continue
            seen[k] = val
            out.append((k, val))
        return out

    def _mark(self, tok, reads, writes):
        for v in writes:
            for r in v.res:
                r.w = tok
                r.r = []
        for v in reads:
            for r in v.res:
                r.r = [t for t in r.r if (t[0], t[1]) != (tok[0], tok[1])]
                r.r.append(tok)

    def _semof(self, k):
        kind, key = k
        return self.sem[key] if kind == "e" else self.lane_sem[key]

    def emit(self, eng, fn, reads=(), writes=()):
        reads = list(reads)
        writes = list(writes)
        waits = self._collect(eng, reads, writes)
        self.cnt[eng] += 1
        val = self.cnt[eng]
        sem = self.sem[eng]
        wl = [(self._semof(k), v) for k, v in waits]

        def run(e, wl=wl, fn=fn, sem=sem):
            for s, v in wl:
                e.wait_ge(s, v)
            fn(e).then_inc(sem, 1)

        self.q[eng].append(run)
        self._mark(("e", eng, val), reads, writes)
        self.ninst += 1

    def dma(self, out, in_, eng="sp", **kw):
        if eng == "pool":
            lane = NLANES_HW + self.lane_next_sw % (NLANES - NLANES_HW)
            self.lane_next_sw += 1
        else:
            lane = self.lane_next % NLANES_HW
            self.lane_next += 1
        reads = [in_]
        writes = [out]
        waits = self._collect(eng, reads, writes)
        prev = self.lane_cnt[lane]
        k = ("d", lane)
        if prev > 0 and self.seen[eng].get(k, 0) < prev:
            self.seen[eng][k] = prev
            waits = [w for w in waits if w[0] != k] + [(k, prev)]
        self.lane_cnt[lane] += 16
        val = self.lane_cnt[lane]
        sem = self.lane_sem[lane]
        wl = [(self._semof(k2), v) for k2, v in waits]
        oap, iap = out.ap, in_.ap

        def run(e, wl=wl, sem=sem, oap=oap, iap=iap, kw=kw):
            for s, v in wl:
                e.wait_ge(s, v)
            e.dma_start(out=oap, in_=iap, **kw).then_inc(sem, 16)

        self.q[eng].append(run)
        self._mark(("d", lane, val), reads, writes)
        self.ninst += 1

    def wait_all(self, eng, views):
        waits = self._collect(eng, list(views), [])
        wl = [(self._semof(k), v) for k, v in waits]

        def run(e, wl=wl):
            for s, v in wl:
                e.wait_ge(s, v)

        self.q[eng].append(run)

    def barrier(self):
        snap_e = dict(self.cnt)
        snap_l = list(self.lane_cnt)
        for eng in ENGS:
            wl = []
            seen = self.seen[eng]
            for e2, v in snap_e.items():
                if v > 0 and seen.get(("e", e2), 0) < v and not (e2 == eng and eng == "pe"):
                    seen[("e", e2)] = v
                    wl.append((self.sem[e2], v))
            for i, v in enumerate(snap_l):
                if v > 0 and seen.get(("d", i), 0) < v:
                    seen[("d", i)] = v
                    wl.append((self.lane_sem[i], v))

            def run(e, wl=wl):
                for s, v in wl:
                    e.wait_ge(s, v)

            self.q[eng].append(run)

    def flush(self):
        nc = self.nc
        q = self.q
        self.q = {e: [] for e in ENGS}
        with nc.Block() as block:
            @block.sync
            def _(e):
                for f in q["sp"]:
                    f(e)

            @block.tensor
            def _(e):
                for f in q["pe"]:
                    f(e)

            @block.vector
            def _(e):
                for f in q["dve"]:
                    f(e)

            @block.scalar
            def _(e):
                for f in q["act"]:
                    f(e)

            @block.gpsimd
            def _(e):
                for f in q["pool"]:
                    f(e)

    def phase_end(self):
        self.barrier()
        self.flush()

    def finish(self, final_views):
        self.wait_all("sp", final_views)
        self.flush()

    def mm(self, out, lhsT, rhs, start=True, stop=True):
        self.emit("pe", lambda e: e.matmul(out.ap, lhsT.ap, rhs.ap, start=start, stop=stop),
                  reads=[lhsT, rhs], writes=[out])

    def tr(self, out, in_, ident):
        self.emit("pe", lambda e: e.transpose(out.ap, in_.ap, ident.ap), reads=[in_, ident], writes=[out])

    def act(self, out, in_, func, bias=None, scale=None, accum_out=None):
        reads = [in_]
        writes = [out]
        kw = {}
        if bias is not None:
            if isinstance(bias, View):
                reads.append(bias)
                kw["bias"] = bias.ap
            else:
                kw["bias"] = bias
        if scale is not None:
            if isinstance(scale, View):
                reads.append(scale)
                kw["scale"] = scale.ap
            else:
                kw["scale"] = scale
        if accum_out is not None:
            writes.append(accum_out)
            kw["accum_out"] = accum_out.ap
        self.emit("act", lambda e: e.activation(out.ap, in_.ap, func, **kw), reads=reads, writes=writes)

    def tt(self, out, a, b, op, eng="dve"):
        self.emit(eng, lambda e: e.tensor_tensor(out=out.ap, in0=a.ap, in1=b.ap, op=op), reads=[a, b], writes=[out])

    def ts(self, out, a, s1, op0, s2=None, op1=None, accum_out=None, eng="dve"):
        reads = [a]
        writes = [out]
        s1v = s1.ap if isinstance(s1, View) else s1
        s2v = s2.ap if isinstance(s2, View) else s2
        if isinstance(s1, View):
            reads.append(s1)
        if isinstance(s2, View):
            reads.append(s2)
        kw = {}
        if op1 is not None:
            kw["op1"] = op1
        if accum_out is not None:
            kw["accum_out"] = accum_out.ap
            writes.append(accum_out)
        self.emit(eng, lambda e: e.tensor_scalar(out=out.ap, in0=a.ap, scalar1=s1v, scalar2=s2v, op0=op0, **kw),
                  reads=reads, writes=writes)

    def stt(self, out, a, s, b, op0, op1, eng="dve"):
        reads = [a, b]
        sv = s.ap if isinstance(s, View) else s
        if isinstance(s, View):
            reads.append(s)
        self.emit(eng, lambda e: e.scalar_tensor_tensor(out=out.ap, in0=a.ap, scalar=sv, in1=b.ap, op0=op0, op1=op1),
                  reads=reads, writes=[out])

    def copy(self, out, in_, eng="dve"):
        if eng == "act":
            self.emit("act", lambda e: e.activation(out.ap, in_.ap, AF.Copy), reads=[in_], writes=[out])
        else:
            self.emit(eng, lambda e: e.tensor_copy(out=out.ap, in_=in_.ap), reads=[in_], writes=[out])

    def memset(self, out, val, eng="dve"):
        self.emit(eng, lambda e: e.memset(out.ap, val), reads=[], writes=[out])

    def reduce(self, out, in_, op, axis=None):
        axis = axis or AX.X
        self.emit("dve", lambda e: e.tensor_reduce(out=out.ap, in_=in_.ap, axis=axis, op=op), reads=[in_], writes=[out])

    def recip(self, out, in_):
        self.emit("dve", lambda e: e.reciprocal(out=out.ap, in_=in_.ap), reads=[in_], writes=[out])


def pbf(bank):
    return View(bank.t[:].bitcast(BF16), bank.res)

D = 1024
LAT = 2048
CTXL = 256
NT = 18
TOK = NT * 128
DI = 2048
NH = 32
NG = 8
EPS = 1e-6
NB = 2


class K:
    pass


def setup_consts(S, k):
    nc = S.nc
    k.iota = S.sb("iota", [128, 256], F32)
    k.pid = S.sb("pid", [128, 1], F32)
    k.pid128 = S.sb("pid128", [128, 1], F32)
    k.eps = S.sb("epsc", [128, 1], F32)
    S.emit("pool", lambda e: e.iota(k.iota.t[:], [[1, 256]], base=0, channel_multiplier=0,
                                    allow_small_or_imprecise_dtypes=True), writes=[k.iota.all()])
    S.emit("pool", lambda e: e.iota(k.pid.t[:], [[0, 1]], base=0, channel_multiplier=1,
                                    allow_small_or_imprecise_dtypes=True), writes=[k.pid.all()])
    S.ts(k.pid128.all(), k.pid.all(), 128.0, ALU.add)
    S.memset(k.eps.all(), EPS)
    io = k.iota[:, 0:128]

    def mk(name, dt, op):
        t = S.sb(name, [128, 128], dt)
        S.ts(t.all(), io, k.pid.all(), op)
        return t

    k.identb = mk("identb", BF16, ALU.is_equal)
    k.identf = mk("identf", F32, ALU.is_equal)
    k.U_le = mk("U_le", BF16, ALU.is_ge)
    k.L_ge = mk("L_ge", BF16, ALU.is_le)
    k.SLt = mk("SLt", BF16, ALU.is_lt)
    k.SUt = mk("SUt", BF16, ALU.is_gt)
    k.U_le_f = mk("U_le_f", F32, ALU.is_ge)
    k.L_ge_f = mk("L_ge_f", F32, ALU.is_le)
    k.ones_f = S.sb("ones_f", [128, 128], F32)
    S.memset(k.ones_f.all(), 1.0)
    k.ones_b = S.sb("ones_b", [128, 128], BF16)
    S.memset(k.ones_b.all(), 1.0)


def load_bc(S, dst, dram_tl, ap):
    S.dma(dst, View(ap.partition_broadcast(dst.ap.shape[0]), dram_tl.res))


def rms_rstd(S, k, rs, src, n, junk):
    ss = k.small.next()
    S.act(junk, src, AF.Square, accum_out=ss[:, 0:1])
    S.act(ss[:, 1:2], ss[:, 0:1], AF.Sqrt, bias=k.eps.all(), scale=1.0 / n)
    S.recip(rs, ss[:, 1:2])


def phase_mod(S, k, layer):
    with ExitStack() as st:
        S.stack = st
        cin = S.sb("cin", [128, 3, 8], F32)
        for b in range(NB):
            S.dma(cin[:, b, :], View(k.c.t[b].rearrange("(p kk) -> p kk", kk=8), k.c.res))
        S.dma(cin[:, 2, :], View(k.c_ctx.t.rearrange("(p kk) -> p kk", kk=8), k.c_ctx.res))
        sc = S.sb("silc", [128, 8, 3], BF16)
        S.act(sc.all().rearrange("p kk v -> p v kk"), cin.all(), AF.Silu)
        bias = S.sb("adab", [3, 6144], F32)
        load_bc(S, bias.all(), k.ada_b, k.ada_b.t[layer])
        nw = S.sb("nw12", [3, 2, 1024], F32)
        load_bc(S, nw[:, 0, :], k.norm1_w, k.norm1_w.t[layer])
        load_bc(S, nw[:, 1, :], k.norm2_w, k.norm2_w.t[layer])
        res = S.sb("modres", [3, 6144], F32)
        wrot = S.rot("adaw", [128, 8, 1024], BF16, 2)
        pp = S.pool([0, 1, 2, 3])
        for blk in range(6):
            w = wrot.next()
            S.dma(w.all(), View(k.ada_w.t[layer].rearrange("(p kk) n -> p kk n", kk=8)[:, :, blk * 1024:(blk + 1) * 1024],
                                k.ada_w.res), eng="pool")
            for half in range(2):
                ps = pp.next()
                for kk in range(8):
                    S.mm(ps[0:3, :], sc[:, kk, :], w[:, kk, half * 512:(half + 1) * 512], start=(kk == 0), stop=(kk == 7))
                c0 = blk * 1024 + half * 512
                S.tt(res[:, c0:c0 + 512], ps[0:3, :], bias[:, c0:c0 + 512], ALU.add)
        for j, blk in enumerate((1, 4)):
            S.stt(res[:, blk * 1024:(blk + 1) * 1024], res[:, blk * 1024:(blk + 1) * 1024], 1.0, nw[:, j, :], ALU.add, ALU.mult)
        S.dma(k.modv[layer].all(), res.all())
        S.phase_end()


def load_mod(S, k, layer, row, blk, dst):
    load_bc(S, dst, k.modv[layer], k.modv[layer].t[row, blk * 1024:(blk + 1) * 1024])


def norm_T(S, k, b, layer, which, src_fn, hT, tiles, pp):
    blkA, blkS = (1, 0) if which == 1 else (4, 3)
    mods = {}
    for kind in (0, 1):
        if not any((ti < 2) == (kind == 0) for ti, _ in tiles):
            continue
        A = S.sb("modA", [128, 1024], F32)
        sh = S.sb("modS", [128, 1024], F32)
        row = 2 if kind == 0 else b
        load_mod(S, k, layer, row, blkA, A.all())
        load_mod(S, k, layer, row, blkS, sh.all())
        mods[kind] = (A, sh)
    xrot = S.rot("nx", [128, 1024], F32, 3)
    trot = S.rot("ntmp", [128, 1024], F32, 2)
    hrot = S.rot("nhb", [128, 1024], BF16, 2)
    for ti, col in tiles:
        A, sh = mods[0 if ti < 2 else 1]
        xt = xrot.next()
        S.dma(xt.all(), src_fn(ti))
        tmp = trot.next()
        rs = k.small.next()
        rms_rstd(S, k, rs[:, 2:3], xt.all(), 1024, tmp.all())
        S.stt(tmp.all(), xt.all(), rs[:, 2:3], A.all(), ALU.mult, ALU.mult)
        hb = hrot.next()
        S.tt(hb.all(), tmp.all(), sh.all(), ALU.add)
        ps = pp.next()
        pv = pbf(ps)
        for kk in range(8):
            S.tr(pv[:, kk * 128:(kk + 1) * 128], hb[:, kk * 128:(kk + 1) * 128], k.identb.all())
        S.copy(hT[:, :, col:col + 128], pv.rearrange("p (kk t) -> p kk t", kk=8), eng="act")

def ssd_mixer(S, k, b, layer, src_fn, dst_fn):
    with ExitStack() as st_outer:
        S.stack = st_outer
        hT = S.sb("hT", [128, 8, TOK], BF16)
        with ExitStack() as st:
            S.stack = st
            norm_T(S, k, b, layer, 1, src_fn, hT, [(i, i * 128) for i in range(NT)], S.pool([0, 1, 2, 3]))
            S.phase_end()
        with ExitStack() as st:
            S.stack = st
            ssd_phase_a(S, k, hT)
            S.phase_end()
        for d in (0, 1):
            with ExitStack() as st:
                S.stack = st
                ssd_scan(S, k, b, layer, hT, d, src_fn, dst_fn)
                S.phase_end()
        S.stack = st_outer
    with ExitStack() as st:
        S.stack = st
        ssd_phase_d(S, k, b, layer, src_fn, dst_fn)
        S.phase_end()
    S.stack = None


def ssd_phase_a(S, k, hT):
    stg = S.sb("cstg", [6, 4096], F32)
    S.dma(stg[0:5, :], k.ssd_conv_w[0])
    S.dma(stg[5:6, :], View(k.ssd_conv_b.t[0:1, :], k.ssd_conv_b.res))
    cw = S.sb("cw", [128, 32, 8], F32)
    pp = S.pool([0, 1, 2, 3])
    for cc0 in range(0, 32, 8):
        ps = pp.next()
        for j in range(8):
            cc = cc0 + j
            S.tr(ps[:, j * 8:j * 8 + 6], stg[0:6, cc * 128:(cc + 1) * 128], k.identf[0:6, 0:6])
        S.copy(cw[:, cc0:cc0 + 8, :], ps[:, 0:64].rearrange("p (j e) -> p j e", e=8))
    wrot = S.rot("wA", [128, 8, 512], BF16, 2)
    urot = S.rot("uA", [128, TOK], F32, 2)
    arot = S.rot("accA", [128, TOK], F32, 2)
    crot = S.rot("cvA", [128, TOK], BF16, 3)
    trot = S.rot("tmA", [128, 8, 128], BF16, 3)
    pp2 = S.pool([4, 5, 6, 7])
    blocks = [(0, 256), (256, 512), (768, 512), (1280, 512), (1792, 512)]
    segs = [(0, 256), (256, TOK)]
    w = None
    prev = None

    def tail(cc, cv):
        if cc >= 16:
            S.dma(View(k.bc_d.t[cc - 16], k.bc_d.res), cv.all())
        if cc < 24:
            for i0 in range(0, NT, 8):
                n = min(8, NT - i0)
                ps = pp2.next()
                pv = pbf(ps)
                for j in range(n):
                    S.tr(pv[:, j * 128:(j + 1) * 128], cv[:, (i0 + j) * 128:(i0 + j + 1) * 128], k.identb.all())
                tm = trot.next()
                S.copy(tm[:, 0:n, :], pv[:, 0:n * 128].rearrange("p (j c) -> p j c", j=n))
                S.dma(View(k.xtm_d.t[i0:i0 + n, :, cc * 128:(cc + 1) * 128].rearrange("i t c -> t i c"), k.xtm_d.res),
                      tm[:, 0:n, :])

    for cc in range(32):
        if cc % 4 == 0:
            w = wrot.next()
            c0 = 2048 + cc * 128
            S.dma(w.all(), View(k.ssd_in_w.t[0].rearrange("(kk p) n -> p kk n", p=128)[:, :, c0:c0 + 512], k.ssd_in_w.res),
                  eng="pool")
        u = urot.next()
        for (t0, n) in blocks:
            ps = pp.next()
            for kk in range(8):
                S.mm(ps[:, 0:n], w[:, kk, (cc % 4) * 128:(cc % 4 + 1) * 128], hT[:, kk, t0:t0 + n],
                     start=(kk == 0), stop=(kk == 7))
            S.copy(u[:, t0:t0 + n], ps[:, 0:n], eng="act")
        acc = arot.next()
        for (a, e) in segs:
            S.ts(acc[:, a:e], u[:, a:e], cw[:, cc, 2:3], ALU.mult, s2=cw[:, cc, 5:6], op1=ALU.add)
            for tap in (0, 1, 3, 4):
                dl = tap - 2
                lo = a + max(0, -dl)
                hi = e - max(0, dl)
                S.stt(acc[:, lo:hi], u[:, lo + dl:hi + dl], cw[:, cc, tap:tap + 1], acc[:, lo:hi], ALU.mult, ALU.add)
        cv = crot.next()
        S.act(cv.all(), acc.all(), AF.Silu)
        if prev is not None:
            tail(*prev)
        prev = (cc, cv)
    tail(*prev)


def ssd_scan(S, k, b, layer, hT, d, src_fn, dst_fn):
    fwd = (d == 0)
    dc = d * 32
    wdt = S.sb("wdt", [128, 8, 64], BF16)
    S.dma(wdt.all(), View(k.ssd_in_w.t[0].rearrange("(kk p) n -> p kk n", p=128)[:, :, 6144:6208], k.ssd_in_w.res), eng="pool")
    dtb = S.sb("dtb", [128, 64], F32)
    load_bc(S, dtb.all(), k.ssd_dt_bias, k.ssd_dt_bias.t[0].rearrange("a h -> (a h)"))
    Abc = S.sb("Abc", [128, 64], F32)
    load_bc(S, Abc.all(), k.ssd_A_log, k.ssd_A_log.t[0].rearrange("a h -> (a h)"))
    S.act(Abc.all(), Abc.all(), AF.Exp)
    S.ts(Abc.all(), Abc.all(), -1.0, ALU.mult)
    h = S.sb("hst", [128, NG, 256], F32, nslots=NG)
    hb = S.sb("hstb", [128, NG, 256], BF16, nslots=NG)
    S.memset(h.all(), 0.0)
    S.memset(hb.all(), 0.0)
    if not fwd:
        wz = S.sb("wz", [128, 8, 2048], BF16)
        for q in range(4):
            S.dma(wz[:, :, q * 512:(q + 1) * 512],
                  View(k.ssd_in_w.t[0].rearrange("(kk p) n -> p kk n", p=128)[:, :, q * 512:(q + 1) * 512], k.ssd_in_w.res), eng="pool")
        Dbc = S.sb("Dbc", [128, 32], F32)
        load_bc(S, Dbc.all(), k.ssd_D, k.ssd_D.t[0])
        nwbc = S.sb("snw", [128, 2048], F32)
        load_bc(S, nwbc.all(), k.ssd_norm_w, k.ssd_norm_w.t[0])
        yfrot = S.rot("yf", [128, 2048], F32, 1)
        szrot = S.rot("sz", [128, 2048], F32, 1)
        gnrot = S.rot("gn", [128, 2048], BF16, 1)
    xbrot = S.rot("xB", [128, 3072], BF16, 2)
    bcrot = S.rot("bct", [128, 16, 128], BF16, 2)
    ydrot = S.rot("ydir", [128, 2048], F32, 2)
    xdtrot = S.rot("xdt", [128, 2048], BF16, 2)
    xddrot = S.rot("xdd", [128, 2048], BF16, 2)
    smrot = S.rot("ssm", [128, 8, 32], F32, 2)
    lharot = S.rot("lha", [128, 32, 128], BF16, 2)
    cbrot = S.rot("cbm", [128, 128], BF16, 4)
    sgrot = S.rot("seg", [128, 512], BF16, 4)
    mtrot = S.rot("MT", [128, 4, 128], BF16, 4)
    t3rot = S.rot("t3", [128, 256], F32, 6)
    ppA = S.pool([0, 1, 2, 3] if fwd else [0, 1, 2])
    ppB = S.pool([3])
    ppY = S.pool([4, 5, 6, 7])
    U = k.U_le if fwd else k.L_ge
    Uf = k.U_le_f if fwd else k.L_ge_f
    LM = k.SLt if fwd else k.SUt
    order = list(range(NT)) if fwd else [1, 0] + list(range(NT - 1, 1, -1))

    def prologue(i):
        c = {}
        c["i"] = i
        xB = c["xB"] = xbrot.next()
        S.dma(xB.all(), View(k.xtm_d.t[i], k.xtm_d.res))
        bct = c["bct"] = bcrot.next()
        S.dma(bct.all(), View(k.bc_d.t[:, :, i * 128:(i + 1) * 128].rearrange("c n t -> n c t"), k.bc_d.res))
        sm = c["sm"] = smrot.next()
        ps = ppA.next()
        for kk in range(8):
            S.mm(ps[:, 0:64], hT[:, kk, i * 128:(i + 1) * 128], wdt[:, kk, :], start=(kk == 0), stop=(kk == 7))
        S.tt(sm[:, 0, :], ps[:, dc:dc + 32], dtb[:, dc:dc + 32], ALU.add)
        S.act(sm[:, 1, :], sm[:, 0, :], AF.Exp)
        S.act(sm[:, 2, :], sm[:, 1, :], AF.Ln, bias=1.0)
        S.tt(sm[:, 3, :], sm[:, 2, :], Abc[:, dc:dc + 32], ALU.mult)
        lha = c["lha"] = lharot.next()
        S.tt(lha.all(), LM.all().bc(1, 32), sm[:, 3, :].bc(2, 128), ALU.mult)
        ps2 = ppA.next()
        S.mm(ps2[:, 0:32], Uf.all(), sm[:, 3, :])
        S.mm(ps2[:, 32:64], k.ones_f.all(), sm[:, 3, :])
        S.copy(sm[:, 4:6, :], ps2[:, 0:64].rearrange("p (a h) -> p a h", a=2))
        S.act(sm[:, 6, :], sm[:, 4, :], AF.Exp)
        S.tt(sm[:, 0, :], sm[:, 5, :], sm[:, 4, :], ALU.subtract)
        S.act(sm[:, 7, :], sm[:, 0, :], AF.Exp)
        S.act(sm[:, 1, :], sm[:, 5, :], AF.Exp)
        S.tt(sm[:, 0, :], sm[:, 2, :], sm[:, 7, :], ALU.mult)
        xdt = c["xdt"] = xdtrot.next()
        xdd = c["xdd"] = xddrot.next()
        x3 = c["x3"] = xB[:, 0:2048].rearrange("p (h j) -> p h j", h=32)
        S.tt(xdt.all().rearrange("p (h j) -> p h j", h=32), x3, sm[:, 2, :].bc(2, 64), ALU.mult)
        S.tt(xdd.all().rearrange("p (h j) -> p h j", h=32), x3, sm[:, 0, :].bc(2, 64), ALU.mult)
        c["ydir"] = ydrot.next()
        return c

    def stA(c, g):
        bct = c["bct"]
        BT = bct[:, g, :]
        CT = bct[:, 8 + g, :]
        pc = ppA.next()
        S.mm(pc[:, 0:128], BT, CT)
        pD = ppA.next()
        for r in range(4):
            S.mm(pD[:, r * 128:(r + 1) * 128], c["lha"][:, 4 * g + r, :], U.all())
        c[("a", g)] = (pc, pD, CT)

    def stB(c, g):
        pc, pD, CT = c[("a", g)]
        cbm = cbrot.next()
        S.tt(cbm.all(), pc[:, 0:128], U.all(), ALU.mult)
        seg = sgrot.next()
        S.act(seg.all(), pD.all(), AF.Exp)
        c[("f", g)] = (cbm, seg, CT)

    def stC1(c, g):
        cbm, seg, CT = c[("f", g)]
        MT = mtrot.next()
        S.tt(MT.all(), seg.all().rearrange("p (r l) -> p r l", r=4), cbm.all().bc(1, 4), ALU.mult)
        c[("m", g)] = MT

    def stC2(c, g):
        cbm, seg, CT = c[("f", g)]
        MT = c[("m", g)]
        xB, xdt, xdd = c["xB"], c["xdt"], c["xdd"]
        py = ppY.next()
        for r in range(4):
            hh = 4 * g + r
            S.mm(py[:, r * 64:(r + 1) * 64], MT[:, r, :], xdt[:, hh * 64:(hh + 1) * 64])
        S.mm(py[:, 256:512], CT, hb.s(g))
        pst = ppY.next()
        S.mm(pst[:, 0:256], xB[:, 2048 + g * 128:2048 + (g + 1) * 128], xdd[:, g * 256:(g + 1) * 256])
        c[("p", g)] = (py, pst)

    def stE(c, g):
        py, pst = c[("p", g)]
        sm, ydir = c["sm"], c["ydir"]
        t3 = t3rot.next()
        S.tt(t3.all().rearrange("p (r j) -> p r j", r=4), py[:, 256:512].rearrange("p (r j) -> p r j", r=4),
             sm[:, 6, 4 * g:4 * g + 4].bc(2, 64), ALU.mult)
        S.tt(ydir[:, g * 256:(g + 1) * 256], t3.all(), py[:, 0:256], ALU.add)
        t4 = t3rot.next()
        S.tt(t4.all().rearrange("p (r j) -> p r j", r=4), h.s(g).rearrange("p (r j) -> p r j", r=4),
             sm[:, 1, 4 * g:4 * g + 4].bc(2, 64), ALU.mult)
        S.tt(h.s(g), t4.all(), pst[:, 0:256], ALU.add)
        S.copy(hb.s(g), h.s(g), eng="act")

    def epilogue(c):
        i = c["i"]
        ydir = c["ydir"]
        if fwd:
            S.dma(View(k.yf_d.t[i], k.yf_d.res), ydir.all())
            return
        yf = yfrot.next()
        S.dma(yf.all(), View(k.yf_d.t[i], k.yf_d.res))
        S.tt(ydir.all(), ydir.all(), yf.all(), ALU.add)
        S.tt(yf.all().rearrange("p (h j) -> p h j", h=32), c["x3"], Dbc.all().bc(2, 64), ALU.mult)
        S.tt(ydir.all(), ydir.all(), yf.all(), ALU.add)
        sz = szrot.next()
        for q in range(4):
            pz = ppB.next()
            for kk in range(8):
                S.mm(pz.all(), hT[:, kk, i * 128:(i + 1) * 128], wz[:, kk, q * 512:(q + 1) * 512],
                     start=(kk == 0), stop=(kk == 7))
            S.act(sz[:, q * 512:(q + 1) * 512], pz.all(), AF.Silu)
        S.tt(ydir.all(), ydir.all(), sz.all(), ALU.mult)
        rs = k.small.next()
        rms_rstd(S, k, rs[:, 2:3], ydir.all(), 2048, sz.all())
        gn = gnrot.next()
        S.stt(gn.all(), ydir.all(), rs[:, 2:3], nwbc.all(), ALU.mult, ALU.mult)
        S.dma(View(k.gn_d.t[i], k.gn_d.res), gn.all())

    ctxs = {0: prologue(order[0])}
    n = len(order) * NG
    for st_ in range(n + 2):
        if st_ < n:
            ci, g = divmod(st_, NG)
            if g == 3 and ci + 1 < len(order):
                ctxs[ci + 1] = prologue(order[ci + 1])
            stA(ctxs[ci], g)
        if 1 <= st_ <= n:
            ci, g = divmod(st_ - 1, NG)
            stC1(ctxs[ci], g)
        if st_ < n:
            ci, g = divmod(st_, NG)
            stB(ctxs[ci], g)
        if 1 <= st_ <= n:
            ci, g = divmod(st_ - 1, NG)
            stC2(ctxs[ci], g)
        if st_ >= 2:
            ci, g = divmod(st_ - 2, NG)
            stE(ctxs[ci], g)
            if g == NG - 1:
                epilogue(ctxs[ci])
                del ctxs[ci]


def ssd_phase_d(S, k, b, layer, src_fn, dst_fn):
    wo = S.sb("wo", [128, 16, 1024], BF16)
    for q in range(4):
        S.dma(wo[:, q * 4:(q + 1) * 4, :],
              View(k.ssd_out_w.t[0].rearrange("(kk p) n -> p kk n", p=128)[:, q * 4:(q + 1) * 4, :], k.ssd_out_w.res), eng="pool")
    g1 = {}
    for kind in (0, 1):
        g1[kind] = S.sb("g1", [128, 1024], F32)
        load_mod(S, k, layer, 2 if kind == 0 else b, 2, g1[kind].all())
    gnrot = S.rot("gnD", [128, 2048], BF16, 2)
    gTrot = S.rot("gT", [128, 16, 128], BF16, 2)
    xrrot = S.rot("xr", [128, 1024], F32, 2)
    xorot = S.rot("xo", [128, 1024], F32, 2)
    ppB = S.pool([0, 1, 2, 3])
    ppC = S.pool([4, 5, 6, 7])
    for i in range(NT):
        kind = 0 if i < 2 else 1
        gn = gnrot.next()
        S.dma(gn.all(), View(k.gn_d.t[i], k.gn_d.res))
        gT = gTrot.next()
        for hf in range(2):
            pt = ppB.next()
            pv = pbf(pt)
            for j in range(8):
                c = hf * 8 + j
                S.tr(pv[:, j * 128:(j + 1) * 128], gn[:, c * 128:(c + 1) * 128], k.identb.all())
            S.copy(gT[:, hf * 8:(hf + 1) * 8, :], pv.rearrange("p (j t) -> p j t", j=8), eng="act")
        xr = xrrot.next()
        S.dma(xr.all(), src_fn(i))
        xo = xorot.next()
        for hf in range(2):
            po = ppC.next()
            for c in range(16):
                S.mm(po.all(), gT[:, c, :], wo[:, c, hf * 512:(hf + 1) * 512], start=(c == 0), stop=(c == 15))
            S.tt(xo[:, hf * 512:(hf + 1) * 512], po.all(), g1[kind][:, hf * 512:(hf + 1) * 512], ALU.mult)
        S.tt(xo.all(), xo.all(), xr.all(), ALU.add)
        S.dma(dst_fn(i), xo.all())

NE = 16
U32 = mybir.dt.uint32


def moe_layer(S, k, layer, xs_fn):
    has_ctx = (layer == 0)
    ngrp = 2 if has_ctx else 1
    GNT = [16, 2]
    GCAP = [256, 32]
    GTI0 = [2, 0]
    NSLOT = 512 + (64 if has_ctx else 0)
    with ExitStack() as st_outer:
        S.stack = st_outer
        slT = [[S.sb(f"slT{g}{b}", [16, GNT[g] * 128], BF16) for b in range(NB)] for g in range(ngrp)]
        with ExitStack() as st_mid:
            S.stack = st_mid
            hn = [[S.sb(f"hn{g}{b}", [128, GNT[g], 1024], BF16) for b in range(NB)] for g in range(ngrp)]
            sl = [S.sb(f"sl{g}", [128, NB, GNT[g], NE], F32) for g in range(ngrp)]
            affhl = [S.sb(f"affhl{g}", [128, NB, GNT[g], NE, 2], BF16) for g in range(ngrp)]
            with ExitStack() as st:
                S.stack = st
                moe_route(S, k, layer, xs_fn, ngrp, GNT, GCAP, GTI0, hn, sl, affhl, slT)
                S.phase_end()
            with ExitStack() as st:
                S.stack = st
                moe_experts(S, k, layer, ngrp, GNT, hn, sl, affhl, NSLOT)
                S.phase_end()
        with ExitStack() as st:
            S.stack = st
            moe_scatter(S, k, layer, xs_fn, ngrp, GNT, GTI0, slT)
            S.phase_end()
    S.stack = None


def moe_route(S, k, layer, xs_fn, ngrp, GNT, GCAP, GTI0, hn, sl, affhl, slT):
    rw = S.sb("rw", [128, 8, NE], F32)
    S.dma(rw.all(), View(k.moe_router_w.t[layer].rearrange("(kk p) e -> p kk e", p=128), k.moe_router_w.res))
    aff = [S.sb(f"aff{g}", [128, NB, GNT[g], NE], F32) for g in range(ngrp)]
    xrot = S.rot("mx", [128, 1024], F32, 3)
    trot = S.rot("mtmp", [128, 1024], F32, 2)
    hfrot = S.rot("mhf", [128, 1024], F32, 2)
    hTrot = S.rot("mhfT", [128, 8, 128], F32, 2)
    erot = S.rot("mexp", [128, NE], F32, 3)
    ppA = S.pool([0, 1, 2, 3])
    ppB = S.pool([4, 5])
    ppC = S.pool([6, 7])
    for g in range(ngrp):
        for b in range(NB):
            A = S.sb("m2A", [128, 1024], F32)
            sh = S.sb("m2S", [128, 1024], F32)
            row = 2 if g == 1 else b
            load_mod(S, k, layer, row, 4, A.all())
            load_mod(S, k, layer, row, 3, sh.all())
            for j in range(GNT[g]):
                ti = GTI0[g] + j
                xt = xrot.next()
                S.dma(xt.all(), xs_fn(b)(ti))
                tmp = trot.next()
                rs = k.small.next()
                rms_rstd(S, k, rs[:, 2:3], xt.all(), 1024, tmp.all())
                S.stt(tmp.all(), xt.all(), rs[:, 2:3], A.all(), ALU.mult, ALU.mult)
                hf = hfrot.next()
                S.tt(hf.all(), tmp.all(), sh.all(), ALU.add)
                S.copy(hn[g][b][:, j, :], hf.all(), eng="act")
                hfT = hTrot.next()
                for q in range(2):
                    ps = ppA.next()
                    for c in range(4):
                        kk = q * 4 + c
                        S.tr(ps[:, c * 128:(c + 1) * 128], hf[:, kk * 128:(kk + 1) * 128], k.identf.all())
                    S.copy(hfT[:, q * 4:(q + 1) * 4, :], ps.all().rearrange("p (c t) -> p c t", c=4), eng="act")
                pl = ppB.next()
                for kk in range(8):
                    S.mm(pl[:, 0:NE], hfT[:, kk, :], rw[:, kk, :], start=(kk == 0), stop=(kk == 7))
                sm = k.small.next()
                S.reduce(sm[:, 0:1], pl[:, 0:NE], ALU.max)
                S.ts(sm[:, 1:2], sm[:, 0:1], -1.0, ALU.mult)
                ex = erot.next()
                S.act(ex.all(), pl[:, 0:NE], AF.Exp, bias=sm[:, 1:2], accum_out=sm[:, 2:3])
                S.recip(sm[:, 3:4], sm[:, 2:3])
                S.ts(aff[g][:, b, j, :], ex.all(), sm[:, 3:4], ALU.mult)
    lo = [S.sb(f"lo{g}", [128, NB, NE], F32) for g in range(ngrp)]
    hi = [S.sb(f"hi{g}", [128, NB, NE], F32) for g in range(ngrp)]
    mid = [S.sb(f"mid{g}", [128, NB, NE], F32) for g in range(ngrp)]
    cnt = [S.sb(f"cnt{g}", [128, NB, NE], F32) for g in range(ngrp)]
    selp = [S.sb(f"selp{g}", [128, NB, NE], F32) for g in range(ngrp)]
    seln = [S.sb(f"seln{g}", [128, NB, NE], F32) for g in range(ngrp)]
    ge = [S.sb(f"ge{g}", [128, NB, GNT[g], NE], BF16) for g in range(ngrp)]
    for g in range(ngrp):
        S.memset(lo[g].all(), 0.0)
        S.memset(hi[g].all(), 1.5)
    for it in range(31):
        for g in range(ngrp):
            nt = GNT[g]
            n = NB * nt * NE
            S.tt(mid[g].all(), lo[g].all(), hi[g].all(), ALU.add)
            S.ts(mid[g].all(), mid[g].all(), 0.5, ALU.mult)
            S.tt(ge[g].all(), aff[g].all(), mid[g].all().bc(2, nt), ALU.is_ge)
            pc = ppC.next()
            S.mm(pc[:, 0:n], k.ones_b.all(), ge[g].all().rearrange("p s t e -> p (s t e)"))
            S.reduce(cnt[g].all(), pc[:, 0:n].rearrange("p (s t e) -> p s e t", s=NB, t=nt), ALU.add)
            S.ts(selp[g].all(), cnt[g].all(), float(GCAP[g]), ALU.is_ge)
            S.ts(seln[g].all(), cnt[g].all(), float(GCAP[g]), ALU.is_lt)
            S.emit("dve", lambda e, g=g: e.copy_predicated(lo[g].t[:], selp[g].t[:].bitcast(U32), mid[g].t[:]),
                   reads=[selp[g].all(), mid[g].all()], writes=[lo[g].all()])
            S.emit("dve", lambda e, g=g: e.copy_predicated(hi[g].t[:], seln[g].t[:].bitcast(U32), mid[g].t[:]),
                   reads=[seln[g].all(), mid[g].all()], writes=[hi[g].all()])
    for g in range(ngrp):
        nt = GNT[g]
        n = NB * nt * NE
        cap = GCAP[g]
        S.tt(ge[g].all(), aff[g].all(), lo[g].all().bc(2, nt), ALU.is_ge)
        gef = ge[g].all().rearrange("p s t e -> p (s t e)")
        pr = ppC.next()
        S.mm(pr[:, 0:n], k.SUt.all(), gef)
        ptot = ppC.next()
        S.mm(ptot[:, 0:n], k.ones_b.all(), gef)
        tot = S.sb(f"tot{g}", [128, NB, nt, NE], F32)
        S.copy(tot.all(), ptot[:, 0:n].rearrange("p (s t e) -> p s t e", s=NB, t=nt))
        off = S.sb(f"off{g}", [128, NB, nt, NE], F32)
        S.memset(off[:, :, 0, :], 0.0)
        for j in range(1, nt):
            S.tt(off[:, :, j, :], off[:, :, j - 1, :], tot[:, :, j - 1, :], ALU.add)
        rank = S.sb(f"rank{g}", [128, NB, nt, NE], F32)
        S.tt(rank.all(), pr[:, 0:n].rearrange("p (s t e) -> p s t e", s=NB, t=nt), off.all(), ALU.add)
        ok = S.sb(f"ok{g}", [128, NB, nt, NE], F32)
        S.ts(ok.all(), rank.all(), float(cap), ALU.is_lt)
        S.tt(ok.all(), ok.all(), ge[g].all(), ALU.mult)
        for b in range(NB):
            offs = 1.0 + (32.0 * b if g == 1 else 0.0)
            S.stt(sl[g][:, b], rank[:, b], offs, ok[:, b], ALU.add, ALU.mult)
        S.ts(sl[g].all(), sl[g].all(), -1.0, ALU.add)
        for b in range(NB):
            for j0 in range(0, nt, 4):
                m = min(4, nt - j0)
                ps = ppA.next()
                for c in range(m):
                    S.tr(ps[0:NE, c * 128:(c + 1) * 128], sl[g][:, b, j0 + c, :], k.identf.all())
                S.copy(slT[g][b][:, j0 * 128:(j0 + m) * 128], ps[0:NE, 0:m * 128])
        S.copy(affhl[g][:, :, :, :, 0], aff[g].all())
        S.tt(affhl[g][:, :, :, :, 1], aff[g].all(), affhl[g][:, :, :, :, 0], ALU.subtract)


def moe_experts(S, k, layer, ngrp, GNT, hn, sl, affhl, NSLOT):
    has_ctx = ngrp == 2
    wrot = S.rot("mw", [128, 8, 1024], BF16, 4)
    xgT = S.sb("xgT", [128, 8, NSLOT], BF16)
    hT = S.sb("mhT", [128, 8, NSLOT], BF16)
    serot = S.rot("Se", [128, 16, 256], BF16, 2)
    secrot = S.rot("SeC", [128, NB, 2, 64], BF16, 2)
    sarot = S.rot("msa", [128, 512], F32, 2)
    yrot = S.rot("my", [128, 1024], BF16, 2)
    gates = S.rot("mgate", [128, 8], F32, 2)
    ppA = S.pool([0, 1, 2])
    ppB = S.pool([3, 4, 5])
    ppG = S.pool([6, 7])
    nchunks = [(0, 512)] + ([(512, 64)] if has_ctx else [])
    jchunks = [(0, 128), (128, 128), (256, 128), (384, 128)] + ([(512, 64)] if has_ctx else [])
    wsrc = {1: k.moe_w1, 3: k.moe_w3, 2: k.moe_w2}

    def loadw(which, e):
        w = wrot.next()
        src = wsrc[which]
        for q in range(2):
            S.dma(w[:, q * 4:(q + 1) * 4, :],
                  View(src.t[layer, e].rearrange("(kk p) n -> p kk n", p=128)[:, q * 4:(q + 1) * 4, :], src.res), eng="pool")
        return w

    for e in range(NE):
        w1 = loadw(1, e)
        w3 = loadw(3, e)
        gt = gates.next()
        pgt = ppG.next()
        for b in range(NB):
            Se = serot.next()
            for tt_ in range(16):
                S.ts(Se[:, tt_, :], k.iota[:, 0:256], sl[0][:, b, tt_, e:e + 1], ALU.is_equal)
            for kk0 in range(0, 8, 2):
                pg = ppA.next()
                for c in range(2):
                    kk = kk0 + c
                    for tt_ in range(16):
                        S.mm(pg[:, c * 256:(c + 1) * 256], hn[0][b][:, tt_, kk * 128:(kk + 1) * 128], Se[:, tt_, :],
                             start=(tt_ == 0), stop=(tt_ == 15))
                S.copy(xgT[:, kk0:kk0 + 2, b * 256:(b + 1) * 256], pg.all().rearrange("p (c j) -> p c j", c=2), eng="act")
            for jc in range(2):
                col = (b * 2 + jc) * 2
                for tt_ in range(16):
                    S.mm(pgt[:, col:col + 2], Se[:, tt_, jc * 128:(jc + 1) * 128], affhl[0][:, b, tt_, e, :],
                         start=(tt_ == 0), stop=(tt_ == 15))
        if has_ctx:
            SeC = secrot.next()
            for b in range(NB):
                for tt_ in range(2):
                    S.ts(SeC[:, b, tt_, :], k.iota[:, 0:64], sl[1][:, b, tt_, e:e + 1], ALU.is_equal)
            for kk0 in range(0, 8, 4):
                pg = ppA.next()
                for c in range(4):
                    kk = kk0 + c
                    n = 0
                    for b in range(NB):
                        for tt_ in range(2):
                            S.mm(pg[:, c * 64:(c + 1) * 64], hn[1][b][:, tt_, kk * 128:(kk + 1) * 128], SeC[:, b, tt_, :],
                                 start=(n == 0), stop=(n == 3))
                            n += 1
                S.copy(xgT[:, kk0:kk0 + 4, 512:576], pg[:, 0:256].rearrange("p (c j) -> p c j", c=4), eng="act")
            n = 0
            for b in range(NB):
                for tt_ in range(2):
                    S.mm(pgt[0:64, 8:10], SeC[:, b, tt_, :], affhl[1][:, b, tt_, e, :], start=(n == 0), stop=(n == 3))
                    n += 1
        ncol = 5 if has_ctx else 4
        S.reduce(gt[:, 0:ncol], pgt[:, 0:2 * ncol].rearrange("p (c two) -> p c two", two=2), ALU.add)
        for fc in range(8):
            for (n0, n) in nchunks:
                pa = ppA.next()
                for kk in range(8):
                    S.mm(pa[:, 0:n], w1[:, kk, fc * 128:(fc + 1) * 128], xgT[:, kk, n0:n0 + n], start=(kk == 0), stop=(kk == 7))
                pb = ppB.next()
                for kk in range(8):
                    S.mm(pb[:, 0:n], w3[:, kk, fc * 128:(fc + 1) * 128], xgT[:, kk, n0:n0 + n], start=(kk == 0), stop=(kk == 7))
                sa = sarot.next()
                S.act(sa[:, 0:n], pa[:, 0:n], AF.Silu)
                S.tt(hT[:, fc, n0:n0 + n], sa[:, 0:n], pb[:, 0:n], ALU.mult)
        w2 = loadw(2, e)
        for ji, (j0, m) in enumerate(jchunks):
            y = yrot.next()
            for hf in range(2):
                py = ppB.next()
                for fc in range(8):
                    S.mm(py[0:m, :], hT[:, fc, j0:j0 + m], w2[:, fc, hf * 512:(hf + 1) * 512], start=(fc == 0), stop=(fc == 7))
                S.act(y[0:m, hf * 512:(hf + 1) * 512], py[0:m, :], AF.Copy, scale=gt[0:m, ji:ji + 1])
            S.dma(View(k.y_d.t[e, j0:j0 + m, :], k.y_d.res), y[0:m, :])


def moe_scatter(S, k, layer, xs_fn, ngrp, GNT, GTI0, slT):
    sel16 = S.sb("sel16", [16, NE, 128], BF16)
    selio = S.sb("selio", [16, NE, 128], F32)
    S.emit("pool", lambda e: e.iota(selio.t[:], [[1, NE], [0, 128]], base=0, channel_multiplier=0,
                                    allow_small_or_imprecise_dtypes=True), writes=[selio.all()])
    S.ts(sel16.all(), selio.all(), k.pid[0:16, :], ALU.is_equal)
    pid32 = S.sb("pid32", [128, 1], F32)
    S.ts(pid32.all(), k.pid.all(), 32.0, ALU.add)
    yall = S.sb("yall", [128, 2 * NE, 1024], BF16)
    strot = S.rot("ST", [128, 2, NE, 128], BF16, 2)
    xrot = S.rot("sx", [128, 1024], F32, 2)
    xorot = S.rot("sxo", [128, 1024], F32, 2)
    ppA = S.pool([0, 1, 2, 3])
    ppB = S.pool([4, 5, 6, 7])
    for g in range(ngrp):
        for b in range(NB):
            g2 = S.sb("g2v", [128, 1024], F32)
            load_mod(S, k, layer, 2 if g == 1 else b, 5, g2.all())
            if g == 0:
                kr = 128
                njc = 2
                for e in range(NE):
                    S.dma(yall[:, 2 * e:2 * e + 2, :],
                          View(k.y_d.t[e, b * 256:(b + 1) * 256, :].rearrange("(jc j) d -> j jc d", jc=2), k.y_d.res))
                cmpv = [k.pid, k.pid128]
            else:
                kr = 32
                njc = 1
                S.dma(yall[0:32, 0:NE, :],
                      View(k.y_d.t[:, 512 + b * 32:512 + (b + 1) * 32, :].rearrange("e j d -> j e d"), k.y_d.res))
                cmpv = [k.pid if b == 0 else pid32]
            def make_ST(j):
                banks = [ppA.next() for _ in range(4)]
                for e in range(NE):
                    S.mm(banks[e // 4][:, (e % 4) * 128:(e % 4 + 1) * 128], sel16[:, e, :], slT[g][b][:, j * 128:(j + 1) * 128])
                ST_ = strot.next()
                for jc in range(njc):
                    for q in range(4):
                        S.ts(ST_[:, jc, q * 4:(q + 1) * 4, :], banks[q].all().rearrange("p (e t) -> p e t", e=4),
                             cmpv[jc].all(), ALU.is_equal)
                return ST_

            ST_next = make_ST(0)
            for j in range(GNT[g]):
                ti = GTI0[g] + j
                ST = ST_next
                if j + 1 < GNT[g]:
                    ST_next = make_ST(j + 1)
                xt = xrot.next()
                S.dma(xt.all(), xs_fn(b)(ti))
                xo = xorot.next()
                for hf in range(2):
                    po = ppB.next()
                    n = 0
                    tot = NE * njc
                    for e in range(NE):
                        for jc in range(njc):
                            yi = 2 * e + jc if g == 0 else e
                            S.mm(po.all(), ST[0:kr, jc, e, :], yall[0:kr, yi, hf * 512:(hf + 1) * 512],
                                 start=(n == 0), stop=(n == tot - 1))
                            n += 1
                    S.tt(xo[:, hf * 512:(hf + 1) * 512], po.all(), g2[:, hf * 512:(hf + 1) * 512], ALU.mult)
                S.tt(xo.all(), xo.all(), xt.all(), ALU.add)
                S.dma(xs_fn(b)(ti), xo.all())

def attn_mixer(S, k, b, layer, xs):
    NLT = NT - 2
    with ExitStack() as st_outer:
        S.stack = st_outer
        qT = S.sb("qT", [128, 8, LAT], BF16)
        kTd = S.sb("kTd", [128, 4, TOK], BF16)
        vaug = S.sb("vaug", [128, NT, 4, 72], BF16)
        with ExitStack() as st_h:
            S.stack = st_h
            hT = S.sb("ahT", [128, 8, TOK], BF16)
            with ExitStack() as st:
                S.stack = st
                norm_T(S, k, b, layer, 1, xs, hT, [(i, i * 128) for i in range(NT)], S.pool([0, 1, 2, 3]))
                S.phase_end()
            with ExitStack() as st:
                S.stack = st
                attn_qkv(S, k, hT, qT, kTd, vaug)
                S.phase_end()
        with ExitStack() as st:
            S.stack = st
            attn_core(S, k, b, layer, xs, qT, kTd, vaug)
            S.phase_end()
    S.stack = None


def attn_qkv(S, k, hT, qT, kTd, vaug):
    w = S.sb("wqkv", [128, 8, 1536], BF16)
    for q in range(3):
        S.dma(w[:, :, q * 512:(q + 1) * 512],
              View(k.attn_qkv_w.t[0].rearrange("(kk p) n -> p kk n", p=128)[:, :, q * 512:(q + 1) * 512], k.attn_qkv_w.res), eng="pool")
    qw = S.sb("qwbc", [128, 64], F32)
    kw = S.sb("kwbc", [128, 64], F32)
    load_bc(S, qw.all(), k.attn_q_norm_w, k.attn_q_norm_w.t[0])
    load_bc(S, kw.all(), k.attn_k_norm_w, k.attn_k_norm_w.t[0])
    S.memset(vaug.all(), 1.0)
    qkrot = S.rot("qk", [128, 20, 64], F32, 2)
    sqrot = S.rot("qsq", [128, 20, 64], F32, 1)
    qnrot = S.rot("qn", [128, 20, 64], F32, 2)
    qrrot = S.rot("qr", [128, 20, 64], BF16, 3)
    kdrot = S.rot("kd", [128, 4, 2, 64], BF16, 2)
    csrot = S.rot("cs", [128, 2, 32], F32, 2)
    r1rot = S.rot("rp1", [128, 20, 32], F32, 2)
    r2rot = S.rot("rp2", [128, 20, 32], F32, 2)
    ssrot = S.rot("qss", [128, 20], F32, 3)
    ppA = S.pool([0, 1, 2, 3, 4, 5])
    ppB = S.pool([6, 7])
    prev = None

    def tail(i, qr):
        if i >= 2:
            ps = ppB.next()
            pv = pbf(ps)
            for pr in range(8):
                S.tr(pv[:, pr * 128:(pr + 1) * 128], qr[:, 2 * pr:2 * pr + 2, :].rearrange("p h d -> p (h d)"), k.identb.all())
            S.copy(qT[:, :, (i - 2) * 128:(i - 1) * 128], pv.rearrange("p (c t) -> p c t", c=8), eng="act")
        kd = kdrot.next()
        S.copy(kd[:, :, 0, :], qr[:, 16:20, :])
        S.copy(kd[:, :, 1, :], qr[:, 16:20, :], eng="act")
        ps = ppB.next()
        pv = pbf(ps)
        for kv in range(4):
            S.tr(pv[:, kv * 128:(kv + 1) * 128], kd[:, kv].rearrange("p a d -> p (a d)"), k.identb.all())
        S.copy(kTd[:, :, i * 128:(i + 1) * 128], pv[:, 0:512].rearrange("p (c t) -> p c t", c=4))

    for i in range(NT):
        banks = []
        for nb_ in range(3):
            ps = ppA.next()
            for kk in range(8):
                S.mm(ps.all(), hT[:, kk, i * 128:(i + 1) * 128], w[:, kk, nb_ * 512:(nb_ + 1) * 512], start=(kk == 0), stop=(kk == 7))
            banks.append(ps)
        qk = qkrot.next()
        S.copy(qk[:, 0:8, :], banks[0].all().rearrange("p (h d) -> p h d", h=8), eng="act")
        S.copy(qk[:, 8:16, :], banks[1].all().rearrange("p (h d) -> p h d", h=8), eng="act")
        S.copy(qk[:, 16:20, :], banks[2][:, 0:256].rearrange("p (h d) -> p h d", h=4), eng="act")
        S.copy(vaug[:, i, :, 0:64], banks[2][:, 256:512].rearrange("p (h d) -> p h d", h=4), eng="act")
        sq = sqrot.next()
        S.tt(sq.all(), qk.all(), qk.all(), ALU.mult)
        ss = ssrot.next()
        S.reduce(ss.all(), sq.all(), ALU.add)
        S.act(ss.all(), ss.all(), AF.Ln, bias=k.eps.all(), scale=1.0 / 64)
        S.act(ss.all(), ss.all(), AF.Exp, scale=-0.5)
        qn = qnrot.next()
        S.tt(qn.all(), qk.all(), ss.all().bc(2, 64), ALU.mult)
        S.tt(qn[:, 0:16, :], qn[:, 0:16, :], qw.all().bc(1, 16), ALU.mult)
        S.tt(qn[:, 16:20, :], qn[:, 16:20, :], kw.all().bc(1, 4), ALU.mult)
        qr = qrrot.next()
        if i >= 2:
            cs = csrot.next()
            t0 = (i - 2) * 128
            S.dma(cs[:, 0, :], View(k.cos.t[t0:t0 + 128, :], k.cos.res))
            S.dma(cs[:, 1, :], View(k.sin.t[t0:t0 + 128, :], k.sin.res))
            cosb = cs[:, 0, :].bc(1, 20)
            sinb = cs[:, 1, :].bc(1, 20)
            t1 = qn[:, :, 0:32]
            t2 = qn[:, :, 32:64]
            a1 = r1rot.next()
            a2 = r2rot.next()
            S.tt(a1.all(), t1, cosb, ALU.mult)
            S.tt(a2.all(), t2, sinb, ALU.mult)
            S.tt(qr[:, :, 0:32], a1.all(), a2.all(), ALU.subtract)
            a3 = r1rot.next()
            a4 = r2rot.next()
            S.tt(a3.all(), t2, cosb, ALU.mult)
            S.tt(a4.all(), t1, sinb, ALU.mult)
            S.tt(qr[:, :, 32:64], a3.all(), a4.all(), ALU.add)
        else:
            S.copy(qr.all(), qn.all())
        if prev is not None:
            tail(*prev)
        prev = (i, qr)
    tail(*prev)


def attn_core(S, k, b, layer, xs, qT, kTd, vaug):
    wo = S.sb("awo", [128, 8, 1024], BF16)
    for q in range(2):
        S.dma(wo[:, q * 4:(q + 1) * 4, :],
              View(k.attn_out_w.t[0].rearrange("(kk p) n -> p kk n", p=128)[:, q * 4:(q + 1) * 4, :], k.attn_out_w.res), eng="pool")
    g1 = S.sb("ag1", [128, 1024], F32)
    load_mod(S, k, layer, b, 2, g1.all())
    ptrot = S.rot("PT", [128, 512], BF16, 3)
    otrot = S.rot("otm", [128, 4, 1024], BF16, 2)
    oTrot = S.rot("aoT", [128, 8, 128], BF16, 2)
    xrot = S.rot("ax", [128, 1024], F32, 2)
    xorot = S.rot("axo", [128, 1024], F32, 2)
    ppS = S.pool([0, 1, 2, 3])
    ppAcc = S.pool([4, 5])
    ppO = S.pool([6, 7])
    for tqb in range(4):
        tq0 = tqb * 512
        otm = otrot.next()
        steps = [(h, tk) for h in range(16) for tk in range(NT)]

        def emit_scores(h, tk):
            pair, e = h // 2, h % 2
            kv = h // 4
            p0, p1 = e * 64, (e + 1) * 64
            psc = ppS.next()
            S.mm(psc.all(), kTd[p0:p1, kv, tk * 128:(tk + 1) * 128], qT[p0:p1, pair, tq0:tq0 + 512])
            return psc

        pending = emit_scores(*steps[0])
        acc = None
        for si, (h, tk) in enumerate(steps):
            kv = h // 4
            psc = pending
            if si + 1 < len(steps):
                pending = emit_scores(*steps[si + 1])
            if tk == 0:
                acc = ppAcc.next()
            PT = ptrot.next()
            S.act(PT.all(), psc.all(), AF.Exp, scale=0.125)
            for r in range(4):
                S.mm(acc[:, r * 128:r * 128 + 65], PT[:, r * 128:(r + 1) * 128], vaug[:, tk, kv, 0:65],
                     start=(tk == 0 and r == 0), stop=(tk == NT - 1 and r == 3))
            if tk == NT - 1:
                rc = k.small.next()
                a3 = acc.all().rearrange("p (r c) -> p r c", r=4)
                S.recip(rc[:, 0:4], a3[:, :, 64])
                S.tt(otm[:, :, h * 64:(h + 1) * 64], a3[:, :, 0:64], rc[:, 0:4].bc(2, 64), ALU.mult)
        for r in range(4):
            ti = 2 + tqb * 4 + r
            ps = ppO.next()
            pv = pbf(ps)
            for kk in range(8):
                S.tr(pv[:, kk * 128:(kk + 1) * 128], otm[:, r, kk * 128:(kk + 1) * 128], k.identb.all())
            oT = oTrot.next()
            S.copy(oT.all(), pv.rearrange("p (c t) -> p c t", c=8), eng="act")
            xt = xrot.next()
            S.dma(xt.all(), xs(ti))
            xo = xorot.next()
            for hf in range(2):
                po = ppO.next()
                for kk in range(8):
                    S.mm(po.all(), oT[:, kk, :], wo[:, kk, hf * 512:(hf + 1) * 512], start=(kk == 0), stop=(kk == 7))
                S.tt(xo[:, hf * 512:(hf + 1) * 512], po.all(), g1[:, hf * 512:(hf + 1) * 512], ALU.mult)
            S.tt(xo.all(), xo.all(), xt.all(), ALU.add)
            S.dma(xs(ti), xo.all())


def final_norm(S, k, xs_fn):
    with ExitStack() as st:
        S.stack = st
        fw = S.sb("fnw", [128, 1024], F32)
        load_bc(S, fw.all(), k.final_norm_w, k.final_norm_w.t)
        xrot = S.rot("fx", [128, 1024], F32, 3)
        jrot = S.rot("fj", [128, 1024], F32, 2)
        orot = S.rot("fo", [128, 1024], F32, 3)
        for b in range(NB):
            for j in range(NT - 2):
                xt = xrot.next()
                S.dma(xt.all(), xs_fn(b)(2 + j))
                rs = k.small.next()
                rms_rstd(S, k, rs[:, 2:3], xt.all(), 1024, jrot.next().all())
                xo = orot.next()
                S.stt(xo.all(), xt.all(), rs[:, 2:3], fw.all(), ALU.mult, ALU.mult)
                S.dma(View(k.out.t[b, j * 128:(j + 1) * 128, :], k.out.res), xo.all())
        S.phase_end()
    S.stack = None


WEIGHT_SPECS = [
    ("ada_w", [2, 1024, 6144]), ("ada_b", [2, 6144]), ("norm1_w", [2, 1024]), ("norm2_w", [2, 1024]),
    ("ssd_in_w", [1, 1024, 6208]), ("ssd_conv_w", [1, 5, 4096]), ("ssd_conv_b", [1, 4096]),
    ("ssd_dt_bias", [1, 2, 32]), ("ssd_A_log", [1, 2, 32]), ("ssd_D", [1, 32]), ("ssd_norm_w", [1, 2048]),
    ("ssd_out_w", [1, 2048, 1024]), ("attn_qkv_w", [1, 1024, 1536]), ("attn_q_norm_w", [1, 64]),
    ("attn_k_norm_w", [1, 64]), ("attn_out_w", [1, 1024, 1024]), ("moe_router_w", [2, 1024, 16]),
    ("moe_w1", [2, 16, 1024, 1024]), ("moe_w3", [2, 16, 1024, 1024]), ("moe_w2", [2, 16, 1024, 1024]),
    ("final_norm_w", [1024]),
]


def build_program(stop=None, dbg=False, only=None):
    nc = bass.Bass("TRN2", target_bir_lowering=False)
    k = K()
    with ExitStack() as st:
        S = Sched(nc, st)
        S.init_psum(8)

        def din(name, shape):
            t = nc.dram_tensor(name, list(shape), F32, kind="ExternalInput")
            return Tl(t.ap(), 1, name)

        k.x = din("x", [NB, LAT, D])
        k.c = din("c", [NB, D])
        k.ctx = din("ctx", [NB, CTXL, D])
        k.c_ctx = din("c_ctx", [D])
        for name, shape in WEIGHT_SPECS:
            setattr(k, name, din(name, shape))
        k.cos = din("rope_cos", [LAT, 32])
        k.sin = din("rope_sin", [LAT, 32])
        k.out = Tl(nc.dram_tensor("out", [NB, LAT, D], F32, kind="ExternalOutput").ap(), 1, "out")
        xkind = "ExternalOutput"
        k.xs = [Tl(nc.dram_tensor(f"xs{b}", [NT, 128, D], F32, kind=xkind).ap(), NT, f"xs{b}") for b in range(NB)]
        k.modv = [S.dram(f"modv{l}", [3, 6144], F32) for l in range(2)]
        k.xtm_d = S.dram("xtm_d", [NT, 128, 3072], BF16)
        k.bc_d = S.dram("bc_d", [16, 128, TOK], BF16)
        k.yf_d = S.dram("yf_d", [NT, 128, 2048], F32)
        k.gn_d = S.dram("gn_d", [NT, 128, 2048], BF16)
        k.y_d = S.dram("y_d", [16, 576, 1024], BF16)
        k.small = S.rot("small", [128, 4], F32, 12)
        setup_consts(S, k)
        S.phase_end()

        def in_src(b):
            def f(i):
                if i < 2:
                    return View(k.ctx.t[b, i * 128:(i + 1) * 128, :], k.ctx.res)
                return View(k.x.t[b, (i - 2) * 128:(i - 1) * 128, :], k.x.res)
            return f

        def xs_fn(b):
            def f(i):
                return View(k.xs[b].t[i], [k.xs[b].res[i]])
            return f

        stages = []
        stages.append(("mod0", lambda: phase_mod(S, k, 0)))
        for b in range(NB):
            stages.append((f"ssd{b}", lambda b=b: ssd_mixer(S, k, b, 0, in_src(b), xs_fn(b))))
        stages.append(("moe0", lambda: moe_layer(S, k, 0, xs_fn)))
        stages.append(("mod1", lambda: phase_mod(S, k, 1)))
        for b in range(NB):
            stages.append((f"attn{b}", lambda b=b: attn_mixer(S, k, b, 1, xs_fn(b))))
        stages.append(("moe1", lambda: moe_layer(S, k, 1, xs_fn)))
        stages.append(("final", lambda: final_norm(S, k, xs_fn)))
        if only is not None:
            with ExitStack() as st2:
                S.stack = st2
                rot = S.rot("initx", [128, D], F32, 3)
                for b in range(NB):
                    xi = din(f"xs_init{b}", [NT, 128, D])
                    for i in range(NT):
                        t = rot.next()
                        S.dma(t.all(), View(xi.t[i], xi.res))
                        S.dma(xs_fn(b)(i), t.all())
                S.phase_end()
        for name, fn in stages:
            if only is not None and name not in only:
                continue
            S.stack = st
            fn()
            if stop == name:
                break
        S.stack = st
        outs = [k.out.all()] + [x.all() for x in k.xs]
        S.finish(outs)
        k.ninst = S.ninst
    return nc, k


_ROPE = None


def rope_tables():
    global _ROPE
    if _ROPE is None:
        S_ = LAT
        row = np.repeat(np.arange(S_ // 64), 64).astype(np.float32)
        col = np.tile(np.arange(64), S_ // 64).astype(np.float32)
        inv = (np.float32(10000.0) ** (-np.arange(0, 32, 2, dtype=np.float32) / np.float32(32))).astype(np.float32)
        ang = np.concatenate([row[:, None] * inv, col[:, None] * inv], axis=-1).astype(np.float32)
        _ROPE = (np.cos(ang).astype(np.float32), np.sin(ang).astype(np.float32))
    return _ROPE


_PROG = {}


def kernel(**inputs):
    n_cores = 8
    if "full" not in _PROG:
        _PROG["full"] = build_program()[0]
    nc = _PROG["full"]
    cos, sin = rope_tables()
    shared = {name: np.ascontiguousarray(inputs[name], dtype=np.float32) for name, _ in WEIGHT_SPECS}
    shared["c_ctx"] = np.ascontiguousarray(inputs["c_ctx"], dtype=np.float32)
    shared["rope_cos"] = cos
    shared["rope_sin"] = sin
    in_maps = []
    for cid in range(n_cores):
        m = dict(shared)
        sl = slice(cid * NB, (cid + 1) * NB)
        m["x"] = np.ascontiguousarray(inputs["x"][sl], dtype=np.float32)
        m["c"] = np.ascontiguousarray(inputs["c"][sl], dtype=np.float32)
        m["ctx"] = np.ascontiguousarray(inputs["ctx"][sl], dtype=np.float32)
        in_maps.append(m)
    res = run_bass_kernel_spmd(nc, in_maps, core_ids=list(range(n_cores)))
    return np.concatenate([np.asarray(r["out"]) for r in res.results], axis=0).astype(np.float32)
```
